# Optimizing a Trainium2 kernel written in Bass

```python
import math
import jax
import jax.numpy as jnp
from jax import lax
import numpy as np

D_MODEL = 1024
BATCH = 8
SEQ = 2048
DEPTH = 4

GRID_W = 64
CTX_LEN = 256
N_MIXERS = 3

DN_ALPHA = (2 * DEPTH) ** 0.25
DN_BETA = (8 * DEPTH) ** -0.25
LN_EPS = 1e-5

DA_HEAD_DIM = 64
DA_HEADS = D_MODEL // (2 * DA_HEAD_DIM)
DA_V_DIM = 2 * DA_HEAD_DIM
Q_BLOCK = 128
ROPE_BASE = 10000.0

S5_GROUP = 16
S5_GROUPS = D_MODEL // S5_GROUP
S5_STATE = 64
DT_MIN = 1e-3
DT_MAX = 1e-1

LRU_WIDTH = 1408
LRU_BLOCKS = 16
LRU_BLOCK = LRU_WIDTH // LRU_BLOCKS
LRU_C = 8.0
CONV_W = 4

N_EXPERTS = 16
N_GROUPS = 4
EXPERTS_PER_GROUP = N_EXPERTS // N_GROUPS
TOP_K = 2
D_EXPERT = 1024
MOE_BLOCK = 256

N_ATTN = (DEPTH + 2) // 3
N_S5 = (DEPTH + 1) // 3
N_LRU = DEPTH // 3

kernel_name = "hybrid_diffattn_s5_rglru_grouped_moe_trunk"


def _layer_norm(x, g, b):
    xf = x.astype(jnp.float32)
    mu = jnp.mean(xf, -1, keepdims=True)
    var = jnp.mean(jnp.square(xf - mu), -1, keepdims=True)
    return ((xf - mu) * lax.rsqrt(var + LN_EPS) * g.astype(jnp.float32) + b.astype(jnp.float32)).astype(x.dtype)


def _modulate(h, shift, scale):
    return h * (1 + scale) + shift


def _rope_1d(x, pos):
    half = x.shape[-1] // 2
    freq = ROPE_BASE ** (-jnp.arange(half, dtype=jnp.float32) / half)
    ang = pos.astype(jnp.float32)[:, None] * freq[None, :]
    cos = jnp.cos(ang)[:, None, None, :]
    sin = jnp.sin(ang)[:, None, None, :]
    x1, x2 = x[..., :half], x[..., half:]
    return jnp.concatenate([x1 * cos - x2 * sin, x2 * cos + x1 * sin], axis=-1)


def _rope_2d(x, row, col):
    q = x.shape[-1] // 2
    out = jnp.concatenate([_rope_1d(x[..., :q], row), _rope_1d(x[..., q:], col)], axis=-1)
    return out.astype(x.dtype)


def _diff_attention(hc, hl, w_in, w_out, lam, subln_g, lam_init, need_ctx):
    B, T, D = hl.shape
    n_rows = T // GRID_W
    row = jnp.repeat(jnp.arange(n_rows), GRID_W)
    col = jnp.tile(jnp.arange(GRID_W), n_rows)

    def proj(h):
        L = h.shape[1]
        u = h @ w_in
        q = u[..., :D].reshape(B, L, 2, DA_HEADS, DA_HEAD_DIM)
        k = u[..., D:2 * D].reshape(B, L, 2, DA_HEADS, DA_HEAD_DIM)
        v = u[..., 2 * D:].reshape(B, L, DA_HEADS, DA_V_DIM)
        return q, k, v

    q_c, k_c, v_c = proj(hc)
    q_l, k_l, v_l = proj(hl)
    q_l = _rope_2d(q_l, row, col)
    k_l = _rope_2d(k_l, row, col)
    k_all = jnp.concatenate([k_c, k_l], axis=1)
    v_all = jnp.concatenate([v_c, v_l], axis=1)

    lf = lam.astype(jnp.float32)
    lam_full = jnp.exp(jnp.sum(lf[0] * lf[1])) - jnp.exp(jnp.sum(lf[2] * lf[3])) + lam_init
    scale = DA_HEAD_DIM ** -0.5

    def attend(q, k, v):
        s = jnp.einsum('bqmhd,bkmhd->bmhqk', q, k).astype(jnp.float32) * scale
        p = jax.nn.softmax(s, axis=-1)
        w = p[:, 0] - lam_full * p[:, 1]
        return jnp.einsum('bhqk,bkhe->bqhe', w, v.astype(jnp.float32))

    nb = T // Q_BLOCK
    qb = q_l.reshape(B, nb, Q_BLOCK, 2, DA_HEADS, DA_HEAD_DIM).swapaxes(0, 1)
    o_l = lax.map(lambda qq: attend(qq, k_all, v_all), qb)
    o_l = o_l.swapaxes(0, 1).reshape(B, T, DA_HEADS, DA_V_DIM)

    def finish(o):
        ms = jnp.mean(jnp.square(o), -1, keepdims=True)
        o = o * lax.rsqrt(ms + LN_EPS) * subln_g.astype(jnp.float32) * (1.0 - lam_init)
        return o.reshape(o.shape[0], o.shape[1], D).astype(hl.dtype) @ w_out

    y_l = finish(o_l)
    y_c = finish(attend(q_c, k_c, v_c)) if need_ctx else None
    return y_c, y_l


def _cmul(ar, ai, br, bi):
    return ar * br - ai * bi, ar * bi + ai * br


def _complex_op(l, r):
    lar, lai, lbr, lbi = l
    rar, rai, rbr, rbi = r
    ar, ai = _cmul(lar, lai, rar, rai)
    br, bi = _cmul(rar, rai, lbr, lbi)
    return ar, ai, br + rbr, bi + rbi


def _complex_linear_scan(ar, ai, br, bi, h0r, h0i, reverse):
    idx = -1 if reverse else 0
    add_r, add_i = _cmul(ar[:, idx], ai[:, idx], h0r, h0i)
    br = br.at[:, idx].add(add_r)
    bi = bi.at[:, idx].add(add_i)
    _, _, hr, hi = lax.associative_scan(_complex_op, (ar, ai, br, bi), reverse=reverse, axis=1)
    return hr, hi


def _real_op(l, r):
    return l[0] * r[0], r[0] * l[1] + r[1]


def _real_linear_scan(a, b, h0, reverse):
    idx = -1 if reverse else 0
    b = b.at[:, idx].add(a[:, idx] * h0)
    _, h = lax.associative_scan(_real_op, (a, b), reverse=reverse, axis=1)
    return h


def _s5_mixer(hc, hl, w_in, a_re, a_im, b_re, b_im, c_re, c_im, log_dt, d_skip, glu_v, glu_g, need_ctx):
    B, T, D = hl.shape
    Cn = hc.shape[1]
    f32 = jnp.float32
    uc = (hc @ w_in).astype(f32).reshape(B, Cn, S5_GROUPS, S5_GROUP)
    ul = (hl @ w_in).astype(f32).reshape(B, T, S5_GROUPS, S5_GROUP)
    y_l = d_skip.astype(f32) * ul.reshape(B, T, D)
    y_c = d_skip.astype(f32) * uc.reshape(B, Cn, D)
    for d, reverse in ((0, False), (1, True)):
        dt = jnp.exp(log_dt[d].astype(f32))[:, None]
        Ar, Ai = a_re[d].astype(f32), a_im[d].astype(f32)
        mag = jnp.exp(Ar * dt)
        zr, zi = mag * jnp.cos(Ai * dt), mag * jnp.sin(Ai * dt)
        den = Ar * Ar + Ai * Ai
        kr = ((zr - 1.0) * Ar + zi * Ai) / den
        ki = (zi * Ar - (zr - 1.0) * Ai) / den
        bbr, bbi = _cmul(kr[..., None], ki[..., None], b_re[d].astype(f32), b_im[d].astype(f32))
        cr, ci = c_re[d].astype(f32), c_im[d].astype(f32)

        def drive(u):
            return (jnp.einsum('btgn,gpn->btgp', u, bbr), jnp.einsum('btgn,gpn->btgp', u, bbi))

        def decay(L):
            shp = (1, L, S5_GROUPS, S5_STATE)
            return jnp.broadcast_to(zr, shp), jnp.broadcast_to(zi, shp)

        def readout(hr, hi):
            y = jnp.einsum('btgp,gnp->btgn', hr, cr) - jnp.einsum('btgp,gnp->btgn', hi, ci)
            return y.reshape(y.shape[0], y.shape[1], D)

        zero = jnp.zeros((B, S5_GROUPS, S5_STATE), f32)
        hcr, hci = _complex_linear_scan(*decay(Cn), *drive(uc), zero, zero, reverse)
        fidx = 0 if reverse else -1
        hlr, hli = _complex_linear_scan(*decay(T), *drive(ul), hcr[:, fidx], hci[:, fidx], reverse)
        y_l = y_l + readout(hlr, hli)
        if need_ctx:
            y_c = y_c + readout(hcr, hci)

    def glu(y):
        g = jax.nn.gelu(y).astype(hl.dtype)
        return (g @ glu_v) * jax.nn.sigmoid(g @ glu_g)

    return (glu(y_c) if need_ctx else None), glu(y_l)


def _block_diag(x, w):
    xs = x.reshape(x.shape[0], x.shape[1], LRU_BLOCKS, LRU_BLOCK)
    return jnp.einsum('btnd,nde->btne', xs, w).reshape(x.shape)


def _depthwise_conv(x, w, b):
    y = lax.conv_general_dilated(x, w[:, None, :], window_strides=(1,), padding=[(1, 2)],
                                 dimension_numbers=('NWC', 'WIO', 'NWC'),
                                 feature_group_count=x.shape[-1])
    return y + b


def _rglru_mixer(hc, hl, w_in, conv_w, conv_b, w_a, b_a, w_x, b_x, lam, w_out, need_ctx):
    B = hl.shape[0]
    f32 = jnp.float32

    def branches(h):
        u = h @ w_in
        return jax.nn.gelu(u[..., :LRU_WIDTH]), _depthwise_conv(u[..., LRU_WIDTH:], conv_w, conv_b)

    gc, xc = branches(hc)
    gl, xl = branches(hl)
    hsum_l = 0.0
    hsum_c = 0.0
    for d, reverse in ((0, False), (1, True)):
        def coeffs(xs):
            r = jax.nn.sigmoid(_block_diag(xs, w_a[d]).astype(f32) + b_a[d].astype(f32))
            i = jax.nn.sigmoid(_block_diag(xs, w_x[d]).astype(f32) + b_x[d].astype(f32))
            log_a = -LRU_C * r * jax.nn.softplus(-lam[d].astype(f32))
            return jnp.exp(log_a), jnp.sqrt(-jnp.expm1(2.0 * log_a)) * (i * xs.astype(f32))

        ac, bc = coeffs(xc)
        hcs = _real_linear_scan(ac, bc, jnp.zeros((B, LRU_WIDTH), f32), reverse)
        h0 = hcs[:, 0 if reverse else -1]
        al, bl = coeffs(xl)
        hsum_l = hsum_l + _real_linear_scan(al, bl, h0, reverse)
        if need_ctx:
            hsum_c = hsum_c + hcs
    y_l = (hsum_l.astype(hl.dtype) * gl) @ w_out
    y_c = ((hsum_c.astype(hc.dtype) * gc) @ w_out) if need_ctx else None
    return y_c, y_l


def _moe(h, router_w, router_b, w_gate, w_up, w_down):
    N, D = h.shape
    probs = jax.nn.softmax((h @ router_w).astype(jnp.float32), axis=-1)
    sel = probs + router_b.astype(jnp.float32)
    grp = sel.reshape(N, N_GROUPS, EXPERTS_PER_GROUP)
    g_idx = jnp.argmax(lax.top_k(grp, TOP_K)[0].sum(-1), axis=-1)
    in_grp = (jnp.arange(N_GROUPS)[None, :] == g_idx[:, None])[:, :, None]
    masked = jnp.where(in_grp, grp, -jnp.inf).reshape(N, N_EXPERTS)
    _, e_idx = lax.top_k(masked, TOP_K)
    w = jnp.take_along_axis(probs, e_idx, axis=1)
    w = w / jnp.sum(w, -1, keepdims=True)

    A = N * TOP_K
    e_flat = e_idx.reshape(-1)
    order = jnp.argsort(e_flat)
    es = e_flat[order]
    counts = jnp.bincount(e_flat, length=N_EXPERTS)
    padded = ((counts + MOE_BLOCK - 1) // MOE_BLOCK) * MOE_BLOCK
    pend = jnp.cumsum(padded)
    pstart = pend - padded
    cstart = jnp.cumsum(counts) - counts
    dest = pstart[es] + jnp.arange(A) - cstart[es]
    n_blk = -(-A // MOE_BLOCK) + N_EXPERTS
    R = n_blk * MOE_BLOCK
    row_tok = jnp.full((R,), N, jnp.int32).at[dest].set((order // TOP_K).astype(jnp.int32))
    row_w = jnp.zeros((R,), jnp.float32).at[dest].set(w.reshape(-1)[order])
    blk_e = jnp.minimum(jnp.searchsorted(pend, jnp.arange(n_blk) * MOE_BLOCK, side='right'), N_EXPERTS - 1)
    h_pad = jnp.concatenate([h, jnp.zeros((1, D), h.dtype)], axis=0)
    xr = h_pad[row_tok].reshape(n_blk, MOE_BLOCK, D)

    def expert_block(args):
        xb, e = args
        return (jax.nn.silu(xb @ w_gate[e]) * (xb @ w_up[e])) @ w_down[e]

    yr = lax.map(expert_block, (xr, blk_e)).reshape(R, D)
    y = jnp.zeros((N + 1, D), yr.dtype).at[row_tok].add(yr * row_w[:, None].astype(yr.dtype))
    return y[:N]


def setup_inputs(seed: int = 0) -> dict:
    key = jax.random.key(seed)
    ks = iter(jax.random.split(key, 48))
    f32 = jnp.float32

    def nrm(shape, scale):
        return jax.random.normal(next(ks), shape, f32) * scale

    D = D_MODEL
    G, P = S5_GROUPS, S5_STATE
    W = LRU_WIDTH
    inp = {}
    inp["x"] = nrm((BATCH, SEQ, D), 1.0)
    inp["c"] = nrm((BATCH, D), 1.0)
    inp["ctx"] = nrm((BATCH, CTX_LEN, D), 1.0)
    inp["c_ctx"] = nrm((D,), 1.0)
    inp["w_ada"] = nrm((DEPTH, D, 6 * D), 0.5 * D ** -0.5)
    inp["b_ada"] = nrm((DEPTH, 6 * D), 0.02)
    inp["ln_g"] = 1.0 + nrm((DEPTH, 2, D), 0.02)
    inp["ln_b"] = nrm((DEPTH, 2, D), 0.02)
    inp["attn_w_in"] = nrm((N_ATTN, D, 3 * D), D ** -0.5)
    inp["attn_w_out"] = nrm((N_ATTN, D, D), D ** -0.5 * DN_BETA)
    inp["attn_lam"] = nrm((N_ATTN, 4, DA_HEAD_DIM), 0.1)
    inp["attn_subln"] = 1.0 + nrm((N_ATTN, DA_V_DIM), 0.02)
    inp["s5_w_in"] = nrm((N_S5, D, D), D ** -0.5)
    inp["s5_a_re"] = -0.5 + nrm((N_S5, 2, G, P), 0.01)
    inp["s5_a_im"] = math.pi * jnp.arange(P, dtype=f32) + nrm((N_S5, 2, G, P), 0.01)
    inp["s5_b_re"] = nrm((N_S5, 2, G, P, S5_GROUP), (2 * S5_GROUP) ** -0.5)
    inp["s5_b_im"] = nrm((N_S5, 2, G, P, S5_GROUP), (2 * S5_GROUP) ** -0.5)
    inp["s5_c_re"] = nrm((N_S5, 2, G, S5_GROUP, P), (2 * P) ** -0.5)
    inp["s5_c_im"] = nrm((N_S5, 2, G, S5_GROUP, P), (2 * P) ** -0.5)
    inp["s5_log_dt"] = jax.random.uniform(next(ks), (N_S5, 2, G), f32, math.log(DT_MIN), math.log(DT_MAX))
    inp["s5_d"] = nrm((N_S5, D), 1.0)
    inp["s5_glu_v"] = nrm((N_S5, D, D), D ** -0.5 * DN_BETA)
    inp["s5_glu_g"] = nrm((N_S5, D, D), D ** -0.5)
    inp["lru_w_in"] = nrm((N_LRU, D, 2 * W), D ** -0.5)
    inp["lru_conv_w"] = nrm((N_LRU, CONV_W, W), CONV_W ** -0.5)
    inp["lru_conv_b"] = nrm((N_LRU, W), 0.02)
    inp["lru_w_a"] = nrm((N_LRU, 2, LRU_BLOCKS, LRU_BLOCK, LRU_BLOCK), LRU_BLOCK ** -0.5)
    inp["lru_b_a"] = nrm((N_LRU, 2, W), 0.02)
    inp["lru_w_x"] = nrm((N_LRU, 2, LRU_BLOCKS, LRU_BLOCK, LRU_BLOCK), LRU_BLOCK ** -0.5)
    inp["lru_b_x"] = nrm((N_LRU, 2, W), 0.02)
    u = jax.random.uniform(next(ks), (N_LRU, 2, W), f32, 0.81, 0.998)
    s = u ** (1.0 / LRU_C)
    inp["lru_lam"] = jnp.log(s) - jnp.log1p(-s)
    inp["lru_w_out"] = nrm((N_LRU, W, D), W ** -0.5 * DN_BETA)
    inp["router_w"] = nrm((D, N_EXPERTS), D ** -0.5)
    inp["router_b"] = nrm((N_EXPERTS,), 0.01)
    inp["moe_w_gate"] = nrm((DEPTH, N_EXPERTS, D, D_EXPERT), D ** -0.5)
    inp["moe_w_up"] = nrm((DEPTH, N_EXPERTS, D, D_EXPERT), D ** -0.5)
    inp["moe_w_down"] = nrm((DEPTH, N_EXPERTS, D_EXPERT, D), D_EXPERT ** -0.5 * DN_BETA)
    return inp


def reference(x, c, ctx, c_ctx, w_ada, b_ada, ln_g, ln_b,
              attn_w_in, attn_w_out, attn_lam, attn_subln,
              s5_w_in, s5_a_re, s5_a_im, s5_b_re, s5_b_im, s5_c_re, s5_c_im, s5_log_dt, s5_d, s5_glu_v, s5_glu_g,
              lru_w_in, lru_conv_w, lru_conv_b, lru_w_a, lru_b_a, lru_w_x, lru_b_x, lru_lam, lru_w_out,
              router_w, router_b, moe_w_gate, moe_w_up, moe_w_down):
    B, T, D = x.shape
    Cn = ctx.shape[1]
    s_lat = jax.nn.silu(c)
    s_ctx = jax.nn.silu(c_ctx)
    for i in range(DEPTH):
        kind, slot, last = i % N_MIXERS, i // N_MIXERS, i == DEPTH - 1
        need_ctx = not last
        m_l = (s_lat @ w_ada[i] + b_ada[i])[:, None, :]
        m_c = s_ctx @ w_ada[i] + b_ada[i]
        sh1_l, sc1_l, g1_l, sh2_l, sc2_l, g2_l = jnp.split(m_l, 6, axis=-1)
        sh1_c, sc1_c, g1_c, sh2_c, sc2_c, g2_c = jnp.split(m_c, 6, axis=-1)

        hl = _modulate(x, sh1_l, sc1_l)
        hc = _modulate(ctx, sh1_c, sc1_c)
        if kind == 0:
            lam_init = 0.8 - 0.6 * math.exp(-0.3 * i)
            yc, yl = _diff_attention(hc, hl, attn_w_in[slot], attn_w_out[slot], attn_lam[slot],
                                     attn_subln[slot], lam_init, need_ctx)
        elif kind == 1:
            yc, yl = _s5_mixer(hc, hl, s5_w_in[slot], s5_a_re[slot], s5_a_im[slot], s5_b_re[slot],
                               s5_b_im[slot], s5_c_re[slot], s5_c_im[slot], s5_log_dt[slot], s5_d[slot],
                               s5_glu_v[slot], s5_glu_g[slot], need_ctx)
        else:
            yc, yl = _rglru_mixer(hc, hl, lru_w_in[slot], lru_conv_w[slot], lru_conv_b[slot], lru_w_a[slot],
                                  lru_b_a[slot], lru_w_x[slot], lru_b_x[slot], lru_lam[slot],
                                  lru_w_out[slot], need_ctx)
        x = _layer_norm(DN_ALPHA * x + g1_l * yl, ln_g[i, 0], ln_b[i, 0])
        if need_ctx:
            ctx = _layer_norm(DN_ALPHA * ctx + g1_c * yc, ln_g[i, 0], ln_b[i, 0])

        hl = _modulate(x, sh2_l, sc2_l)
        if need_ctx:
            hc = _modulate(ctx, sh2_c, sc2_c)
            tok = jnp.concatenate([hc, hl], axis=1).reshape(-1, D)
            y = _moe(tok, router_w, router_b, moe_w_gate[i], moe_w_up[i], moe_w_down[i]).reshape(B, Cn + T, D)
            ctx = _layer_norm(DN_ALPHA * ctx + g2_c * y[:, :Cn], ln_g[i, 1], ln_b[i, 1])
            yl = y[:, Cn:]
        else:
            yl = _moe(hl.reshape(-1, D), router_w, router_b, moe_w_gate[i], moe_w_up[i],
                      moe_w_down[i]).reshape(B, T, D)
        x = _layer_norm(DN_ALPHA * x + g2_l * yl, ln_g[i, 1], ln_b[i, 1])
    return x
```

```python
import contextlib
import math
import numpy as np
import concourse.bass as bass
import concourse.mybir as mybir
from concourse.bass_utils import run_bass_kernel_spmd

F32 = mybir.dt.float32
BF16 = mybir.dt.bfloat16
I32 = mybir.dt.int32
ALU = mybir.AluOpType
AF = mybir.ActivationFunctionType
AX = mybir.AxisListType

D = 1024
T_LAT = 2048
T_CTX = 256
T_ALL = T_LAT + T_CTX
NT = T_ALL // 128
DEPTH = 4
DN_ALPHA = (2 * DEPTH) ** 0.25
LN_EPS = 1e-5
N_EXP = 16
C_CAP = 1024
LRU_W = 1408
NCORES = 8


class Dep:
    __slots__ = ("w", "r")

    def __init__(self):
        self.w = None
        self.r = {}


class KB:
    EPOCH = 20000
    NPOOL = 8

    def __init__(self, nc):
        self.nc = nc
        self.stack = contextlib.ExitStack()
        self.ops = {e: [] for e in ("sp", "pool", "act", "dve", "pe")}
        self.cnt = {e: 0 for e in self.ops}
        self.esems = {e: [] for e in self.ops}
        self.known = {e: {} for e in self.ops}
        self.dma_sems = {}
        self.dma_cnt = {}
        self.dma_i = {e: 0 for e in self.ops}
        self.last_tok = {}
        self.nsem = 0
        self.ntens = 0
        self.out_tokens = []

    def sem(self, name):
        self.nsem += 1
        return self.stack.enter_context(self.nc.semaphore(f"{name}_{self.nsem}"))

    def sbuf(self, shape, dtype, name="t"):
        self.ntens += 1
        return self.stack.enter_context(self.nc.sbuf_tensor(f"{name}_{self.ntens}", list(shape), dtype))

    def psum(self, shape, dtype, name="ps"):
        self.ntens += 1
        return self.stack.enter_context(self.nc.psum_tensor(f"{name}_{self.ntens}", list(shape), dtype))

    def _need(self, eng, tok, waits):
        sem, val, _ = tok
        k = self.known[eng]
        key = id(sem)
        if k.get(key, (None, 0))[1] >= val:
            return
        k[key] = (sem, val)
        for i, (s, v) in enumerate(waits):
            if s is sem:
                waits[i] = (s, max(v, val))
                return
        waits.append((sem, val))

    def _collect(self, eng, reads, writes):
        waits = []
        for d in reads:
            if d.w is not None:
                if d.w[2] == eng and eng == "pe":
                    continue
                self._need(eng, d.w, waits)
        for d in writes:
            if d.w is not None and d.w[2] != eng:
                self._need(eng, d.w, waits)
            for t in d.r.values():
                if t[2] != eng:
                    self._need(eng, t, waits)
        return waits

    def _commit(self, tok, reads, writes):
        for d in reads:
            d.r[id(tok[0])] = tok
        for d in writes:
            d.w = tok
            d.r = {}
        self.last_tok[id(tok[0])] = tok

    def op(self, eng, fn, reads=(), writes=()):
        waits = self._collect(eng, reads, writes)
        c = self.cnt[eng]
        ep = c // self.EPOCH
        while len(self.esems[eng]) <= ep:
            self.esems[eng].append(self.sem(f"e_{eng}"))
        sem = self.esems[eng][ep]
        val = c % self.EPOCH + 1
        self.cnt[eng] = c + 1
        tok = (sem, val, eng)
        self.ops[eng].append((waits, fn, sem, 1))
        self._commit(tok, reads, writes)
        return tok

    def dma(self, q, fn, reads=(), writes=(), is_output=False):
        waits = self._collect(q, reads, writes)
        if q not in self.dma_sems:
            self.dma_sems[q] = [self.sem(f"d_{q}") for _ in range(self.NPOOL)]
            self.dma_cnt[q] = [0] * self.NPOOL
        i = self.dma_i[q] % self.NPOOL
        self.dma_i[q] += 1
        sem = self.dma_sems[q][i]
        prev = self.dma_cnt[q][i]
        if prev > 0:
            self._need(q, (sem, prev, "dma"), waits)
        self.dma_cnt[q][i] = prev + 16
        tok = (sem, prev + 16, "dma")
        self.ops[q].append((waits, fn, sem, 16))
        self._commit(tok, reads, writes)
        if is_output:
            self.out_tokens.append(tok)
        return tok

    def barrier(self):
        toks = list(self.last_tok.values())
        for eng in self.ops:
            waits = []
            for t in toks:
                self._need(eng, t, waits)
            if waits:
                self.ops[eng].append((waits, None, None, 0))

    def finish(self):
        waits = []
        for t in self.out_tokens:
            self._need("sp", t, waits)
        if waits:
            self.ops["sp"].append((waits, None, None, 0))
        nc = self.nc
        with nc.Block() as block:
            for name, deco in (("sp", block.sync), ("pool", block.gpsimd), ("act", block.scalar),
                               ("dve", block.vector), ("pe", block.tensor)):
                ops = self.ops[name]

                def body(e, ops=ops):
                    for waits, fn, sem, inc in ops:
                        for s, v in waits:
                            e.wait_ge(s, v)
                        if fn is None:
                            continue
                        ins = fn(e)
                        ins.then_inc(sem, inc)

                deco(body)
        self.stack.close()


class Tl:
    def __init__(self, ap):
        self.ap = ap
        self.ds = {}

    def d(self, key=0):
        if key not in self.ds:
            self.ds[key] = Dep()
        return self.ds[key]

    def dl(self, keys):
        return [self.d(k) for k in keys]


class Arena:
    def __init__(self, tens, n):
        self.t = tens
        self.n = n
        self.off = 0

    def alloc(self, n, shape=None):
        n2 = (n + 15) // 16 * 16
        assert self.off + n2 <= self.n, (self.off, n2, self.n)
        ap = self.t[:, self.off:self.off + n]
        self.off += n2
        if shape is not None:
            names = " ".join(f"a{i}" for i in range(len(shape)))
            kw = {f"a{i}": s for i, s in enumerate(shape)}
            ap = ap.rearrange(f"p ({names}) -> p {names}", **kw)
        return Tl(ap)

    def mark(self):
        return self.off

    def release(self, m):
        self.off = m


class Prog:
    def __init__(self, debug=None, layers=None):
        self.debug = debug or []
        self.layers = list(range(DEPTH)) if layers is None else list(layers)
        nc = bass.Bass("TRN2", target_bir_lowering=False)
        self.nc = nc
        self.kb = KB(nc)
        kb = self.kb
        self.inp = {}
        self.dr = {}
        self.dd = {}
        NB, NF = 64000, 18500
        self.AB = Arena(kb.sbuf([128, NB], BF16, "arb"), NB)
        self.AF_ = Arena(kb.sbuf([128, NF], F32, "arf"), NF)
        self.AI = Arena(kb.sbuf([128, 1280], I32, "ari"), 1280)
        self.ps = [Tl(kb.psum([128, 512], F32, f"ps{i}")[:]) for i in range(7)]
        self.psb = [Tl(kb.psum([128, 1024], BF16, f"psb{i}")[:]) for i in range(1)]
        self.ps_n = 7
        self.ps_i = 0
        self.psb_i = 0
        self.ev_i = 0

    def din(self, name, shape, dtype=F32):
        self.inp[name] = self.nc.dram_tensor(name, list(shape), dtype, kind="ExternalInput").ap()
        return self.inp[name]

    def dscr(self, name, shape, dtype=F32):
        kind = "ExternalOutput" if name in self.debug else "Internal"
        self.dr[name] = self.nc.dram_tensor(name, list(shape), dtype, kind=kind).ap()
        self.dd[name] = {}
        return self.dr[name]

    def ddep(self, name, key=0):
        dd = self.dd.setdefault(name, {})
        if key not in dd:
            dd[key] = Dep()
        return dd[key]

    def bound_reg(self, e):
        if getattr(self, "_breg", None) is None:
            self._breg = e.to_reg(N_EXP * C_CAP - 1)
        return self._breg

    def nps(self):
        p = self.ps[self.ps_i % self.ps_n]
        self.ps_i += 1
        return p

    def npsb(self):
        p = self.psb[self.psb_i % len(self.psb)]
        self.psb_i += 1
        return p

    def ev(self):
        self.ev_i += 1
        return "act" if self.ev_i % 2 else "dve"

    def tt(self, eng, out, in0, in1, op, r, w):
        self.kb.op(eng, lambda e: e.tensor_tensor(out=out, in0=in0, in1=in1, op=op), r, w)

    def ts(self, eng, out, in0, s1, s2, op0, op1, r, w):
        if s2 is None:
            self.kb.op(eng, lambda e: e.tensor_scalar(out=out, in0=in0, scalar1=s1, scalar2=None, op0=op0), r, w)
        else:
            self.kb.op(eng, lambda e: e.tensor_scalar(out=out, in0=in0, scalar1=s1, scalar2=s2, op0=op0, op1=op1), r, w)

    def stt(self, out, in0, sc, in1, op0, op1, r, w):
        self.kb.op("dve", lambda e: e.scalar_tensor_tensor(out=out, in0=in0, scalar=sc, in1=in1, op0=op0, op1=op1), r, w)

    def act(self, out, in_, func, r, w, bias=None, scale=None, accum=None):
        kw = {}
        if bias is not None:
            kw["bias"] = bias
        if scale is not None:
            kw["scale"] = scale
        if accum is not None:
            kw["accum_out"] = accum
        self.kb.op("act", lambda e: e.activation(out=out, in_=in_, func=func, **kw), r, w)

    def red(self, kind, out, in_, r, w):
        if kind == "max":
            self.kb.op("dve", lambda e: e.reduce_max(out=out, in_=in_, axis=AX.X), r, w)
        else:
            self.kb.op("dve", lambda e: e.reduce_sum(out=out, in_=in_, axis=AX.X), r, w)

    def recip(self, out, in_, r, w):
        self.kb.op("dve", lambda e: e.reciprocal(out=out, in_=in_), r, w)

    def scan(self, out, d0, d1, init, r, w):
        self.kb.op("dve", lambda e: e.tensor_tensor_scan(out=out, data0=d0, data1=d1, initial=init, op0=ALU.mult, op1=ALU.add), r, w)

    def gelu_tanh(self, out, src, srcdeps, wk, outdeps):
        (xs, xsap), (t, tap) = wk
        self.cp("act", xsap, src, srcdeps, [xs.d()])
        self.tt("dve", tap, xsap, xsap, ALU.mult, [xs.d()], [t.d()])
        self.ts("dve", tap, tap, 0.044715, 1.0, ALU.mult, ALU.add, [t.d()], [t.d()])
        self.tt("dve", tap, tap, xsap, ALU.mult, [t.d(), xs.d()], [t.d()])
        self.act(tap, tap, AF.Sigmoid, [t.d()], [t.d()], scale=1.5957691216057308)
        self.tt("pool", out, xsap, tap, ALU.mult, [xs.d(), t.d()], outdeps)

    def sincos(self, ang, cos_out, sin_out, tmpf, tmpi, r, w):
        TWO_PI = 2.0 * math.pi
        for out, shift in ((sin_out, 0.0), (cos_out, 0.25)):
            self.ts("dve", tmpf, ang, 1.0 / TWO_PI, shift, ALU.mult, ALU.add, r, w)
            self.cp("dve", tmpi, tmpf, w, w)
            self.cp("dve", out, tmpi, w, w)
            self.tt("dve", tmpf, tmpf, out, ALU.subtract, w, w)
            self.ts("dve", out, tmpf, 0.5, None, ALU.is_gt, None, w, w)
            self.tt("dve", tmpf, tmpf, out, ALU.subtract, w, w)
            self.ts("dve", out, tmpf, -0.5, None, ALU.is_lt, None, w, w)
            self.tt("dve", tmpf, tmpf, out, ALU.add, w, w)
            self.act(out, tmpf, AF.Sin, w, w, scale=TWO_PI * (1.0 - 1e-6))

    def cp(self, eng, out, in_, r, w):
        if eng == "act":
            self.kb.op("act", lambda e: e.activation(out=out, in_=in_, func=AF.Copy), r, w)
        else:
            self.kb.op(eng, lambda e: e.tensor_copy(out=out, in_=in_), r, w)

    def mm(self, out, lhsT, rhs, start, stop, r, w):
        self.kb.op("pe", lambda e: e.matmul(out, lhsT=lhsT, rhs=rhs, start=start, stop=stop), r, w)

    def tr(self, out, in_, ident, r, w):
        self.kb.op("pe", lambda e: e.transpose(out, in_, ident), r, w)

    def dma(self, q, out, in_, r, w, is_output=False, slow=False):
        if slow:
            self.kb.dma(q, lambda e: e.dma_start(out=out, in_=in_, allow_slow_non_contiguous=True), r, w, is_output=is_output)
        else:
            self.kb.dma(q, lambda e: e.dma_start(out=out, in_=in_), r, w, is_output=is_output)

    def memset(self, eng, ap, val, w):
        self.kb.op(eng, lambda e: e.memset(ap, val), (), w)

    def build(self):
        kb = self.kb
        din, dscr = self.din, self.dscr
        x_in = din("x", [T_LAT, D])
        ctx_in = din("ctx", [T_CTX, D])
        cc_in = din("cc", [2, D])
        w_ada = din("w_ada", [DEPTH, D, 6 * D])
        b_ada = din("b_ada", [DEPTH, 6 * D])
        ln_g = din("ln_g", [DEPTH, 2, D])
        ln_b = din("ln_b", [DEPTH, 2, D])
        din("attn_w_in", [2, D, 3 * D])
        din("attn_w_in_p", [2, D, 2 * D])
        din("attn_w_out", [2, D, D])
        din("attn_lam", [2, 256])
        din("attn_subln", [2, 128])
        din("router_w", [D, N_EXP])
        din("router_b", [1, N_EXP])
        din("moe_w_gate", [DEPTH, N_EXP, D, D])
        din("moe_w_up", [DEPTH, N_EXP, D, D])
        din("moe_w_down", [DEPTH, N_EXP, D, D])
        din("s5_w_in", [1, D, D])
        for nm in ("s5_a_re", "s5_a_im"):
            din(nm, [1, 2, 64, 64])
        for nm in ("s5_b_re", "s5_b_im"):
            din(nm, [1, 2, 64, 64, 16])
        for nm in ("s5_c_re", "s5_c_im"):
            din(nm, [1, 2, 64, 16, 64])
        din("s5_log_dt", [1, 2, 64])
        din("s5_d", [1, D])
        din("s5_glu_v", [1, D, D])
        din("s5_glu_g", [1, D, D])
        din("lru_w_in", [1, D, 2 * LRU_W])
        din("lru_conv_w", [1, 4, LRU_W])
        din("lru_conv_b", [1, LRU_W])
        din("lru_w_a", [1, 2, 16, 88, 88])
        din("lru_b_a", [1, 2, LRU_W])
        din("lru_w_x", [1, 2, 16, 88, 88])
        din("lru_b_x", [1, 2, LRU_W])
        din("lru_lam", [1, 2, LRU_W])
        din("lru_w_out", [1, LRU_W, D])
        din("k_tau", [128, 32])
        din("k_ident", [128, 128])
        din("k_ltri", [128, 128])
        din("k_eoff", [128, N_EXP])
        din("k_cos", [128, T_ALL])
        din("k_sin", [128, T_ALL])
        out = self.nc.dram_tensor("out", [T_LAT, D], F32, kind="ExternalOutput").ap()
        xs = dscr("xs", [T_ALL, D])
        mod = dscr("mod", [DEPTH, 2, 6 * D])
        dscr("Xg", [N_EXP * C_CAP, D], BF16)
        dscr("Yg", [N_EXP * C_CAP, D], F32)
        dscr("gT", [11, 128, T_ALL], BF16)

        AB, AFa = self.AB, self.AF_
        self.ident_f = AFa.alloc(128)
        self.ident_b = AB.alloc(128)
        self.ltri_b = AB.alloc(128)
        self.ones_b = AB.alloc(128)
        self.eoff = AFa.alloc(N_EXP)
        self.rb_bc = AFa.alloc(N_EXP)
        self.rw = AFa.alloc(8 * N_EXP, [8, N_EXP])
        self.dma("sp", self.ident_f.ap, self.inp["k_ident"], [], [self.ident_f.d()])
        self.cp("dve", self.ident_b.ap, self.ident_f.ap, [self.ident_f.d()], [self.ident_b.d()])
        tmpf = AFa.alloc(128)
        self.dma("sp", tmpf.ap, self.inp["k_ltri"], [], [tmpf.d()])
        self.cp("dve", self.ltri_b.ap, tmpf.ap, [tmpf.d()], [self.ltri_b.d()])
        self.memset("dve", self.ones_b.ap, 1.0, [self.ones_b.d()])
        self.ones_f = AFa.alloc(128)
        self.memset("dve", self.ones_f.ap, 1.0, [self.ones_f.d()])
        self.dma("sp", self.eoff.ap, self.inp["k_eoff"], [], [self.eoff.d()])
        self.dma("sp", self.rb_bc.ap, self.inp["router_b"].partition_broadcast(128), [], [self.rb_bc.d()])
        self.dma("sp", self.rw.ap, self.inp["router_w"].rearrange("(kt p) e -> p kt e", p=128), [], [self.rw.d()])
        self.dest_i = Tl(self.AI.t[:, 0:2 * NT].rearrange("p (t k) -> p t k", k=2))
        self.dest_g = Tl(self.AI.t[:, 64:64 + 2 * NT].rearrange("p (t k) -> p t k", k=2))
        self.itmp = Tl(self.AI.t[:, 256:1280])
        self.gatew = AFa.alloc(2 * NT, [NT, 2])
        self.bc = {nm: AFa.alloc(D) for nm in ("sh_l", "sc_l", "g_l", "sh_c", "sc_c", "g_c", "lng", "lnb")}
        self.base_mark_b = AB.mark()
        self.base_mark_f = AFa.mark()

        self.dma("sp", xs[0:T_CTX, :], ctx_in, [], [self.ddep("xs", t) for t in range(2)])
        self.dma("sp", xs[T_CTX:, :], x_in, [], [self.ddep("xs", t) for t in range(2, NT)])

        self.adaln(cc_in, w_ada, b_ada, mod)

        for l in self.layers:
            kind, slot, last = l % 3, l // 3, l == DEPTH - 1
            tiles = list(range(2, NT)) if last else list(range(NT))
            if kind == 0:
                lam_init = 0.8 - 0.6 * math.exp(-0.3 * l)
                self.attention(l, slot, lam_init, last)
            elif kind == 1:
                self.s5(l, slot)
            else:
                self.rglru(l, slot)
            self.moe(l, tiles)

        kb.barrier()
        self.dma("sp", out, xs[T_CTX:, :], [self.ddep("xs", t) for t in range(2, NT)], [], is_output=True)
        kb.finish()
        return self.nc

    def adaln(self, cc_in, w_ada, b_ada, mod):
        AFa = self.AF_
        m = AFa.mark()
        craw = AFa.alloc(16, [8, 2])
        sT = AFa.alloc(16, [8, 2])
        for r in range(2):
            self.dma("sp", craw.ap[:, :, r], cc_in[r:r + 1, :].rearrange("o (kt p) -> p (o kt)", p=128), [], [craw.d()], slow=True)
        self.act(sT.ap, craw.ap, AF.Silu, [craw.d()], [sT.d()])
        bbs = [AFa.alloc(512) for _ in range(2)]
        obs = [AFa.alloc(512) for _ in range(2)]
        wbuf = [AFa.alloc(8 * 256, [8, 256]) for _ in range(2)]
        i = 0
        for l in range(DEPTH):
            for n in range(24):
                wb, bb, ob = wbuf[i % 2], bbs[i % 2], obs[i % 2]
                i += 1
                cs = slice(n * 256, (n + 1) * 256)
                self.dma("sp", bb.ap[0:2, 0:256], b_ada[l:l + 1, cs].partition_broadcast(2), [], [bb.d()])
                self.dma("sp", wb.ap, w_ada[l].rearrange("(kt p) n -> p kt n", p=128)[:, :, cs], [], [wb.d()])
                ps = self.nps()
                for kt in range(8):
                    self.mm(ps.ap[0:2, 0:256], sT.ap[:, kt, :], wb.ap[:, kt, :], kt == 0, kt == 7, [sT.d(), wb.d()], [ps.d()])
                self.tt("dve", ob.ap[0:2, 0:256], ps.ap[0:2, 0:256], bb.ap[0:2, 0:256], ALU.add, [ps.d(), bb.d()], [ob.d()])
                self.dma("sp", mod[l, :, cs], ob.ap[0:2, 0:256], [ob.d()], [self.ddep("mod", l)])
        self.kb.barrier()
        AFa.release(m)

    def load_mod(self, l, sub):
        mod = self.dr["mod"]
        for r, sfx in ((0, "l"), (1, "c")):
            for j, nm in enumerate(("sh", "sc", "g")):
                t = self.bc[f"{nm}_{sfx}"]
                col = (3 * sub + j) * D
                self.dma("sp", t.ap, mod[l, r:r + 1, col:col + D].partition_broadcast(128), [self.ddep("mod", l)], [t.d()])
            t = self.bc[f"sc_{sfx}"]
            self.ts("dve", t.ap, t.ap, 1.0, None, ALU.add, None, [t.d()], [t.d()])
        self.dma("sp", self.bc["lng"].ap, self.inp["ln_g"][l, sub:sub + 1, :].partition_broadcast(128), [], [self.bc["lng"].d()])
        self.dma("sp", self.bc["lnb"].ap, self.inp["ln_b"][l, sub:sub + 1, :].partition_broadcast(128), [], [self.bc["lnb"].d()])

    def modulate(self, t, hx):
        xs = self.dr["xs"]
        sfx = "c" if t < 2 else "l"
        self.dma("sp", hx.ap, xs[t * 128:(t + 1) * 128, :], [self.ddep("xs", t)], [hx.d()])
        sc, sh = self.bc[f"sc_{sfx}"], self.bc[f"sh_{sfx}"]
        self.tt("dve", hx.ap, hx.ap, sc.ap, ALU.mult, [hx.d(), sc.d()], [hx.d()])
        self.tt("pool", hx.ap, hx.ap, sh.ap, ALU.add, [hx.d(), sh.d()], [hx.d()])

    def post(self, t, ysrc, ydeps, work):
        xs = self.dr["xs"]
        sfx = "c" if t < 2 else "l"
        g = self.bc[f"g_{sfx}"]
        xr, t1, st, mv = work
        self.dma("sp", xr.ap, xs[t * 128:(t + 1) * 128, :], [self.ddep("xs", t)], [xr.d()])
        for h in range(2):
            sl = slice(h * 512, (h + 1) * 512)
            self.tt("dve", t1.ap[:, sl], ysrc[h], g.ap[:, sl], ALU.mult, ydeps + [g.d()], [t1.d()])
        self.stt(t1.ap, xr.ap, float(DN_ALPHA), t1.ap, ALU.mult, ALU.add, [xr.d(), t1.d()], [t1.d()])
        for h in range(2):
            self.kb.op("dve", lambda e, h=h: e.bn_stats(out=st.ap[:, h * 6:(h + 1) * 6], in_=t1.ap[:, h * 512:(h + 1) * 512]),
                       [t1.d()], [st.d()])
        self.kb.op("dve", lambda e: e.bn_aggr(out=mv.ap[:, 0:2], in_=st.ap[:, 0:12]), [st.d()], [mv.d()])
        self.ts("dve", mv.ap[:, 2:3], mv.ap[:, 1:2], float(LN_EPS), None, ALU.add, None, [mv.d()], [mv.d()])
        self.act(mv.ap[:, 3:4], mv.ap[:, 2:3], AF.Sqrt, [mv.d()], [mv.d()])
        self.kb.op("dve", lambda e: e.reciprocal(out=mv.ap[:, 4:5], in_=mv.ap[:, 3:4]), [mv.d()], [mv.d()])
        self.ts("dve", t1.ap, t1.ap, mv.ap[:, 0:1], mv.ap[:, 4:5], ALU.subtract, ALU.mult, [t1.d(), mv.d()], [t1.d()])
        self.tt("pool", t1.ap, t1.ap, self.bc["lng"].ap, ALU.mult, [t1.d(), self.bc["lng"].d()], [t1.d()])
        self.tt("dve", t1.ap, t1.ap, self.bc["lnb"].ap, ALU.add, [t1.d(), self.bc["lnb"].d()], [t1.d()])
        self.dma("sp", xs[t * 128:(t + 1) * 128, :], t1.ap, [t1.d()], [self.ddep("xs", t)])

    def post_work(self):
        AFa = self.AF_
        return [(AFa.alloc(D), AFa.alloc(D), AFa.alloc(12), AFa.alloc(8)) for _ in range(2)]

    def transpose_tile(self, hx, dst_b, dst_key, t, dst_f=None):
        for half in range(2):
            ps = self.nps()
            for j in range(4):
                kt = half * 4 + j
                self.tr(ps.ap[:, j * 128:(j + 1) * 128], hx.ap[:, kt * 128:(kt + 1) * 128], self.ident_f.ap,
                        [hx.d(), self.ident_f.d()], [ps.d()])
            src = ps.ap.rearrange("p (j q) -> p j q", j=4)
            self.cp(self.ev(), dst_b.ap[:, half * 4:(half + 1) * 4, t * 128:(t + 1) * 128], src, [ps.d()], [dst_b.d(dst_key)])
            if dst_f is not None:
                self.cp(self.ev(), dst_f.ap[:, half * 4:(half + 1) * 4, :], src, [ps.d()], [dst_f.d()])

    def moe(self, l, tiles):
        kb = self.kb
        AB, AFa = self.AB, self.AF_
        Xg, Yg = self.dr["Xg"], self.dr["Yg"]
        kb.barrier()
        mb, mf = AB.mark(), AFa.mark()
        self.load_mod(l, 1)
        wg = [AB.alloc(8 * D, [8, D]) for _ in range(2)]
        wu = [AB.alloc(8 * D, [8, D]) for _ in range(2)]
        wd = [AB.alloc(8 * D, [8, D]) for _ in range(2)]
        wsrc = {"g": self.inp["moe_w_gate"], "u": self.inp["moe_w_up"], "d": self.inp["moe_w_down"]}

        def load_w(e):
            for W, nm in ((wg[e % 2], "g"), (wu[e % 2], "u"), (wd[e % 2], "d")):
                src = wsrc[nm][l, e].rearrange("(kt p) n -> p kt n", p=128)
                for hh in range(2):
                    self.dma("pool", W.ap[:, hh * 4:(hh + 1) * 4, :], src[:, hh * 4:(hh + 1) * 4, :], [], [W.d()])

        load_w(0)
        mbA = AB.mark()
        hxs = [AFa.alloc(D) for _ in range(2)]
        hbs = [AB.alloc(D) for _ in range(2)]
        hTf = [AFa.alloc(8 * 128, [8, 128]) for _ in range(2)]
        rt = [AFa.alloc(256) for _ in range(2)]
        base = AFa.alloc(N_EXP)
        mask_b = [AB.alloc(N_EXP) for _ in range(2)]
        self.memset("dve", base.ap, 0.0, [base.d()])
        for it, t in enumerate(tiles):
            hx, hb, hf, r, mk = hxs[it % 2], hbs[it % 2], hTf[it % 2], rt[it % 2], mask_b[it % 2]
            R = r.ap
            rd = [r.d()]
            self.modulate(t, hx)
            self.cp("act", hb.ap, hx.ap, [hx.d()], [hb.d()])
            for half in range(2):
                ps = self.nps()
                for j in range(4):
                    kt = half * 4 + j
                    self.tr(ps.ap[:, j * 128:(j + 1) * 128], hx.ap[:, kt * 128:(kt + 1) * 128], self.ident_f.ap,
                            [hx.d(), self.ident_f.d()], [ps.d()])
                self.cp(self.ev(), hf.ap[:, half * 4:(half + 1) * 4, :], ps.ap.rearrange("p (j q) -> p j q", j=4), [ps.d()], [hf.d()])
            pl = self.nps()
            for kt in range(8):
                self.mm(pl.ap[:, 0:N_EXP], hf.ap[:, kt, :], self.rw.ap[:, kt, :], kt == 0, kt == 7, [hf.d(), self.rw.d()], [pl.d()])
            lg, pr, sel, tmp, oh1, oh2 = (R[:, 0:16], R[:, 16:32], R[:, 32:48], R[:, 48:64], R[:, 64:80], R[:, 80:96])
            g4, s6, sc1 = R[:, 96:100], R[:, 100:124], R[:, 124:140]
            mx, sm, rs, m1, m2, p1, p2, ps_, d1, d2 = (R[:, 140 + i:141 + i] for i in range(10))
            self.cp("dve", lg, pl.ap[:, 0:N_EXP], [pl.d()], rd)
            self.red("max", mx, lg, rd, rd)
            self.ts("dve", mx, mx, -1.0, None, ALU.mult, None, rd, rd)
            self.act(pr, lg, AF.Exp, rd, rd, bias=mx, accum=sm)
            self.recip(rs, sm, rd, rd)
            self.ts("dve", pr, pr, rs, None, ALU.mult, None, rd, rd)
            self.tt("dve", sel, pr, self.rb_bc.ap, ALU.add, rd + [self.rb_bc.d()], rd)
            sv = sel.rearrange("p (g k) -> p g k", k=4)
            s6v = s6.rearrange("p (q g) -> p q g", q=6)
            pairs = [(0, 1), (0, 2), (0, 3), (1, 2), (1, 3), (2, 3)]
            for qi, (a, b) in enumerate(pairs):
                self.tt("dve", s6v[:, qi, :], sv[:, :, a], sv[:, :, b], ALU.add, rd, rd)
            self.tt("dve", g4, s6v[:, 0, :], s6v[:, 1, :], ALU.max, rd, rd)
            for qi in range(2, 6):
                self.tt("dve", g4, g4, s6v[:, qi, :], ALU.max, rd, rd)
            self.red("max", mx, g4, rd, rd)
            self.ts("dve", g4, g4, mx, -1e30, ALU.is_lt, ALU.mult, rd, rd)
            tv = tmp.rearrange("p (g k) -> p g k", k=4)
            for k in range(4):
                self.tt("dve", tv[:, :, k], sv[:, :, k], g4, ALU.add, rd, rd)
            self.red("max", m1, tmp, rd, rd)
            self.ts("dve", oh1, tmp, m1, None, ALU.is_ge, None, rd, rd)
            self.stt(sc1, oh1, -1e30, tmp, ALU.mult, ALU.add, rd, rd)
            self.red("max", m2, sc1, rd, rd)
            self.ts("dve", oh2, sc1, m2, None, ALU.is_ge, None, rd, rd)
            self.tt("dve", sc1, pr, oh1, ALU.mult, rd, rd)
            self.red("sum", p1, sc1, rd, rd)
            self.tt("dve", sc1, pr, oh2, ALU.mult, rd, rd)
            self.red("sum", p2, sc1, rd, rd)
            self.tt("dve", ps_, p1, p2, ALU.add, rd, rd)
            self.recip(ps_, ps_, rd, rd)
            gw = self.gatew
            self.tt("dve", gw.ap[:, t, 0:1], p1, ps_, ALU.mult, rd, [gw.d(t)])
            self.tt("dve", gw.ap[:, t, 1:2], p2, ps_, ALU.mult, rd, [gw.d(t)])
            self.tt("dve", mk.ap, oh1, oh2, ALU.add, rd, [mk.d()])
            pc = self.nps()
            self.mm(pc.ap[:, 0:N_EXP], self.ltri_b.ap, mk.ap, True, True, [self.ltri_b.d(), mk.d()], [pc.d()])
            self.mm(pc.ap[:, 16:16 + N_EXP], self.ones_b.ap, mk.ap, True, True, [self.ones_b.d(), mk.d()], [pc.d()])
            self.tt("dve", sc1, pc.ap[:, 0:N_EXP], base.ap, ALU.add, [pc.d(), base.d()], rd)
            self.tt("dve", base.ap, base.ap, pc.ap[:, 16:16 + N_EXP], ALU.add, [pc.d(), base.d()], [base.d()])
            self.ts("dve", lg, sc1, float(C_CAP) - 0.5, 1.0e6, ALU.is_gt, ALU.mult, rd, rd)
            self.tt("dve", sc1, sc1, lg, ALU.add, rd, rd)
            self.tt("dve", sc1, sc1, self.eoff.ap, ALU.add, rd + [self.eoff.d()], rd)
            self.tt("dve", lg, sc1, oh1, ALU.mult, rd, rd)
            self.red("sum", d1, lg, rd, rd)
            self.tt("dve", lg, sc1, oh2, ALU.mult, rd, rd)
            self.red("sum", d2, lg, rd, rd)
            di = self.dest_i
            self.cp("dve", di.ap[:, t, 0:1], d1, rd, [di.d(t)])
            self.cp("dve", di.ap[:, t, 1:2], d2, rd, [di.d(t)])
            dg = self.dest_g
            self.ts("dve", d1, d1, float(N_EXP * C_CAP - 1), None, ALU.min, None, rd, rd)
            self.ts("dve", d2, d2, float(N_EXP * C_CAP - 1), None, ALU.min, None, rd, rd)
            self.cp("dve", dg.ap[:, t, 0:1], d1, rd, [dg.d(t)])
            self.cp("dve", dg.ap[:, t, 1:2], d2, rd, [dg.d(t)])
            for k in range(2):
                self.kb.dma("pool", lambda e, k=k, t=t, hb=hb: e.indirect_dma_start(
                    out=Xg, out_offset=bass.IndirectOffsetOnAxis(ap=di.ap[:, t, k:k + 1], axis=0),
                    in_=hb.ap, in_offset=None, bounds_check=self.bound_reg(e), oob_is_err=False),
                    [di.d(t), hb.d()], [self.ddep("Xg")])
        if "dest" in self.debug:
            pass
        kb.barrier()
        AB.release(mbA)
        AFa.release(mf)
        mf = AFa.mark()
        RB = 256
        xtm = [AB.alloc(2 * D, [2, D]) for _ in range(2)]
        xT = [AB.alloc(8 * RB, [8, RB]) for _ in range(2)]
        hT = [AB.alloc(8 * RB, [8, RB]) for _ in range(2)]
        sg = [AFa.alloc(RB) for _ in range(2)]
        yst = [AFa.alloc(D) for _ in range(2)]
        blocks = [(e, rb) for e in range(N_EXP) for rb in range(C_CAP // RB)]

        def stT(bi):
            e, rb = blocks[bi]
            X, XT = xtm[bi % 2], xT[bi % 2]
            r0 = e * C_CAP + rb * RB
            self.dma("sp", X.ap, Xg[r0:r0 + RB, :].rearrange("(j p) d -> p j d", p=128), [self.ddep("Xg")], [X.d()])
            for j in range(RB // 128):
                pb = self.npsb()
                for kt in range(8):
                    self.tr(pb.ap[:, kt * 128:(kt + 1) * 128], X.ap[:, j, kt * 128:(kt + 1) * 128], self.ident_b.ap,
                            [X.d(), self.ident_b.d()], [pb.d()])
                self.cp(self.ev(), XT.ap[:, :, j * 128:(j + 1) * 128], pb.ap.rearrange("p (k q) -> p k q", k=8), [pb.d()], [XT.d()])

        def stGU(bi):
            e, rb = blocks[bi]
            Wg, Wu = wg[e % 2], wu[e % 2]
            XT, HT = xT[bi % 2], hT[bi % 2]
            for j in range(8):
                pg, pu = self.nps(), self.nps()
                for kt in range(8):
                    self.mm(pg.ap[:, 0:RB], Wg.ap[:, kt, j * 128:(j + 1) * 128], XT.ap[:, kt, :], kt == 0, kt == 7, [Wg.d(), XT.d()], [pg.d()])
                for kt in range(8):
                    self.mm(pu.ap[:, 0:RB], Wu.ap[:, kt, j * 128:(j + 1) * 128], XT.ap[:, kt, :], kt == 0, kt == 7, [Wu.d(), XT.d()], [pu.d()])
                s_ = sg[j % 2]
                self.act(s_.ap, pg.ap[:, 0:RB], AF.Silu, [pg.d()], [s_.d()])
                self.tt("dve", HT.ap[:, j, :], s_.ap, pu.ap[:, 0:RB], ALU.mult, [s_.d(), pu.d()], [HT.d()])

        def stY(bi):
            e, rb = blocks[bi]
            Wd, HT = wd[e % 2], hT[bi % 2]
            r0 = e * C_CAP + rb * RB
            for r in range(RB // 128):
                ys = yst[r % 2]
                for c in range(2):
                    py = self.nps()
                    for j in range(8):
                        self.mm(py.ap, HT.ap[:, j, r * 128:(r + 1) * 128], Wd.ap[:, j, c * 512:(c + 1) * 512], j == 0, j == 7,
                                [HT.d(), Wd.d()], [py.d()])
                    self.cp(self.ev(), ys.ap[:, c * 512:(c + 1) * 512], py.ap, [py.d()], [ys.d()])
                rr = r0 + r * 128
                self.dma("sp", Yg[rr:rr + 128, :], ys.ap, [ys.d()], [self.ddep("Yg")])

        stT(0)
        for bi, (e, rb) in enumerate(blocks):
            if rb == 0 and e + 1 < N_EXP:
                load_w(e + 1)
            stGU(bi)
            if bi + 1 < len(blocks):
                stT(bi + 1)
            stY(bi)
        kb.barrier()
        AB.release(mb)
        AFa.release(mf)
        mb, mf = AB.mark(), AFa.mark()
        y1 = [AFa.alloc(D) for _ in range(2)]
        y2 = [AFa.alloc(D) for _ in range(2)]
        pw = self.post_work()
        di, gw, dg = self.dest_i, self.gatew, self.dest_g
        for it, t in enumerate(tiles):
            a, b = y1[it % 2], y2[it % 2]
            for k, dst in ((0, a), (1, b)):
                self.memset("pool", dst.ap, 0.0, [dst.d()])
                self.kb.dma("pool", lambda e, k=k, t=t, dst=dst: e.indirect_dma_start(
                    out=dst.ap, out_offset=None, in_=Yg,
                    in_offset=bass.IndirectOffsetOnAxis(ap=dg.ap[:, t, k:k + 1], axis=0)),
                    [dg.d(t), self.ddep("Yg")], [dst.d()])
            self.ts("dve", a.ap, a.ap, gw.ap[:, t, 0:1], None, ALU.mult, None, [a.d(), gw.d(t)], [a.d()])
            self.stt(a.ap, b.ap, gw.ap[:, t, 1:2], a.ap, ALU.mult, ALU.add, [a.d(), b.d(), gw.d(t)], [a.d()])
            self.post(t, [a.ap[:, 0:512], a.ap[:, 512:1024]], [a.d()], pw[it % 2])
        kb.barrier()
        AB.release(mb)
        AFa.release(mf)

    def s5(self, l, slot):
        kb = self.kb
        AB, AFa = self.AB, self.AF_
        kb.barrier()
        mb, mf = AB.mark(), AFa.mark()
        self.load_mod(l, 0)
        L = 32
        NCH = T_ALL // L
        chunks = [(0, 256)] + [(256 + i * 512, 512) for i in range(4)]
        yT = AB.alloc(8 * T_ALL, [8, T_ALL])
        uT = AB.alloc(8 * T_ALL, [8, T_ALL])
        mb_w = AB.mark()
        mf1 = AFa.mark()
        hxs = [AFa.alloc(D) for _ in range(2)]
        for t in range(NT):
            hx = hxs[t % 2]
            self.modulate(t, hx)
            self.transpose_tile(hx, yT, t, t)
        win = AB.alloc(8 * D, [8, D])
        w_in = self.inp["s5_w_in"][slot].rearrange("(kt p) n -> p kt n", p=128)
        for hh in range(2):
            self.dma("pool", win.ap[:, hh * 4:(hh + 1) * 4, :], w_in[:, hh * 4:(hh + 1) * 4, :], [], [win.d()])
        for ft in range(8):
            for ci, (c0, cn) in enumerate(chunks):
                ps = self.nps()
                hdeps = yT.dl(range(c0 // 128, (c0 + cn) // 128))
                for kt in range(8):
                    self.mm(ps.ap[:, 0:cn], win.ap[:, kt, ft * 128:(ft + 1) * 128], yT.ap[:, kt, c0:c0 + cn], kt == 0, kt == 7, [win.d()] + hdeps, [ps.d()])
                self.cp(self.ev(), uT.ap[:, ft, c0:c0 + cn], ps.ap[:, 0:cn], [ps.d()], [uT.d()])
        kb.barrier()
        AB.release(mb_w)
        AFa.release(mf1)
        dsk = AFa.alloc(8)
        self.dma("sp", dsk.ap, self.inp["s5_d"][slot:slot + 1, :].rearrange("o (f p) -> p (o f)", p=128), [], [dsk.d()], slow=True)
        yd = [yT.d("y")]
        for ft in range(8):
            self.ts("dve", yT.ap[:, ft, :], uT.ap[:, ft, :], dsk.ap[:, ft:ft + 1], None, ALU.mult, None, [uT.d(), dsk.d()], yd)
        tau = AFa.alloc(32)
        self.dma("sp", tau.ap, self.inp["k_tau"], [], [tau.d()])
        Bw = [AB.alloc(32 * 128, [32, 128]) for _ in range(2)]
        Cw = [AB.alloc(32 * 128, [32, 128]) for _ in range(2)]
        hb_ = [[AB.alloc(8 * L, [8, L]) for _ in range(2)] for _ in range(2)]
        mf2 = AFa.mark()
        for d in range(2):
            kb.barrier()
            AFa.release(mf2)
            stg = AFa.alloc(32 * 128, [32, 128])
            for ri, nm in enumerate(("s5_b_re", "s5_b_im")):
                self.memset("pool", stg.ap, 0.0, [stg.d()])
                for g in range(64):
                    dst = stg.ap[(g % 8) * 16:(g % 8) * 16 + 16, g // 2, (g % 2) * 64:(g % 2) * 64 + 64]
                    self.dma("sp", dst, self.inp[nm][slot, d, g].rearrange("p n -> n p"), [], [stg.d()], slow=True)
                self.cp("act", Bw[ri].ap, stg.ap, [stg.d()], [Bw[ri].d()])
            for ri, nm in enumerate(("s5_c_re", "s5_c_im")):
                self.memset("pool", stg.ap, 0.0, [stg.d()])
                for g in range(64):
                    dst = stg.ap[(g % 2) * 64:(g % 2) * 64 + 64, g // 2, (g % 8) * 16:(g % 8) * 16 + 16]
                    self.dma("sp", dst, self.inp[nm][slot, d, g].rearrange("n p -> p n"), [], [stg.d()], slow=True)
                if ri == 0:
                    self.cp("act", Cw[0].ap, stg.ap, [stg.d()], [Cw[0].d()])
                else:
                    self.ts("dve", Cw[1].ap, stg.ap, -1.0, None, ALU.mult, None, [stg.d()], [Cw[1].d()])
            kb.barrier()
            AFa.release(mf2)
            P = AFa.alloc(16 * 32, [16, 32])
            pd = [P.d()]
            ar, ai, dt, mag, ang, cs, sn, zr, zi, den, kr, ki, t0, t1, rc_r, rc_i = (P.ap[:, i, :] for i in range(16))
            for two in range(2):
                prt = slice(two * 64, two * 64 + 64)
                self.dma("sp", P.ap[prt, 0, :], self.inp["s5_a_re"][slot, d].rearrange("(s two) p -> two p s", two=2)[two], [], pd, slow=True)
                self.dma("sp", P.ap[prt, 1, :], self.inp["s5_a_im"][slot, d].rearrange("(s two) p -> two p s", two=2)[two], [], pd, slow=True)
                self.dma("sp", P.ap[prt, 2, :], self.inp["s5_log_dt"][slot, d:d + 1, :].rearrange("o (s two) -> two o s", two=2)[two].partition_broadcast(64), [], pd, slow=True)
            self.act(dt, dt, AF.Exp, pd, pd)
            self.tt("dve", mag, ar, dt, ALU.mult, pd, pd)
            self.act(mag, mag, AF.Exp, pd, pd)
            self.tt("dve", ang, ai, dt, ALU.mult, pd, pd)
            it_ = self.itmp
            self.sincos(ang, cs, sn, t0, it_.ap[:, 0:32], pd + [it_.d()], pd + [it_.d()])
            self.tt("dve", zr, mag, cs, ALU.mult, pd, pd)
            self.tt("dve", zi, mag, sn, ALU.mult, pd, pd)
            self.tt("dve", den, ar, ar, ALU.mult, pd, pd)
            self.tt("dve", t0, ai, ai, ALU.mult, pd, pd)
            self.tt("dve", den, den, t0, ALU.add, pd, pd)
            self.recip(den, den, pd, pd)
            self.ts("dve", zr, zr, -1.0, None, ALU.add, None, pd, pd)
            self.tt("dve", kr, zr, ar, ALU.mult, pd, pd)
            self.tt("dve", t0, zi, ai, ALU.mult, pd, pd)
            self.tt("dve", kr, kr, t0, ALU.add, pd, pd)
            self.tt("dve", kr, kr, den, ALU.mult, pd, pd)
            self.tt("dve", ki, zi, ar, ALU.mult, pd, pd)
            self.tt("dve", t0, zr, ai, ALU.mult, pd, pd)
            self.tt("dve", ki, ki, t0, ALU.subtract, pd, pd)
            self.tt("dve", ki, ki, den, ALU.mult, pd, pd)
            NTB = 32 * L
            Ct, St, T1r, T1i, rz, tmp = (AFa.alloc(NTB) for _ in range(6))
            td = [Ct.d()]
            v3 = lambda tl: tl.ap.rearrange("p (s l) -> p s l", s=32)
            bc_s = lambda a2: a2.unsqueeze(2).to_broadcast([128, 32, L])
            self.tt("dve", v3(tmp), bc_s(ang), tau.ap.unsqueeze(1).to_broadcast([128, 32, L]), ALU.mult, pd + [tau.d()], td)
            self.sincos(tmp.ap, Ct.ap, St.ap, T1r.ap, it_.ap[:, 0:NTB], td + [it_.d()], td + [it_.d()])
            self.tt("dve", v3(T1r), v3(Ct), bc_s(kr), ALU.mult, td + pd, td)
            self.tt("dve", v3(tmp), v3(St), bc_s(ki), ALU.mult, td + pd, td)
            self.tt("dve", T1r.ap, T1r.ap, tmp.ap, ALU.add, td, td)
            self.tt("dve", v3(T1i), v3(Ct), bc_s(ki), ALU.mult, td + pd, td)
            self.tt("dve", v3(tmp), v3(St), bc_s(kr), ALU.mult, td + pd, td)
            self.tt("dve", T1i.ap, T1i.ap, tmp.ap, ALU.subtract, td, td)
            self.cp("dve", v3(rz), bc_s(mag), td + pd, td)
            self.memset("dve", v3(rz)[:, :, 0], 0.0, td)
            self.memset("dve", rc_r, 0.0, pd)
            self.memset("dve", rc_i, 0.0, pd)
            wk = [AFa.alloc(8 * L, [8, L]) for _ in range(6)]
            gbuf = [[AFa.alloc(8 * L, [8, L]) for _ in range(2)] for _ in range(2)]
            f2 = lambda tl: tl.ap.rearrange("p s l -> p (s l)")

            def mk_tok(lo_, rev):
                def tok(ap2):
                    v = ap2[:, lo_:lo_ + L]
                    return v[:, ::-1] if rev else v
                return tok

            units = []
            for c in range(NCH):
                if d == 0:
                    lo = c * L
                elif c < T_CTX // L:
                    lo = T_CTX - (c + 1) * L
                else:
                    lo = T_ALL - (c - T_CTX // L + 1) * L
                for fp in range(4):
                    units.append(dict(c=c, fp=fp, tok=mk_tok(lo, d == 1), idx=c * 4 + fp))

            def stA(u):
                fp, tok = u["fp"], u["tok"]
                pbu = self.nps()
                u["pbu"] = pbu
                for s8 in range(8):
                    s_ = 8 * fp + s8
                    rhs = tok(uT.ap[:, s_ // 4, :])
                    self.mm(pbu.ap[:, s8 * L:(s8 + 1) * L], Bw[0].ap[:, s_, :], rhs, True, True, [Bw[0].d(), uT.d()], [pbu.d()])
                    self.mm(pbu.ap[:, 256 + s8 * L:256 + (s8 + 1) * L], Bw[1].ap[:, s_, :], rhs, True, True, [Bw[1].d(), uT.d()], [pbu.d()])

            def stB(u):
                fp, c, pbu = u["fp"], u["c"], u["pbu"]
                m1, m2, br, bi, m3, m4 = wk
                gr, gi = gbuf[u["idx"] % 2]
                u["g"] = (gr, gi)
                ss = slice(8 * fp, 8 * fp + 8)
                fl = slice(8 * fp * L, (8 * fp + 8) * L)
                bur = pbu.ap[:, 0:256].rearrange("p (s l) -> p s l", s=8)
                bui = pbu.ap[:, 256:512].rearrange("p (s l) -> p s l", s=8)
                t1r, t1i = v3(T1r)[:, ss, :], v3(T1i)[:, ss, :]
                self.tt("dve", m1.ap, bur, t1r, ALU.mult, [pbu.d()] + td, [m1.d()])
                self.tt("dve", m2.ap, bui, t1i, ALU.mult, [pbu.d()] + td, [m2.d()])
                self.tt("dve", br.ap, m1.ap, m2.ap, ALU.subtract, [m1.d(), m2.d()], [br.d()])
                self.tt("dve", m1.ap, bui, t1r, ALU.mult, [pbu.d()] + td, [m1.d()])
                self.tt("dve", m2.ap, bur, t1i, ALU.mult, [pbu.d()] + td, [m2.d()])
                self.tt("dve", bi.ap, m1.ap, m2.ap, ALU.add, [m1.d(), m2.d()], [bi.d()])
                if c > 0:
                    self.tt("dve", br.ap[:, :, 0], br.ap[:, :, 0], rc_r[:, ss], ALU.add, [br.d(), rcd[fp]], [br.d()])
                    self.tt("dve", bi.ap[:, :, 0], bi.ap[:, :, 0], rc_i[:, ss], ALU.add, [bi.d(), rcd[fp]], [bi.d()])
                self.scan(f2(gr), rz.ap[:, fl], f2(br), 0.0, [br.d()] + td, [gr.d()])
                self.scan(f2(gi), rz.ap[:, fl], f2(bi), 0.0, [bi.d()] + td, [gi.d()])

            def stC(u):
                fp = u["fp"]
                m1, m2, br, bi, m3, m4 = wk
                gr, gi = u["g"]
                hrb, hib = hb_[u["idx"] % 2]
                u["h"] = (hrb, hib)
                ss = slice(8 * fp, 8 * fp + 8)
                ct, st = v3(Ct)[:, ss, :], v3(St)[:, ss, :]
                gd = [gr.d(), gi.d()]
                self.tt("pool", m3.ap, gr.ap, ct, ALU.mult, gd + td, [m3.d()])
                self.tt("pool", m4.ap, gi.ap, st, ALU.mult, gd + td, [m4.d()])
                self.tt("pool", hrb.ap, m3.ap, m4.ap, ALU.subtract, [m3.d(), m4.d()], [hrb.d()])
                self.tt("pool", m3.ap, gr.ap, st, ALU.mult, gd + td, [m3.d()])
                self.tt("pool", m4.ap, gi.ap, ct, ALU.mult, gd + td, [m4.d()])
                self.tt("pool", hib.ap, m3.ap, m4.ap, ALU.add, [m3.d(), m4.d()], [hib.d()])

            def stD(u):
                fp = u["fp"]
                hrb, hib = u["h"]
                ss = slice(8 * fp, 8 * fp + 8)
                self.tt("dve", rc_r[:, ss], hrb.ap[:, :, L - 1], mag[:, ss], ALU.mult, [hrb.d()] + pd, [rcd[fp]])
                self.tt("dve", rc_i[:, ss], hib.ap[:, :, L - 1], mag[:, ss], ALU.mult, [hib.d()] + pd, [rcd[fp]])

            def stE(u):
                fp, tok = u["fp"], u["tok"]
                hrb, hib = u["h"]
                po = self.nps()
                for q in range(2):
                    ft = 2 * fp + q
                    for s4 in range(4):
                        s_ = 4 * ft + s4
                        s8 = q * 4 + s4
                        self.mm(po.ap[:, q * L:(q + 1) * L], Cw[0].ap[:, s_, :], hrb.ap[:, s8, :], s4 == 0, False, [Cw[0].d(), hrb.d()], [po.d()])
                        self.mm(po.ap[:, q * L:(q + 1) * L], Cw[1].ap[:, s_, :], hib.ap[:, s8, :], False, s4 == 3, [Cw[1].d(), hib.d()], [po.d()])
                for q in range(2):
                    ft = 2 * fp + q
                    ysl = tok(yT.ap[:, ft, :])
                    self.tt("dve", ysl, ysl, po.ap[:, q * L:(q + 1) * L], ALU.add, [po.d()] + yd, yd)

            rcd = [Dep() for _ in range(4)]
            NU = len(units)
            for i in range(NU + 2):
                if i < NU:
                    stA(units[i])
                    stB(units[i])
                if i >= 2:
                    stE(units[i - 2])
                if i < NU:
                    stC(units[i])
                if 1 <= i <= NU:
                    stD(units[i - 1])
        kb.barrier()
        AB.release(mb_w)
        AFa.release(mf1)
        gw = [(AFa.alloc(512), AFa.alloc(512)) for _ in range(2)]
        i = 0
        for ft in range(8):
            for (c0, cn) in chunks:
                a_, b_ = gw[i % 2]
                i += 1
                self.gelu_tanh(yT.ap[:, ft, c0:c0 + cn], yT.ap[:, ft, c0:c0 + cn], yd, ((a_, a_.ap[:, 0:cn]), (b_, b_.ap[:, 0:cn])), yd)
        wv = AB.alloc(8 * D, [8, D])
        wg = AB.alloc(8 * D, [8, D])
        for W, nm in ((wv, "s5_glu_v"), (wg, "s5_glu_g")):
            src = self.inp[nm][slot].rearrange("(kt p) n -> p kt n", p=128)
            for hh in range(2):
                self.dma("pool", W.ap[:, hh * 4:(hh + 1) * 4, :], src[:, hh * 4:(hh + 1) * 4, :], [], [W.d()])
        ys = [AFa.alloc(D) for _ in range(2)]
        sg = [AFa.alloc(512) for _ in range(2)]
        pw = self.post_work()
        for t in range(NT):
            yt = ys[t % 2]
            for c in range(2):
                pv, pg = self.nps(), self.nps()
                for kt in range(8):
                    self.mm(pv.ap, yT.ap[:, kt, t * 128:(t + 1) * 128], wv.ap[:, kt, c * 512:(c + 1) * 512], kt == 0, kt == 7, yd + [wv.d()], [pv.d()])
                for kt in range(8):
                    self.mm(pg.ap, yT.ap[:, kt, t * 128:(t + 1) * 128], wg.ap[:, kt, c * 512:(c + 1) * 512], kt == 0, kt == 7, yd + [wg.d()], [pg.d()])
                s_ = sg[c]
                self.act(s_.ap, pg.ap, AF.Sigmoid, [pg.d()], [s_.d()])
                self.tt("dve", yt.ap[:, c * 512:(c + 1) * 512], pv.ap, s_.ap, ALU.mult, [pv.d(), s_.d()], [yt.d()])
            self.post(t, [yt.ap[:, 0:512], yt.ap[:, 512:1024]], [yt.d()], pw[t % 2])
        kb.barrier()
        AB.release(mb)
        AFa.release(mf)

    def rglru(self, l, slot):
        kb = self.kb
        AB, AFa = self.AB, self.AF_
        gT = self.dr["gT"]
        kb.barrier()
        mb, mf = AB.mark(), AFa.mark()
        self.load_mod(l, 0)
        NJ = 11
        w_in = self.inp["lru_w_in"][slot].rearrange("(kt p) n -> p kt n", p=128)
        chunks = [(0, 256)] + [(256 + i * 512, 512) for i in range(4)]
        xsT = AB.alloc(12 * T_ALL, [12, T_ALL])
        mb_h = AB.mark()
        hT = AB.alloc(8 * T_ALL, [8, T_ALL])
        convw = AFa.alloc(NJ * 4, [NJ, 4])
        convb = AFa.alloc(NJ)
        ba = AFa.alloc(2 * NJ, [2, NJ])
        bx = AFa.alloc(2 * NJ, [2, NJ])
        c8 = AFa.alloc(2 * NJ, [2, NJ])
        for k in range(4):
            self.dma("sp", convw.ap[:, :, k], self.inp["lru_conv_w"][slot, k:k + 1, :].rearrange("o (j p) -> p (o j)", p=128), [], [convw.d()], slow=True)
        self.dma("sp", convb.ap, self.inp["lru_conv_b"][slot:slot + 1, :].rearrange("o (j p) -> p (o j)", p=128), [], [convb.d()], slow=True)
        for tl, nm in ((ba, "lru_b_a"), (bx, "lru_b_x"), (c8, "lru_lam")):
            for d in range(2):
                self.dma("sp", tl.ap[:, d, :], self.inp[nm][slot, d:d + 1, :].rearrange("o (j p) -> p (o j)", p=128), [], [tl.d()], slow=True)
        self.act(c8.ap, c8.ap, AF.Exp, [c8.d()], [c8.d()], scale=-1.0)
        self.act(c8.ap, c8.ap, AF.Ln, [c8.d()], [c8.d()], bias=1.0)
        self.ts("dve", c8.ap, c8.ap, -8.0, None, ALU.mult, None, [c8.d()], [c8.d()])
        mf1 = AFa.mark()
        hxs = [AFa.alloc(D) for _ in range(2)]
        for t in range(NT):
            hx = hxs[t % 2]
            self.modulate(t, hx)
            self.transpose_tile(hx, hT, t, t)
        kb.barrier()
        AFa.release(mf1)
        raw = AFa.alloc(T_ALL)
        acc = AFa.alloc(T_ALL)
        gw = [(AFa.alloc(512), AFa.alloc(512)) for _ in range(2)]
        wt = [AB.alloc(8 * 128, [8, 128]) for _ in range(2)]
        gtile = [AB.alloc(T_ALL) for _ in range(2)]
        for o in range(2 * NJ):
            W = wt[o % 2]
            cols = o * 128 if o < NJ else LRU_W + (o - NJ) * 128
            self.dma("pool", W.ap, w_in[:, :, cols:cols + 128], [], [W.d()])
            gt = gtile[o % 2]
            for ci, (c0, cn) in enumerate(chunks):
                ps = self.nps()
                hdeps = hT.dl(range(c0 // 128, (c0 + cn) // 128))
                for kt in range(8):
                    self.mm(ps.ap[:, 0:cn], W.ap[:, kt, :], hT.ap[:, kt, c0:c0 + cn], kt == 0, kt == 7, [W.d()] + hdeps, [ps.d()])
                if o < NJ:
                    a_, b_ = gw[ci % 2]
                    self.gelu_tanh(gt.ap[:, c0:c0 + cn], ps.ap[:, 0:cn], [ps.d()], ((a_, a_.ap[:, 0:cn]), (b_, b_.ap[:, 0:cn])), [gt.d()])
                else:
                    self.cp(self.ev(), raw.ap[:, c0:c0 + cn], ps.ap[:, 0:cn], [ps.d()], [raw.d()])
            if o < NJ:
                self.dma("sp", gT[o], gt.ap, [gt.d()], [self.ddep("gT", o)])
            else:
                j = o - NJ
                rd_, ad_ = [raw.d()], [acc.d()]
                self.ts("dve", acc.ap, raw.ap, convw.ap[:, j, 1:2], convb.ap[:, j:j + 1], ALU.mult, ALU.add, rd_ + [convw.d(), convb.d()], ad_)
                for (s0, s1) in ((0, T_CTX), (T_CTX, T_ALL)):
                    self.stt(acc.ap[:, s0 + 1:s1], raw.ap[:, s0:s1 - 1], convw.ap[:, j, 0:1], acc.ap[:, s0 + 1:s1], ALU.mult, ALU.add, rd_ + ad_, ad_)
                    self.stt(acc.ap[:, s0:s1 - 1], raw.ap[:, s0 + 1:s1], convw.ap[:, j, 2:3], acc.ap[:, s0:s1 - 1], ALU.mult, ALU.add, rd_ + ad_, ad_)
                    self.stt(acc.ap[:, s0:s1 - 2], raw.ap[:, s0 + 2:s1], convw.ap[:, j, 3:4], acc.ap[:, s0:s1 - 2], ALU.mult, ALU.add, rd_ + ad_, ad_)
                self.cp("act", xsT.ap[:, j + 1, :], acc.ap, ad_, [xsT.d(j + 1)])
        kb.barrier()
        AB.release(mb_h)
        AFa.release(mf1)
        bands = {}
        for nm in ("lru_w_a", "lru_w_x"):
            for d in range(2):
                bt = AB.alloc(NJ * 3 * 128, [NJ, 3, 128])
                bands[(nm, d)] = bt
                self.memset("dve", bt.ap, 0.0, [bt.d()])
                wsrc = self.inp[nm]
                for n in range(16):
                    r0, r1 = 88 * n, 88 * n + 88
                    tl_ = list(range(r0 // 128, (r1 - 1) // 128 + 1))
                    for kt in tl_:
                        ra, rb = max(r0, kt * 128), min(r1, (kt + 1) * 128)
                        for j in tl_:
                            ca, cb = max(r0, j * 128), min(r1, (j + 1) * 128)
                            dst = bt.ap[ra - kt * 128:rb - kt * 128, j, kt - j + 1, ca - j * 128:cb - j * 128]
                            src = wsrc[slot, d, n, ra - r0:rb - r0, ca - r0:cb - r0]
                            self.dma("pool", dst, src, [], [bt.d()])
        gtile = [AB.alloc(T_ALL) for _ in range(2)]
        h0 = AFa.alloc(T_ALL)
        h1 = AFa.alloc(T_ALL)
        wk = [[AFa.alloc(512) for _ in range(4)] for _ in range(2)]
        lat_rev = [(256 + i * 512, 512) for i in (3, 2, 1, 0)]
        it = 0
        for j in range(NJ):
            gt = gtile[j % 2]
            self.dma("sp", gt.ap, gT[j], [self.ddep("gT", j)], [gt.d()])
            nb = [kt for kt in (j - 1, j, j + 1) if 0 <= kt < NJ]
            for d in range(2):
                hb = h0 if d == 0 else h1
                order = chunks if d == 0 else [(0, 256)] + lat_rev
                Ba, Bx = bands[("lru_w_a", d)], bands[("lru_w_x", d)]
                for (c0, cn) in order:
                    R, I, A, S = wk[it % 2]
                    it += 1
                    pa, px = self.nps(), self.nps()
                    xdeps = [xsT.d(kt + 1) for kt in nb]
                    for idx, kt in enumerate(nb):
                        self.mm(pa.ap[:, 0:cn], Ba.ap[:, j, kt - j + 1, :], xsT.ap[:, kt + 1, c0:c0 + cn], idx == 0, idx == len(nb) - 1, [Ba.d()] + xdeps, [pa.d()])
                    for idx, kt in enumerate(nb):
                        self.mm(px.ap[:, 0:cn], Bx.ap[:, j, kt - j + 1, :], xsT.ap[:, kt + 1, c0:c0 + cn], idx == 0, idx == len(nb) - 1, [Bx.d()] + xdeps, [px.d()])
                    Ra, Ia, Aa, Sa = R.ap[:, 0:cn], I.ap[:, 0:cn], A.ap[:, 0:cn], S.ap[:, 0:cn]
                    self.act(Ra, pa.ap[:, 0:cn], AF.Sigmoid, [pa.d(), ba.d()], [R.d()], bias=ba.ap[:, d, j:j + 1])
                    self.act(Ia, px.ap[:, 0:cn], AF.Sigmoid, [px.d(), bx.d()], [I.d()], bias=bx.ap[:, d, j:j + 1])
                    self.act(Aa, Ra, AF.Exp, [R.d(), c8.d()], [A.d()], scale=c8.ap[:, d, j:j + 1])
                    self.tt("dve", Sa, Aa, Aa, ALU.mult, [A.d()], [S.d()])
                    self.act(Sa, Sa, AF.Sqrt, [S.d()], [S.d()], scale=-1.0, bias=1.0)
                    self.tt("dve", Ia, Ia, xsT.ap[:, j + 1, c0:c0 + cn], ALU.mult, [I.d(), xsT.d(j + 1)], [I.d()])
                    self.tt("pool", Ia, Ia, Sa, ALU.mult, [I.d(), S.d()], [I.d()])
                    if d == 0:
                        init = 0.0 if c0 == 0 else hb.ap[:, c0 - 1:c0]
                        self.scan(hb.ap[:, c0:c0 + cn], Aa, Ia, init, [A.d(), I.d(), hb.d()], [hb.d()])
                    else:
                        if c0 == 0:
                            init = 0.0
                        elif c0 + cn == T_ALL:
                            init = hb.ap[:, 0:1]
                        else:
                            init = hb.ap[:, c0 + cn:c0 + cn + 1]
                        self.scan(hb.ap[:, c0:c0 + cn][:, ::-1], Aa[:, ::-1], Ia[:, ::-1], init, [A.d(), I.d(), hb.d()], [hb.d()])
            self.tt("dve", h0.ap, h0.ap, h1.ap, ALU.add, [h0.d(), h1.d()], [h0.d()])
            self.tt("dve", xsT.ap[:, j, :], h0.ap, gt.ap, ALU.mult, [h0.d(), gt.d()], [xsT.d(j)])
        kb.barrier()
        AFa.release(mf1)
        w_out = self.inp["lru_w_out"][slot].rearrange("(kt p) n -> p kt n", p=128)
        wo = AB.alloc(NJ * D, [NJ, D])
        for (k0, k1) in ((0, 4), (4, 8), (8, 11)):
            self.dma("pool", wo.ap[:, k0:k1, :], w_out[:, k0:k1, :], [], [wo.d()])
        pw = self.post_work()
        zdeps = [xsT.d(j) for j in range(NJ)]
        for t in range(NT):
            pys = []
            for c in range(2):
                py = self.nps()
                for kt in range(NJ):
                    self.mm(py.ap, xsT.ap[:, kt, t * 128:(t + 1) * 128], wo.ap[:, kt, c * 512:(c + 1) * 512], kt == 0, kt == NJ - 1, zdeps + [wo.d()], [py.d()])
                pys.append(py)
            self.post(t, [pys[0].ap, pys[1].ap], [pys[0].d(), pys[1].d()], pw[t % 2])
        kb.barrier()
        AB.release(mb)
        AFa.release(mf)

    def attention(self, l, slot, lam_init, last):
        kb = self.kb
        AB, AFa = self.AB, self.AF_
        kb.barrier()
        mb, mf = AB.mark(), AFa.mark()
        self.load_mod(l, 0)
        w_in = self.inp["attn_w_in"][slot].rearrange("(kt p) n -> p kt n", p=128)
        w_inp = self.inp["attn_w_in_p"][slot].rearrange("(kt p) n -> p kt n", p=128)
        w_out = self.inp["attn_w_out"][slot].rearrange("(kt p) n -> p kt n", p=128)
        hT = AB.alloc(8 * T_ALL, [8, T_ALL])
        oT = AB.alloc(8 * T_ALL, [8, T_ALL])
        mf0 = AFa.mark()
        hxs = [AFa.alloc(D) for _ in range(2)]
        for t in range(NT):
            hx = hxs[t % 2]
            self.modulate(t, hx)
            self.transpose_tile(hx, hT, t, t)
        kb.barrier()
        AFa.release(mf0)
        cosT = AFa.alloc(T_ALL)
        sinT = AFa.alloc(T_ALL)
        self.dma("sp", cosT.ap, self.inp["k_cos"], [], [cosT.d()])
        self.dma("sp", sinT.ap, self.inp["k_sin"], [], [sinT.d()])
        lamt = AFa.alloc(256 + 16)
        L = lamt.ap
        ld = [lamt.d()]
        self.dma("sp", L[:, 0:256], self.inp["attn_lam"][slot:slot + 1, :].partition_broadcast(128), [], ld)
        self.tt("dve", L[:, 0:64], L[:, 0:64], L[:, 64:128], ALU.mult, ld, ld)
        self.tt("dve", L[:, 128:192], L[:, 128:192], L[:, 192:256], ALU.mult, ld, ld)
        self.red("sum", L[:, 256:257], L[:, 0:64], ld, ld)
        self.red("sum", L[:, 257:258], L[:, 128:192], ld, ld)
        self.act(L[:, 258:260], L[:, 256:258], AF.Exp, ld, ld)
        self.tt("dve", L[:, 260:261], L[:, 258:259], L[:, 259:260], ALU.subtract, ld, ld)
        self.ts("dve", L[:, 261:262], L[:, 260:261], float(lam_init), -1.0, ALU.add, ALU.mult, ld, ld)
        neglam = L[:, 261:262]
        gsub = AFa.alloc(16)
        self.dma("sp", gsub.ap[:, 0:1], self.inp["attn_subln"][slot:slot + 1, :].rearrange("o e -> e o"), [], [gsub.d()], slow=True)
        self.ts("dve", gsub.ap[:, 0:1], gsub.ap[:, 0:1], float(1.0 - lam_init), None, ALU.mult, None, [gsub.d()], [gsub.d()])
        chunks = [(0, 256)] + [(256 + i * 512, 512) for i in range(4)]
        qk = [AB.alloc(T_ALL) for _ in range(4)]
        vext = AB.alloc(NT * 2 * 130, [NT, 2, 130])
        mbw = AB.mark()
        wqk = AB.alloc(8 * 512, [8, 4, 128])
        wqkp = AB.alloc(8 * 512, [8, 4, 128])
        wv = AB.alloc(8 * 256, [8, 256])
        ework = [AB.alloc(512) for _ in range(3)]
        ta = [AFa.alloc(512) for _ in range(2)]
        tb = [AFa.alloc(512) for _ in range(2)]
        fw = AFa.alloc(5 * 512)
        qchunks = chunks[1:] if last else chunks
        self.ps_n = 3
        self.memset("dve", vext.ap, 1.0, [vext.d()])
        qtiles = list(range(2, NT)) if last else list(range(NT))
        it_e = 0
        it_f = 0
        for hp in range(4):
            for j in range(4):
                m, isk = j % 2, j // 2
                c0 = isk * 1024 + m * 512 + hp * 128
                self.dma("pool", wqk.ap[:, :, j, :], w_in[:, :, c0:c0 + 128], [], [wqk.d()])
                self.dma("pool", wqkp.ap[:, :, j, :], w_inp[:, :, c0:c0 + 128], [], [wqkp.d()])
            self.dma("pool", wv.ap, w_in[:, :, 2048 + hp * 256:2048 + (hp + 1) * 256], [], [wv.d()])
            for j in range(4):
                for ci, (c0, cn) in enumerate(chunks):
                    pa, pb_ = self.nps(), self.nps()
                    hdeps = hT.dl(range(c0 // 128, (c0 + cn) // 128))
                    for kt in range(8):
                        self.mm(pa.ap[:, 0:cn], wqk.ap[:, kt, j, :], hT.ap[:, kt, c0:c0 + cn], kt == 0, kt == 7, [wqk.d()] + hdeps, [pa.d()])
                    for kt in range(8):
                        self.mm(pb_.ap[:, 0:cn], wqkp.ap[:, kt, j, :], hT.ap[:, kt, c0:c0 + cn], kt == 0, kt == 7, [wqkp.d()] + hdeps, [pb_.d()])
                    a_, b_ = ta[ci % 2], tb[ci % 2]
                    self.tt("dve", a_.ap[:, 0:cn], pa.ap[:, 0:cn], cosT.ap[:, c0:c0 + cn], ALU.mult, [pa.d(), cosT.d()], [a_.d()])
                    self.tt("dve", b_.ap[:, 0:cn], pb_.ap[:, 0:cn], sinT.ap[:, c0:c0 + cn], ALU.mult, [pb_.d(), sinT.d()], [b_.d()])
                    self.tt("pool", qk[j].ap[:, c0:c0 + cn], a_.ap[:, 0:cn], b_.ap[:, 0:cn], ALU.add, [a_.d(), b_.d()], [qk[j].d(ci)])
            for t in range(NT):
                pv = self.nps()
                for kt in range(8):
                    self.mm(pv.ap[:, 0:256], hT.ap[:, kt, t * 128:(t + 1) * 128], wv.ap[:, kt, :], kt == 0, kt == 7, [hT.d(t), wv.d()], [pv.d()])
                self.cp(self.ev(), vext.ap[:, t, :, 0:128], pv.ap[:, 0:256].rearrange("p (h e) -> p h e", h=2), [pv.d()], [vext.d()])
            qkd = [qk[j].dl(range(5)) for j in range(4)]
            items = []
            for hh in range(2):
                for (c0, cn) in qchunks:
                    kts = [0, 1] if c0 == 0 else list(range(NT))
                    for m in range(2):
                        for i, kt in enumerate(kts):
                            items.append((hh, c0, cn, m, kt, i == 0, i == len(kts) - 1, m == 1 and i == len(kts) - 1))

            def finalize(hh, c0, cn):
                head = hp * 2 + hh
                n0, d0, n1, d1 = self.ps[3], self.ps[4], self.ps[5], self.ps[6]
                wdp = [fw.d()]
                r1, r2, t1, o, sqv = (fw.ap[:, i * 512:i * 512 + cn] for i in range(5))
                self.recip(r1, d0.ap[:, 0:cn], [d0.d()], wdp)
                self.recip(r2, d1.ap[:, 0:cn], [d1.d()], wdp)
                self.ts("dve", r2, r2, neglam, None, ALU.mult, None, wdp + ld, wdp)
                self.tt("dve", t1, n0.ap[:, 0:cn], r1, ALU.mult, [n0.d()] + wdp, wdp)
                self.tt("dve", r2, n1.ap[:, 0:cn], r2, ALU.mult, [n1.d()] + wdp, wdp)
                self.tt("dve", o, r2, t1, ALU.add, wdp, wdp)
                self.tt("pool", sqv, o, o, ALU.mult, wdp, wdp)
                pss = self.nps()
                self.mm(pss.ap[:, 0:cn], self.ones_f.ap, sqv, True, True, wdp + [self.ones_f.d()], [pss.d()])
                self.ts("dve", r1, pss.ap[:, 0:cn], 1.0 / 128.0, float(LN_EPS), ALU.mult, ALU.add, [pss.d()] + wdp, wdp)
                self.act(r1, r1, AF.Sqrt, wdp, wdp)
                self.recip(r1, r1, wdp, wdp)
                self.stt(oT.ap[:, head, c0:c0 + cn], o, gsub.ap[:, 0:1], r1, ALU.mult, ALU.mult, wdp + [gsub.d()],
                         [oT.d(t) for t in range(c0 // 128, (c0 + cn) // 128)])

            def do_pv(item, E):
                hh, c0, cn, m, kt, first, lastk, unit_end = item
                num, den = self.ps[3 + 2 * m], self.ps[4 + 2 * m]
                self.mm(num.ap[:, 0:cn], vext.ap[:, kt, hh, 0:128], E.ap[:, 0:cn], first, lastk, [E.d(), vext.d()], [num.d()])
                self.mm(den.ap[:, 0:cn], self.ones_b.ap, E.ap[:, 0:cn], first, lastk, [E.d(), self.ones_b.d()], [den.d()])
                if unit_end:
                    finalize(hh, c0, cn)

            pend = []
            for item in items:
                hh, c0, cn, m, kt = item[0:5]
                prow = slice(hh * 64, (hh + 1) * 64)
                Q, K = qk[m], qk[2 + m]
                S = self.nps()
                self.mm(S.ap[:, 0:cn], K.ap[prow, kt * 128:(kt + 1) * 128], Q.ap[prow, c0:c0 + cn], True, True,
                        qkd[m] + qkd[2 + m], [S.d()])
                E = ework[it_e % 3]
                it_e += 1
                self.act(E.ap[:, 0:cn], S.ap[:, 0:cn], AF.Exp, [S.d()], [E.d()], scale=0.125)
                pend.append((item, E))
                if len(pend) > 2:
                    do_pv(*pend.pop(0))
            while pend:
                do_pv(*pend.pop(0))
        kb.barrier()
        self.ps_n = 7
        AB.release(mbw)
        AFa.release(mf0)
        wo = AB.alloc(8 * D, [8, D])
        for hh in range(2):
            self.dma("pool", wo.ap[:, hh * 4:(hh + 1) * 4, :], w_out[:, hh * 4:(hh + 1) * 4, :], [], [wo.d()])
        pw = self.post_work()
        for it, t in enumerate(qtiles):
            pys = []
            for c in range(2):
                py = self.nps()
                for h in range(8):
                    self.mm(py.ap, oT.ap[:, h, t * 128:(t + 1) * 128], wo.ap[:, h, c * 512:(c + 1) * 512], h == 0, h == 7, [oT.d(t), wo.d()], [py.d()])
                pys.append(py)
            self.post(t, [pys[0].ap, pys[1].ap], [pys[0].d(), pys[1].d()], pw[it % 2])
        kb.barrier()
        AB.release(mb)
        AFa.release(mf)


def _consts():
    ident = np.eye(128, dtype=np.float32)
    ltri = (np.arange(128)[:, None] < np.arange(128)[None, :]).astype(np.float32)
    eoff = np.broadcast_to((np.arange(N_EXP) * C_CAP).astype(np.float32)[None, :], (128, N_EXP)).copy()
    half = 16
    freq = (10000.0 ** (-np.arange(half, dtype=np.float32) / half)).astype(np.float32)
    tpos = np.arange(T_LAT)
    row = (tpos // 64).astype(np.float32)
    col = (tpos % 64).astype(np.float32)
    cos = np.ones((128, T_ALL), np.float32)
    sin = np.zeros((128, T_ALL), np.float32)
    for p in range(128):
        d = p % 64
        pos = row if d < 32 else col
        dd = d % 32
        ang = (pos * freq[dd % 16]).astype(np.float32)
        cos[p, T_CTX:] = np.cos(ang)
        s = np.sin(ang)
        sin[p, T_CTX:] = -s if dd < 16 else s
    tau = np.broadcast_to(np.arange(1, 33, dtype=np.float32)[None, :], (128, 32)).copy()
    return dict(k_ident=ident, k_ltri=ltri, k_eoff=eoff, k_cos=cos, k_sin=sin, k_tau=tau)


def _perm_qk(w_in):
    qk = w_in[:, :, :2 * D]
    s = qk.shape
    v = qk.reshape(s[0], s[1], -1, 2, 16)
    return np.ascontiguousarray(v[:, :, :, ::-1, :].reshape(s))


_CACHE = {}


def _host_inputs(inputs):
    f = lambda a: np.ascontiguousarray(np.asarray(a, dtype=np.float32))
    shared = dict(
        w_ada=f(inputs["w_ada"]), b_ada=f(inputs["b_ada"]), ln_g=f(inputs["ln_g"]), ln_b=f(inputs["ln_b"]),
        attn_w_in=f(inputs["attn_w_in"]), attn_w_in_p=_perm_qk(f(inputs["attn_w_in"])), attn_w_out=f(inputs["attn_w_out"]),
        attn_lam=f(inputs["attn_lam"]).reshape(2, 256), attn_subln=f(inputs["attn_subln"]),
        router_w=f(inputs["router_w"]), router_b=f(inputs["router_b"]).reshape(1, N_EXP),
        moe_w_gate=f(inputs["moe_w_gate"]), moe_w_up=f(inputs["moe_w_up"]), moe_w_down=f(inputs["moe_w_down"]),
    )
    for nm in ("s5_w_in", "s5_a_re", "s5_a_im", "s5_b_re", "s5_b_im", "s5_c_re", "s5_c_im", "s5_log_dt", "s5_d", "s5_glu_v",
               "s5_glu_g", "lru_w_in", "lru_conv_w", "lru_conv_b", "lru_w_a", "lru_b_a", "lru_w_x", "lru_b_x", "lru_lam", "lru_w_out"):
        shared[nm] = f(inputs[nm])
    shared.update(_consts())
    return shared


def kernel(**inputs):
    shared = _host_inputs(inputs)
    x = np.asarray(inputs["x"], np.float32)
    ctx = np.asarray(inputs["ctx"], np.float32)
    c = np.asarray(inputs["c"], np.float32)
    c_ctx = np.asarray(inputs["c_ctx"], np.float32)
    prog = Prog()
    nc = prog.build()
    in_maps = []
    for b in range(NCORES):
        m = dict(shared)
        m["x"] = np.ascontiguousarray(x[b])
        m["ctx"] = np.ascontiguousarray(ctx[b])
        m["cc"] = np.ascontiguousarray(np.stack([c[b], c_ctx]))
        in_maps.append(m)
    res = run_bass_kernel_spmd(nc, in_maps, core_ids=list(range(NCORES)))
    return np.stack([np.asarray(r["out"], np.float32) for r in res.results])
```

```python
import contextlib
import math
import numpy as np
import concourse.bass as bass
import concourse.mybir as mybir
from concourse.bass_utils import run_bass_kernel_spmd

F32 = mybir.dt.float32
BF16 = mybir.dt.bfloat16
I32 = mybir.dt.int32
ALU = mybir.AluOpType
AF = mybir.ActivationFunctionType
AX = mybir.AxisListType

D = 1024
T_LAT = 2048
T_CTX = 256
T_ALL = T_LAT + T_CTX
NT = T_ALL // 128
DEPTH = 4
DN_ALPHA = (2 * DEPTH) ** 0.25
LN_EPS = 1e-5
N_EXP = 16
C_CAP = 1024
LRU_W = 1408
NCORES = 8


class Dep:
    __slots__ = ("w", "r")

    def __init__(self):
        self.w = None
        self.r = {}


class KB:
    EPOCH = 20000
    NPOOL = 8

    def __init__(self, nc):
        self.nc = nc
        self.stack = contextlib.ExitStack()
        self.ops = {e: [] for e in ("sp", "pool", "act", "dve", "pe")}
        self.cnt = {e: 0 for e in self.ops}
        self.esems = {e: [] for e in self.ops}
        self.known = {e: {} for e in self.ops}
        self.dma_sems = {}
        self.dma_cnt = {}
        self.dma_i = {e: 0 for e in self.ops}
        self.last_tok = {}
        self.nsem = 0
        self.ntens = 0
        self.out_tokens = []

    def sem(self, name):
        self.nsem += 1
        return self.stack.enter_context(self.nc.semaphore(f"{name}_{self.nsem}"))

    def sbuf(self, shape, dtype, name="t"):
        self.ntens += 1
        return self.stack.enter_context(self.nc.sbuf_tensor(f"{name}_{self.ntens}", list(shape), dtype))

    def psum(self, shape, dtype, name="ps"):
        self.ntens += 1
        return self.stack.enter_context(self.nc.psum_tensor(f"{name}_{self.ntens}", list(shape), dtype))

    def _need(self, eng, tok, waits):
        sem, val, _ = tok
        k = self.known[eng]
        key = id(sem)
        if k.get(key, (None, 0))[1] >= val:
            return
        k[key] = (sem, val)
        for i, (s, v) in enumerate(waits):
            if s is sem:
                waits[i] = (s, max(v, val))
                return
        waits.append((sem, val))

    def _collect(self, eng, reads, writes):
        waits = []
        for d in reads:
            if d.w is not None:
                if d.w[2] == eng and eng == "pe":
                    continue
                self._need(eng, d.w, waits)
        for d in writes:
            if d.w is not None and d.w[2] != eng:
                self._need(eng, d.w, waits)
            for t in d.r.values():
                if t[2] != eng:
                    self._need(eng, t, waits)
        return waits

    def _commit(self, tok, reads, writes):
        for d in reads:
            d.r[id(tok[0])] = tok
        for d in writes:
            d.w = tok
            d.r = {}
        self.last_tok[id(tok[0])] = tok

    def op(self, eng, fn, reads=(), writes=()):
        waits = self._collect(eng, reads, writes)
        c = self.cnt[eng]
        ep = c // self.EPOCH
        while len(self.esems[eng]) <= ep:
            self.esems[eng].append(self.sem(f"e_{eng}"))
        sem = self.esems[eng][ep]
        val = c % self.EPOCH + 1
        self.cnt[eng] = c + 1
        tok = (sem, val, eng)
        self.ops[eng].append((waits, fn, sem, 1))
        self._commit(tok, reads, writes)
        return tok

    def dma(self, q, fn, reads=(), writes=(), is_output=False):
        waits = self._collect(q, reads, writes)
        if q not in self.dma_sems:
            self.dma_sems[q] = [self.sem(f"d_{q}") for _ in range(self.NPOOL)]
            self.dma_cnt[q] = [0] * self.NPOOL
        i = self.dma_i[q] % self.NPOOL
        self.dma_i[q] += 1
        sem = self.dma_sems[q][i]
        prev = self.dma_cnt[q][i]
        if prev > 0:
            self._need(q, (sem, prev, "dma"), waits)
        self.dma_cnt[q][i] = prev + 16
        tok = (sem, prev + 16, "dma")
        self.ops[q].append((waits, fn, sem, 16))
        self._commit(tok, reads, writes)
        if is_output:
            self.out_tokens.append(tok)
        return tok

    def barrier(self):
        toks = list(self.last_tok.values())
        for eng in self.ops:
            waits = []
            for t in toks:
                self._need(eng, t, waits)
            if waits:
                self.ops[eng].append((waits, None, None, 0))

    def finish(self):
        waits = []
        for t in self.out_tokens:
            self._need("sp", t, waits)
        if waits:
            self.ops["sp"].append((waits, None, None, 0))
        nc = self.nc
        with nc.Block() as block:
            for name, deco in (("sp", block.sync), ("pool", block.gpsimd), ("act", block.scalar),
                               ("dve", block.vector), ("pe", block.tensor)):
                ops = self.ops[name]

                def body(e, ops=ops):
                    for waits, fn, sem, inc in ops:
                        for s, v in waits:
                            e.wait_ge(s, v)
                        if fn is None:
                            continue
                        ins = fn(e)
                        ins.then_inc(sem, inc)

                deco(body)
        self.stack.close()


class Tl:
    def __init__(self, ap):
        self.ap = ap
        self.ds = {}

    def d(self, key=0):
        if key not in self.ds:
            self.ds[key] = Dep()
        return self.ds[key]

    def dl(self, keys):
        return [self.d(k) for k in keys]


class Arena:
    def __init__(self, tens, n):
        self.t = tens
        self.n = n
        self.off = 0

    def alloc(self, n, shape=None):
        n2 = (n + 15) // 16 * 16
        assert self.off + n2 <= self.n, (self.off, n2, self.n)
        ap = self.t[:, self.off:self.off + n]
        self.off += n2
        if shape is not None:
            names = " ".join(f"a{i}" for i in range(len(shape)))
            kw = {f"a{i}": s for i, s in enumerate(shape)}
            ap = ap.rearrange(f"p ({names}) -> p {names}", **kw)
        return Tl(ap)

    def mark(self):
        return self.off

    def release(self, m):
        self.off = m


class Prog:
    def __init__(self, debug=None, layers=None):
        self.debug = debug or []
        self.layers = list(range(DEPTH)) if layers is None else list(layers)
        nc = bass.Bass("TRN2", target_bir_lowering=False)
        self.nc = nc
        self.kb = KB(nc)
        kb = self.kb
        self.inp = {}
        self.dr = {}
        self.dd = {}
        NB, NF = 64000, 18500
        self.AB = Arena(kb.sbuf([128, NB], BF16, "arb"), NB)
        self.AF_ = Arena(kb.sbuf([128, NF], F32, "arf"), NF)
        self.AI = Arena(kb.sbuf([128, 1280], I32, "ari"), 1280)
        self.ps = [Tl(kb.psum([128, 512], F32, f"ps{i}")[:]) for i in range(7)]
        self.psb = [Tl(kb.psum([128, 1024], BF16, f"psb{i}")[:]) for i in range(1)]
        self.ps_n = 7
        self.ps_i = 0
        self.psb_i = 0
        self.ev_i = 0

    def din(self, name, shape, dtype=F32):
        self.inp[name] = self.nc.dram_tensor(name, list(shape), dtype, kind="ExternalInput").ap()
        return self.inp[name]

    def dscr(self, name, shape, dtype=F32):
        kind = "ExternalOutput" if name in self.debug else "Internal"
        self.dr[name] = self.nc.dram_tensor(name, list(shape), dtype, kind=kind).ap()
        self.dd[name] = {}
        return self.dr[name]

    def ddep(self, name, key=0):
        dd = self.dd.setdefault(name, {})
        if key not in dd:
            dd[key] = Dep()
        return dd[key]

    def bound_reg(self, e):
        if getattr(self, "_breg", None) is None:
            self._breg = e.to_reg(N_EXP * C_CAP - 1)
        return self._breg

    def nps(self):
        p = self.ps[self.ps_i % self.ps_n]
        self.ps_i += 1
        return p

    def npsb(self):
        p = self.psb[self.psb_i % len(self.psb)]
        self.psb_i += 1
        return p

    def ev(self):
        self.ev_i += 1
        return "act" if self.ev_i % 2 else "dve"

    def tt(self, eng, out, in0, in1, op, r, w):
        self.kb.op(eng, lambda e: e.tensor_tensor(out=out, in0=in0, in1=in1, op=op), r, w)

    def ts(self, eng, out, in0, s1, s2, op0, op1, r, w):
        if s2 is None:
            self.kb.op(eng, lambda e: e.tensor_scalar(out=out, in0=in0, scalar1=s1, scalar2=None, op0=op0), r, w)
        else:
            self.kb.op(eng, lambda e: e.tensor_scalar(out=out, in0=in0, scalar1=s1, scalar2=s2, op0=op0, op1=op1), r, w)

    def stt(self, out, in0, sc, in1, op0, op1, r, w):
        self.kb.op("dve", lambda e: e.scalar_tensor_tensor(out=out, in0=in0, scalar=sc, in1=in1, op0=op0, op1=op1), r, w)

    def act(self, out, in_, func, r, w, bias=None, scale=None, accum=None):
        kw = {}
        if bias is not None:
            kw["bias"] = bias
        if scale is not None:
            kw["scale"] = scale
        if accum is not None:
            kw["accum_out"] = accum
        self.kb.op("act", lambda e: e.activation(out=out, in_=in_, func=func, **kw), r, w)

    def red(self, kind, out, in_, r, w):
        if kind == "max":
            self.kb.op("dve", lambda e: e.reduce_max(out=out, in_=in_, axis=AX.X), r, w)
        else:
            self.kb.op("dve", lambda e: e.reduce_sum(out=out, in_=in_, axis=AX.X), r, w)

    def recip(self, out, in_, r, w):
        self.kb.op("dve", lambda e: e.reciprocal(out=out, in_=in_), r, w)

    def scan(self, out, d0, d1, init, r, w):
        self.kb.op("dve", lambda e: e.tensor_tensor_scan(out=out, data0=d0, data1=d1, initial=init, op0=ALU.mult, op1=ALU.add), r, w)

    def gelu_tanh(self, out, src, srcdeps, wk, outdeps):
        (xs, xsap), (t, tap) = wk
        self.cp("act", xsap, src, srcdeps, [xs.d()])
        self.tt("dve", tap, xsap, xsap, ALU.mult, [xs.d()], [t.d()])
        self.ts("dve", tap, tap, 0.044715, 1.0, ALU.mult, ALU.add, [t.d()], [t.d()])
        self.tt("dve", tap, tap, xsap, ALU.mult, [t.d(), xs.d()], [t.d()])
        self.act(tap, tap, AF.Sigmoid, [t.d()], [t.d()], scale=1.5957691216057308)
        self.tt("pool", out, xsap, tap, ALU.mult, [xs.d(), t.d()], outdeps)

    def sincos(self, ang, cos_out, sin_out, tmpf, tmpi, r, w):
        TWO_PI = 2.0 * math.pi
        for out, shift in ((sin_out, 0.0), (cos_out, 0.25)):
            self.ts("dve", tmpf, ang, 1.0 / TWO_PI, shift, ALU.mult, ALU.add, r, w)
            self.cp("dve", tmpi, tmpf, w, w)
            self.cp("dve", out, tmpi, w, w)
            self.tt("dve", tmpf, tmpf, out, ALU.subtract, w, w)
            self.ts("dve", out, tmpf, 0.5, None, ALU.is_gt, None, w, w)
            self.tt("dve", tmpf, tmpf, out, ALU.subtract, w, w)
            self.ts("dve", out, tmpf, -0.5, None, ALU.is_lt, None, w, w)
            self.tt("dve", tmpf, tmpf, out, ALU.add, w, w)
            self.act(out, tmpf, AF.Sin, w, w, scale=TWO_PI * (1.0 - 1e-6))

    def cp(self, eng, out, in_, r, w):
        if eng == "act":
            self.kb.op("act", lambda e: e.activation(out=out, in_=in_, func=AF.Copy), r, w)
        else:
            self.kb.op(eng, lambda e: e.tensor_copy(out=out, in_=in_), r, w)

    def mm(self, out, lhsT, rhs, start, stop, r, w):
        self.kb.op("pe", lambda e: e.matmul(out, lhsT=lhsT, rhs=rhs, start=start, stop=stop), r, w)

    def tr(self, out, in_, ident, r, w):
        self.kb.op("pe", lambda e: e.transpose(out, in_, ident), r, w)

    def dma(self, q, out, in_, r, w, is_output=False, slow=False):
        if slow:
            self.kb.dma(q, lambda e: e.dma_start(out=out, in_=in_, allow_slow_non_contiguous=True), r, w, is_output=is_output)
        else:
            self.kb.dma(q, lambda e: e.dma_start(out=out, in_=in_), r, w, is_output=is_output)

    def memset(self, eng, ap, val, w):
        self.kb.op(eng, lambda e: e.memset(ap, val), (), w)

    def build(self):
        kb = self.kb
        din, dscr = self.din, self.dscr
        x_in = din("x", [T_LAT, D])
        ctx_in = din("ctx", [T_CTX, D])
        cc_in = din("cc", [2, D])
        w_ada = din("w_ada", [DEPTH, D, 6 * D])
        b_ada = din("b_ada", [DEPTH, 6 * D])
        ln_g = din("ln_g", [DEPTH, 2, D])
        ln_b = din("ln_b", [DEPTH, 2, D])
        din("attn_w_in", [2, D, 3 * D])
        din("attn_w_in_p", [2, D, 2 * D])
        din("attn_w_out", [2, D, D])
        din("attn_lam", [2, 256])
        din("attn_subln", [2, 128])
        din("router_w", [D, N_EXP])
        din("router_b", [1, N_EXP])
        din("moe_w_gate", [DEPTH, N_EXP, D, D])
        din("moe_w_up", [DEPTH, N_EXP, D, D])
        din("moe_w_down", [DEPTH, N_EXP, D, D])
        din("s5_w_in", [1, D, D])
        for nm in ("s5_a_re", "s5_a_im"):
            din(nm, [1, 2, 64, 64])
        for nm in ("s5_b_re", "s5_b_im"):
            din(nm, [1, 2, 64, 64, 16])
        for nm in ("s5_c_re", "s5_c_im"):
            din(nm, [1, 2, 64, 16, 64])
        din("s5_log_dt", [1, 2, 64])
        din("s5_d", [1, D])
        din("s5_glu_v", [1, D, D])
        din("s5_glu_g", [1, D, D])
        din("lru_w_in", [1, D, 2 * LRU_W])
        din("lru_conv_w", [1, 4, LRU_W])
        din("lru_conv_b", [1, LRU_W])
        din("lru_w_a", [1, 2, 16, 88, 88])
        din("lru_b_a", [1, 2, LRU_W])
        din("lru_w_x", [1, 2, 16, 88, 88])
        din("lru_b_x", [1, 2, LRU_W])
        din("lru_lam", [1, 2, LRU_W])
        din("lru_w_out", [1, LRU_W, D])
        din("k_tau", [128, 32])
        din("k_ident", [128, 128])
        din("k_ltri", [128, 128])
        din("k_eoff", [128, N_EXP])
        din("k_cos", [128, T_ALL])
        din("k_sin", [128, T_ALL])
        out = self.nc.dram_tensor("out", [T_LAT, D], F32, kind="ExternalOutput").ap()
        xs = dscr("xs", [T_ALL, D])
        mod = dscr("mod", [DEPTH, 2, 6 * D])
        dscr("Xg", [N_EXP * C_CAP, D], BF16)
        dscr("Yg", [N_EXP * C_CAP, D], F32)
        dscr("gT", [11, 128, T_ALL], BF16)

        AB, AFa = self.AB, self.AF_
        self.ident_f = AFa.alloc(128)
        self.ident_b = AB.alloc(128)
        self.ltri_b = AB.alloc(128)
        self.ones_b = AB.alloc(128)
        self.eoff = AFa.alloc(N_EXP)
        self.rb_bc = AFa.alloc(N_EXP)
        self.rw = AFa.alloc(8 * N_EXP, [8, N_EXP])
        self.dma("sp", self.ident_f.ap, self.inp["k_ident"], [], [self.ident_f.d()])
        self.cp("dve", self.ident_b.ap, self.ident_f.ap, [self.ident_f.d()], [self.ident_b.d()])
        tmpf = AFa.alloc(128)
        self.dma("sp", tmpf.ap, self.inp["k_ltri"], [], [tmpf.d()])
        self.cp("dve", self.ltri_b.ap, tmpf.ap, [tmpf.d()], [self.ltri_b.d()])
        self.memset("dve", self.ones_b.ap, 1.0, [self.ones_b.d()])
        self.ones_f = AFa.alloc(128)
        self.memset("dve", self.ones_f.ap, 1.0, [self.ones_f.d()])
        self.dma("sp", self.eoff.ap, self.inp["k_eoff"], [], [self.eoff.d()])
        self.dma("sp", self.rb_bc.ap, self.inp["router_b"].partition_broadcast(128), [], [self.rb_bc.d()])
        self.dma("sp", self.rw.ap, self.inp["router_w"].rearrange("(kt p) e -> p kt e", p=128), [], [self.rw.d()])
        self.dest_i = Tl(self.AI.t[:, 0:2 * NT].rearrange("p (t k) -> p t k", k=2))
        self.dest_g = Tl(self.AI.t[:, 64:64 + 2 * NT].rearrange("p (t k) -> p t k", k=2))
        self.itmp = Tl(self.AI.t[:, 256:1280])
        self.gatew = AFa.alloc(2 * NT, [NT, 2])
        self.bc = {nm: AFa.alloc(D) for nm in ("sh_l", "sc_l", "g_l", "sh_c", "sc_c", "g_c", "lng", "lnb")}
        self.base_mark_b = AB.mark()
        self.base_mark_f = AFa.mark()

        self.dma("sp", xs[0:T_CTX, :], ctx_in, [], [self.ddep("xs", t) for t in range(2)])
        self.dma("sp", xs[T_CTX:, :], x_in, [], [self.ddep("xs", t) for t in range(2, NT)])

        self.adaln(cc_in, w_ada, b_ada, mod)

        for l in self.layers:
            kind, slot, last = l % 3, l // 3, l == DEPTH - 1
            tiles = list(range(2, NT)) if last else list(range(NT))
            if kind == 0:
                lam_init = 0.8 - 0.6 * math.exp(-0.3 * l)
                self.attention(l, slot, lam_init, last)
            elif kind == 1:
                self.s5(l, slot)
            else:
                self.rglru(l, slot)
            self.moe(l, tiles)

        kb.barrier()
        self.dma("sp", out, xs[T_CTX:, :], [self.ddep("xs", t) for t in range(2, NT)], [], is_output=True)
        kb.finish()
        return self.nc

    def adaln(self, cc_in, w_ada, b_ada, mod):
        AFa = self.AF_
        m = AFa.mark()
        craw = AFa.alloc(16, [8, 2])
        sT = AFa.alloc(16, [8, 2])
        for r in range(2):
            self.dma("sp", craw.ap[:, :, r], cc_in[r:r + 1, :].rearrange("o (kt p) -> p (o kt)", p=128), [], [craw.d()], slow=True)
        self.act(sT.ap, craw.ap, AF.Silu, [craw.d()], [sT.d()])
        bbs = [AFa.alloc(512) for _ in range(2)]
        obs = [AFa.alloc(512) for _ in range(2)]
        wbuf = [AFa.alloc(8 * 256, [8, 256]) for _ in range(2)]
        i = 0
        for l in range(DEPTH):
            for n in range(24):
                wb, bb, ob = wbuf[i % 2], bbs[i % 2], obs[i % 2]
                i += 1
                cs = slice(n * 256, (n + 1) * 256)
                self.dma("sp", bb.ap[0:2, 0:256], b_ada[l:l + 1, cs].partition_broadcast(2), [], [bb.d()])
                self.dma("sp", wb.ap, w_ada[l].rearrange("(kt p) n -> p kt n", p=128)[:, :, cs], [], [wb.d()])
                ps = self.nps()
                for kt in range(8):
                    self.mm(ps.ap[0:2, 0:256], sT.ap[:, kt, :], wb.ap[:, kt, :], kt == 0, kt == 7, [sT.d(), wb.d()], [ps.d()])
                self.tt("dve", ob.ap[0:2, 0:256], ps.ap[0:2, 0:256], bb.ap[0:2, 0:256], ALU.add, [ps.d(), bb.d()], [ob.d()])
                self.dma("sp", mod[l, :, cs], ob.ap[0:2, 0:256], [ob.d()], [self.ddep("mod", l)])
        self.kb.barrier()
        AFa.release(m)

    def load_mod(self, l, sub):
        mod = self.dr["mod"]
        for r, sfx in ((0, "l"), (1, "c")):
            for j, nm in enumerate(("sh", "sc", "g")):
                t = self.bc[f"{nm}_{sfx}"]
                col = (3 * sub + j) * D
                self.dma("sp", t.ap, mod[l, r:r + 1, col:col + D].partition_broadcast(128), [self.ddep("mod", l)], [t.d()])
            t = self.bc[f"sc_{sfx}"]
            self.ts("dve", t.ap, t.ap, 1.0, None, ALU.add, None, [t.d()], [t.d()])
        self.dma("sp", self.bc["lng"].ap, self.inp["ln_g"][l, sub:sub + 1, :].partition_broadcast(128), [], [self.bc["lng"].d()])
        self.dma("sp", self.bc["lnb"].ap, self.inp["ln_b"][l, sub:sub + 1, :].partition_broadcast(128), [], [self.bc["lnb"].d()])

    def modulate(self, t, hx):
        xs = self.dr["xs"]
        sfx = "c" if t < 2 else "l"
        self.dma("sp", hx.ap, xs[t * 128:(t + 1) * 128, :], [self.ddep("xs", t)], [hx.d()])
        sc, sh = self.bc[f"sc_{sfx}"], self.bc[f"sh_{sfx}"]
        self.tt("dve", hx.ap, hx.ap, sc.ap, ALU.mult, [hx.d(), sc.d()], [hx.d()])
        self.tt("pool", hx.ap, hx.ap, sh.ap, ALU.add, [hx.d(), sh.d()], [hx.d()])

    def post(self, t, ysrc, ydeps, work):
        xs = self.dr["xs"]
        sfx = "c" if t < 2 else "l"
        g = self.bc[f"g_{sfx}"]
        xr, t1, st, mv = work
        self.dma("sp", xr.ap, xs[t * 128:(t + 1) * 128, :], [self.ddep("xs", t)], [xr.d()])
        for h in range(2):
            sl = slice(h * 512, (h + 1) * 512)
            self.tt("dve", t1.ap[:, sl], ysrc[h], g.ap[:, sl], ALU.mult, ydeps + [g.d()], [t1.d()])
        self.stt(t1.ap, xr.ap, float(DN_ALPHA), t1.ap, ALU.mult, ALU.add, [xr.d(), t1.d()], [t1.d()])
        for h in range(2):
            self.kb.op("dve", lambda e, h=h: e.bn_stats(out=st.ap[:, h * 6:(h + 1) * 6], in_=t1.ap[:, h * 512:(h + 1) * 512]),
                       [t1.d()], [st.d()])
        self.kb.op("dve", lambda e: e.bn_aggr(out=mv.ap[:, 0:2], in_=st.ap[:, 0:12]), [st.d()], [mv.d()])
        self.ts("dve", mv.ap[:, 2:3], mv.ap[:, 1:2], float(LN_EPS), None, ALU.add, None, [mv.d()], [mv.d()])
        self.act(mv.ap[:, 3:4], mv.ap[:, 2:3], AF.Sqrt, [mv.d()], [mv.d()])
        self.kb.op("dve", lambda e: e.reciprocal(out=mv.ap[:, 4:5], in_=mv.ap[:, 3:4]), [mv.d()], [mv.d()])
        self.ts("dve", t1.ap, t1.ap, mv.ap[:, 0:1], mv.ap[:, 4:5], ALU.subtract, ALU.mult, [t1.d(), mv.d()], [t1.d()])
        self.tt("pool", t1.ap, t1.ap, self.bc["lng"].ap, ALU.mult, [t1.d(), self.bc["lng"].d()], [t1.d()])
        self.tt("dve", t1.ap, t1.ap, self.bc["lnb"].ap, ALU.add, [t1.d(), self.bc["lnb"].d()], [t1.d()])
        self.dma("sp", xs[t * 128:(t + 1) * 128, :], t1.ap, [t1.d()], [self.ddep("xs", t)])

    def post_work(self):
        AFa = self.AF_
        return [(AFa.alloc(D), AFa.alloc(D), AFa.alloc(12), AFa.alloc(8)) for _ in range(2)]

    def transpose_tile(self, hx, dst_b, dst_key, t, dst_f=None):
        for half in range(2):
            ps = self.nps()
            for j in range(4):
                kt = half * 4 + j
                self.tr(ps.ap[:, j * 128:(j + 1) * 128], hx.ap[:, kt * 128:(kt + 1) * 128], self.ident_f.ap,
                        [hx.d(), self.ident_f.d()], [ps.d()])
            src = ps.ap.rearrange("p (j q) -> p j q", j=4)
            self.cp(self.ev(), dst_b.ap[:, half * 4:(half + 1) * 4, t * 128:(t + 1) * 128], src, [ps.d()], [dst_b.d(dst_key)])
            if dst_f is not None:
                self.cp(self.ev(), dst_f.ap[:, half * 4:(half + 1) * 4, :], src, [ps.d()], [dst_f.d()])

    def moe(self, l, tiles):
        kb = self.kb
        AB, AFa = self.AB, self.AF_
        Xg, Yg = self.dr["Xg"], self.dr["Yg"]
        kb.barrier()
        mb, mf = AB.mark(), AFa.mark()
        self.load_mod(l, 1)
        wg = [AB.alloc(8 * D, [8, D]) for _ in range(2)]
        wu = [AB.alloc(8 * D, [8, D]) for _ in range(2)]
        wd = [AB.alloc(8 * D, [8, D]) for _ in range(2)]
        wsrc = {"g": self.inp["moe_w_gate"], "u": self.inp["moe_w_up"], "d": self.inp["moe_w_down"]}

        def load_w(e):
            for W, nm in ((wg[e % 2], "g"), (wu[e % 2], "u"), (wd[e % 2], "d")):
                src = wsrc[nm][l, e].rearrange("(kt p) n -> p kt n", p=128)
                for hh in range(2):
                    self.dma("pool", W.ap[:, hh * 4:(hh + 1) * 4, :], src[:, hh * 4:(hh + 1) * 4, :], [], [W.d()])

        load_w(0)
        mbA = AB.mark()
        hxs = [AFa.alloc(D) for _ in range(2)]
        hbs = [AB.alloc(D) for _ in range(2)]
        hTf = [AFa.alloc(8 * 128, [8, 128]) for _ in range(2)]
        rt = [AFa.alloc(256) for _ in range(2)]
        base = AFa.alloc(N_EXP)
        mask_b = [AB.alloc(N_EXP) for _ in range(2)]
        self.memset("dve", base.ap, 0.0, [base.d()])
        for it, t in enumerate(tiles):
            hx, hb, hf, r, mk = hxs[it % 2], hbs[it % 2], hTf[it % 2], rt[it % 2], mask_b[it % 2]
            R = r.ap
            rd = [r.d()]
            self.modulate(t, hx)
            self.cp("act", hb.ap, hx.ap, [hx.d()], [hb.d()])
            for half in range(2):
                ps = self.nps()
                for j in range(4):
                    kt = half * 4 + j
                    self.tr(ps.ap[:, j * 128:(j + 1) * 128], hx.ap[:, kt * 128:(kt + 1) * 128], self.ident_f.ap,
                            [hx.d(), self.ident_f.d()], [ps.d()])
                self.cp(self.ev(), hf.ap[:, half * 4:(half + 1) * 4, :], ps.ap.rearrange("p (j q) -> p j q", j=4), [ps.d()], [hf.d()])
            pl = self.nps()
            for kt in range(8):
                self.mm(pl.ap[:, 0:N_EXP], hf.ap[:, kt, :], self.rw.ap[:, kt, :], kt == 0, kt == 7, [hf.d(), self.rw.d()], [pl.d()])
            lg, pr, sel, tmp, oh1, oh2 = (R[:, 0:16], R[:, 16:32], R[:, 32:48], R[:, 48:64], R[:, 64:80], R[:, 80:96])
            g4, s6, sc1 = R[:, 96:100], R[:, 100:124], R[:, 124:140]
            mx, sm, rs, m1, m2, p1, p2, ps_, d1, d2 = (R[:, 140 + i:141 + i] for i in range(10))
            self.cp("dve", lg, pl.ap[:, 0:N_EXP], [pl.d()], rd)
            self.red("max", mx, lg, rd, rd)
            self.ts("dve", mx, mx, -1.0, None, ALU.mult, None, rd, rd)
            self.act(pr, lg, AF.Exp, rd, rd, bias=mx, accum=sm)
            self.recip(rs, sm, rd, rd)
            self.ts("dve", pr, pr, rs, None, ALU.mult, None, rd, rd)
            self.tt("dve", sel, pr, self.rb_bc.ap, ALU.add, rd + [self.rb_bc.d()], rd)
            sv = sel.rearrange("p (g k) -> p g k", k=4)
            s6v = s6.rearrange("p (q g) -> p q g", q=6)
            pairs = [(0, 1), (0, 2), (0, 3), (1, 2), (1, 3), (2, 3)]
            for qi, (a, b) in enumerate(pairs):
                self.tt("dve", s6v[:, qi, :], sv[:, :, a], sv[:, :, b], ALU.add, rd, rd)
            self.tt("dve", g4, s6v[:, 0, :], s6v[:, 1, :], ALU.max, rd, rd)
            for qi in range(2, 6):
                self.tt("dve", g4, g4, s6v[:, qi, :], ALU.max, rd, rd)
            self.red("max", mx, g4, rd, rd)
            self.ts("dve", g4, g4, mx, -1e30, ALU.is_lt, ALU.mult, rd, rd)
            tv = tmp.rearrange("p (g k) -> p g k", k=4)
            for k in range(4):
                self.tt("dve", tv[:, :, k], sv[:, :, k], g4, ALU.add, rd, rd)
            self.red("max", m1, tmp, rd, rd)
            self.ts("dve", oh1, tmp, m1, None, ALU.is_ge, None, rd, rd)
            self.stt(sc1, oh1, -1e30, tmp, ALU.mult, ALU.add, rd, rd)
            self.red("max", m2, sc1, rd, rd)
            self.ts("dve", oh2, sc1, m2, None, ALU.is_ge, None, rd, rd)
            self.tt("dve", sc1, pr, oh1, ALU.mult, rd, rd)
            self.red("sum", p1, sc1, rd, rd)
            self.tt("dve", sc1, pr, oh2, ALU.mult, rd, rd)
            self.red("sum", p2, sc1, rd, rd)
            self.tt("dve", ps_, p1, p2, ALU.add, rd, rd)
            self.recip(ps_, ps_, rd, rd)
            gw = self.gatew
            self.tt("dve", gw.ap[:, t, 0:1], p1, ps_, ALU.mult, rd, [gw.d(t)])
            self.tt("dve", gw.ap[:, t, 1:2], p2, ps_, ALU.mult, rd, [gw.d(t)])
            self.tt("dve", mk.ap, oh1, oh2, ALU.add, rd, [mk.d()])
            pc = self.nps()
            self.mm(pc.ap[:, 0:N_EXP], self.ltri_b.ap, mk.ap, True, True, [self.ltri_b.d(), mk.d()], [pc.d()])
            self.mm(pc.ap[:, 16:16 + N_EXP], self.ones_b.ap, mk.ap, True, True, [self.ones_b.d(), mk.d()], [pc.d()])
            self.tt("dve", sc1, pc.ap[:, 0:N_EXP], base.ap, ALU.add, [pc.d(), base.d()], rd)
            self.tt("dve", base.ap, base.ap, pc.ap[:, 16:16 + N_EXP], ALU.add, [pc.d(), base.d()], [base.d()])
            self.ts("dve", lg, sc1, float(C_CAP) - 0.5, 1.0e6, ALU.is_gt, ALU.mult, rd, rd)
            self.tt("dve", sc1, sc1, lg, ALU.add, rd, rd)
            self.tt("dve", sc1, sc1, self.eoff.ap, ALU.add, rd + [self.eoff.d()], rd)
            self.tt("dve", lg, sc1, oh1, ALU.mult, rd, rd)
            self.red("sum", d1, lg, rd, rd)
            self.tt("dve", lg, sc1, oh2, ALU.mult, rd, rd)
            self.red("sum", d2, lg, rd, rd)
            di = self.dest_i
            self.cp("dve", di.ap[:, t, 0:1], d1, rd, [di.d(t)])
            self.cp("dve", di.ap[:, t, 1:2], d2, rd, [di.d(t)])
            dg = self.dest_g
            self.ts("dve", d1, d1, float(N_EXP * C_CAP - 1), None, ALU.min, None, rd, rd)
            self.ts("dve", d2, d2, float(N_EXP * C_CAP - 1), None, ALU.min, None, rd, rd)
            self.cp("dve", dg.ap[:, t, 0:1], d1, rd, [dg.d(t)])
            self.cp("dve", dg.ap[:, t, 1:2], d2, rd, [dg.d(t)])
            for k in range(2):
                self.kb.dma("pool", lambda e, k=k, t=t, hb=hb: e.indirect_dma_start(
                    out=Xg, out_offset=bass.IndirectOffsetOnAxis(ap=di.ap[:, t, k:k + 1], axis=0),
                    in_=hb.ap, in_offset=None, bounds_check=self.bound_reg(e), oob_is_err=False),
                    [di.d(t), hb.d()], [self.ddep("Xg")])
        if "dest" in self.debug:
            pass
        kb.barrier()
        AB.release(mbA)
        AFa.release(mf)
        mf = AFa.mark()
        RB = 256
        xtm = [AB.alloc(2 * D, [2, D]) for _ in range(2)]
        xT = [AB.alloc(8 * RB, [8, RB]) for _ in range(2)]
        hT = [AB.alloc(8 * RB, [8, RB]) for _ in range(2)]
        sg = [AFa.alloc(RB) for _ in range(2)]
        yst = [AFa.alloc(D) for _ in range(2)]
        blocks = [(e, rb) for e in range(N_EXP) for rb in range(C_CAP // RB)]

        def stT(bi):
            e, rb = blocks[bi]
            X, XT = xtm[bi % 2], xT[bi % 2]
            r0 = e * C_CAP + rb * RB
            self.dma("sp", X.ap, Xg[r0:r0 + RB, :].rearrange("(j p) d -> p j d", p=128), [self.ddep("Xg")], [X.d()])
            for j in range(RB // 128):
                pb = self.npsb()
                for kt in range(8):
                    self.tr(pb.ap[:, kt * 128:(kt + 1) * 128], X.ap[:, j, kt * 128:(kt + 1) * 128], self.ident_b.ap,
                            [X.d(), self.ident_b.d()], [pb.d()])
                self.cp(self.ev(), XT.ap[:, :, j * 128:(j + 1) * 128], pb.ap.rearrange("p (k q) -> p k q", k=8), [pb.d()], [XT.d()])

        def stGU(bi):
            e, rb = blocks[bi]
            Wg, Wu = wg[e % 2], wu[e % 2]
            XT, HT = xT[bi % 2], hT[bi % 2]
            for j in range(8):
                pg, pu = self.nps(), self.nps()
                for kt in range(8):
                    self.mm(pg.ap[:, 0:RB], Wg.ap[:, kt, j * 128:(j + 1) * 128], XT.ap[:, kt, :], kt == 0, kt == 7, [Wg.d(), XT.d()], [pg.d()])
                for kt in range(8):
                    self.mm(pu.ap[:, 0:RB], Wu.ap[:, kt, j * 128:(j + 1) * 128], XT.ap[:, kt, :], kt == 0, kt == 7, [Wu.d(), XT.d()], [pu.d()])
                s_ = sg[j % 2]
                self.act(s_.ap, pg.ap[:, 0:RB], AF.Silu, [pg.d()], [s_.d()])
                self.tt("dve", HT.ap[:, j, :], s_.ap, pu.ap[:, 0:RB], ALU.mult, [s_.d(), pu.d()], [HT.d()])

        def stY(bi):
            e, rb = blocks[bi]
            Wd, HT = wd[e % 2], hT[bi % 2]
            r0 = e * C_CAP + rb * RB
            for r in range(RB // 128):
                ys = yst[r % 2]
                for c in range(2):
                    py = self.nps()
                    for j in range(8):
                        self.mm(py.ap, HT.ap[:, j, r * 128:(r + 1) * 128], Wd.ap[:, j, c * 512:(c + 1) * 512], j == 0, j == 7,
                                [HT.d(), Wd.d()], [py.d()])
                    self.cp(self.ev(), ys.ap[:, c * 512:(c + 1) * 512], py.ap, [py.d()], [ys.d()])
                rr = r0 + r * 128
                self.dma("sp", Yg[rr:rr + 128, :], ys.ap, [ys.d()], [self.ddep("Yg")])

        stT(0)
        for bi, (e, rb) in enumerate(blocks):
            if rb == 0 and e + 1 < N_EXP:
                load_w(e + 1)
            stGU(bi)
            if bi + 1 < len(blocks):
                stT(bi + 1)
            stY(bi)
        kb.barrier()
        AB.release(mb)
        AFa.release(mf)
        mb, mf = AB.mark(), AFa.mark()
        y1 = [AFa.alloc(D) for _ in range(2)]
        y2 = [AFa.alloc(D) for _ in range(2)]
        pw = self.post_work()
        di, gw, dg = self.dest_i, self.gatew, self.dest_g
        for it, t in enumerate(tiles):
            a, b = y1[it % 2], y2[it % 2]
            for k, dst in ((0, a), (1, b)):
                self.memset("pool", dst.ap, 0.0, [dst.d()])
                self.kb.dma("pool", lambda e, k=k, t=t, dst=dst: e.indirect_dma_start(
                    out=dst.ap, out_offset=None, in_=Yg,
                    in_offset=bass.IndirectOffsetOnAxis(ap=dg.ap[:, t, k:k + 1], axis=0)),
                    [dg.d(t), self.ddep("Yg")], [dst.d()])
            self.ts("dve", a.ap, a.ap, gw.ap[:, t, 0:1], None, ALU.mult, None, [a.d(), gw.d(t)], [a.d()])
            self.stt(a.ap, b.ap, gw.ap[:, t, 1:2], a.ap, ALU.mult, ALU.add, [a.d(), b.d(), gw.d(t)], [a.d()])
            self.post(t, [a.ap[:, 0:512], a.ap[:, 512:1024]], [a.d()], pw[it % 2])
        kb.barrier()
        AB.release(mb)
        AFa.release(mf)

    def s5(self, l, slot):
        kb = self.kb
        AB, AFa = self.AB, self.AF_
        kb.barrier()
        mb, mf = AB.mark(), AFa.mark()
        self.load_mod(l, 0)
        L = 32
        NCH = T_ALL // L
        chunks = [(0, 256)] + [(256 + i * 512, 512) for i in range(4)]
        yT = AB.alloc(8 * T_ALL, [8, T_ALL])
        uT = AB.alloc(8 * T_ALL, [8, T_ALL])
        mb_w = AB.mark()
        mf1 = AFa.mark()
        hxs = [AFa.alloc(D) for _ in range(2)]
        for t in range(NT):
            hx = hxs[t % 2]
            self.modulate(t, hx)
            self.transpose_tile(hx, yT, t, t)
        win = AB.alloc(8 * D, [8, D])
        w_in = self.inp["s5_w_in"][slot].rearrange("(kt p) n -> p kt n", p=128)
        for hh in range(2):
            self.dma("pool", win.ap[:, hh * 4:(hh + 1) * 4, :], w_in[:, hh * 4:(hh + 1) * 4, :], [], [win.d()])
        for ft in range(8):
            for ci, (c0, cn) in enumerate(chunks):
                ps = self.nps()
                hdeps = yT.dl(range(c0 // 128, (c0 + cn) // 128))
                for kt in range(8):
                    self.mm(ps.ap[:, 0:cn], win.ap[:, kt, ft * 128:(ft + 1) * 128], yT.ap[:, kt, c0:c0 + cn], kt == 0, kt == 7, [win.d()] + hdeps, [ps.d()])
                self.cp(self.ev(), uT.ap[:, ft, c0:c0 + cn], ps.ap[:, 0:cn], [ps.d()], [uT.d()])
        kb.barrier()
        AB.release(mb_w)
        AFa.release(mf1)
        dsk = AFa.alloc(8)
        self.dma("sp", dsk.ap, self.inp["s5_d"][slot:slot + 1, :].rearrange("o (f p) -> p (o f)", p=128), [], [dsk.d()], slow=True)
        yd = [yT.d("y")]
        for ft in range(8):
            self.ts("dve", yT.ap[:, ft, :], uT.ap[:, ft, :], dsk.ap[:, ft:ft + 1], None, ALU.mult, None, [uT.d(), dsk.d()], yd)
        tau = AFa.alloc(32)
        self.dma("sp", tau.ap, self.inp["k_tau"], [], [tau.d()])
        Bw = [AB.alloc(32 * 128, [32, 128]) for _ in range(2)]
        Cw = [AB.alloc(32 * 128, [32, 128]) for _ in range(2)]
        hb_ = [[AB.alloc(8 * L, [8, L]) for _ in range(2)] for _ in range(2)]
        mf2 = AFa.mark()
        for d in range(2):
            kb.barrier()
            AFa.release(mf2)
            stg = AFa.alloc(32 * 128, [32, 128])
            for ri, nm in enumerate(("s5_b_re", "s5_b_im")):
                self.memset("pool", stg.ap, 0.0, stg.dl(range(64)))
                for g in range(64):
                    dst = stg.ap[(g % 8) * 16:(g % 8) * 16 + 16, g // 2, (g % 2) * 64:(g % 2) * 64 + 64]
                    self.dma("sp", dst, self.inp[nm][slot, d, g].rearrange("p n -> n p"), [], [stg.d(g)], slow=True)
                self.cp("act", Bw[ri].ap, stg.ap, stg.dl(range(64)), [Bw[ri].d()])
            for ri, nm in enumerate(("s5_c_re", "s5_c_im")):
                self.memset("pool", stg.ap, 0.0, stg.dl(range(64)))
                for g in range(64):
                    dst = stg.ap[(g % 2) * 64:(g % 2) * 64 + 64, g // 2, (g % 8) * 16:(g % 8) * 16 + 16]
                    self.dma("sp", dst, self.inp[nm][slot, d, g].rearrange("n p -> p n"), [], [stg.d(g)], slow=True)
                if ri == 0:
                    self.cp("act", Cw[0].ap, stg.ap, stg.dl(range(64)), [Cw[0].d()])
                else:
                    self.ts("dve", Cw[1].ap, stg.ap, -1.0, None, ALU.mult, None, stg.dl(range(64)), [Cw[1].d()])
            kb.barrier()
            AFa.release(mf2)
            P = AFa.alloc(16 * 32, [16, 32])
            pd = [P.d()]
            ar, ai, dt, mag, ang, cs, sn, zr, zi, den, kr, ki, t0, t1, rc_r, rc_i = (P.ap[:, i, :] for i in range(16))
            for two in range(2):
                prt = slice(two * 64, two * 64 + 64)
                self.dma("sp", P.ap[prt, 0, :], self.inp["s5_a_re"][slot, d].rearrange("(s two) p -> two p s", two=2)[two], [], pd, slow=True)
                self.dma("sp", P.ap[prt, 1, :], self.inp["s5_a_im"][slot, d].rearrange("(s two) p -> two p s", two=2)[two], [], pd, slow=True)
                self.dma("sp", P.ap[prt, 2, :], self.inp["s5_log_dt"][slot, d:d + 1, :].rearrange("o (s two) -> two o s", two=2)[two].partition_broadcast(64), [], pd, slow=True)
            self.act(dt, dt, AF.Exp, pd, pd)
            self.tt("dve", mag, ar, dt, ALU.mult, pd, pd)
            self.act(mag, mag, AF.Exp, pd, pd)
            self.tt("dve", ang, ai, dt, ALU.mult, pd, pd)
            it_ = self.itmp
            self.sincos(ang, cs, sn, t0, it_.ap[:, 0:32], pd + [it_.d()], pd + [it_.d()])
            self.tt("dve", zr, mag, cs, ALU.mult, pd, pd)
            self.tt("dve", zi, mag, sn, ALU.mult, pd, pd)
            self.tt("dve", den, ar, ar, ALU.mult, pd, pd)
            self.tt("dve", t0, ai, ai, ALU.mult, pd, pd)
            self.tt("dve", den, den, t0, ALU.add, pd, pd)
            self.recip(den, den, pd, pd)
            self.ts("dve", zr, zr, -1.0, None, ALU.add, None, pd, pd)
            self.tt("dve", kr, zr, ar, ALU.mult, pd, pd)
            self.tt("dve", t0, zi, ai, ALU.mult, pd, pd)
            self.tt("dve", kr, kr, t0, ALU.add, pd, pd)
            self.tt("dve", kr, kr, den, ALU.mult, pd, pd)
            self.tt("dve", ki, zi, ar, ALU.mult, pd, pd)
            self.tt("dve", t0, zr, ai, ALU.mult, pd, pd)
            self.tt("dve", ki, ki, t0, ALU.subtract, pd, pd)
            self.tt("dve", ki, ki, den, ALU.mult, pd, pd)
            NTB = 32 * L
            Ct, St, T1r, T1i, rz, tmp = (AFa.alloc(NTB) for _ in range(6))
            td = [Ct.d()]
            v3 = lambda tl: tl.ap.rearrange("p (s l) -> p s l", s=32)
            bc_s = lambda a2: a2.unsqueeze(2).to_broadcast([128, 32, L])
            self.tt("dve", v3(tmp), bc_s(ang), tau.ap.unsqueeze(1).to_broadcast([128, 32, L]), ALU.mult, pd + [tau.d()], td)
            self.sincos(tmp.ap, Ct.ap, St.ap, T1r.ap, it_.ap[:, 0:NTB], td + [it_.d()], td + [it_.d()])
            self.tt("dve", v3(T1r), v3(Ct), bc_s(kr), ALU.mult, td + pd, td)
            self.tt("dve", v3(tmp), v3(St), bc_s(ki), ALU.mult, td + pd, td)
            self.tt("dve", T1r.ap, T1r.ap, tmp.ap, ALU.add, td, td)
            self.tt("dve", v3(T1i), v3(Ct), bc_s(ki), ALU.mult, td + pd, td)
            self.tt("dve", v3(tmp), v3(St), bc_s(kr), ALU.mult, td + pd, td)
            self.tt("dve", T1i.ap, T1i.ap, tmp.ap, ALU.subtract, td, td)
            self.cp("dve", v3(rz), bc_s(mag), td + pd, td)
            self.memset("dve", v3(rz)[:, :, 0], 0.0, td)
            self.memset("dve", rc_r, 0.0, pd)
            self.memset("dve", rc_i, 0.0, pd)
            wk = [AFa.alloc(8 * L, [8, L]) for _ in range(6)]
            gbuf = [[AFa.alloc(8 * L, [8, L]) for _ in range(2)] for _ in range(2)]
            f2 = lambda tl: tl.ap.rearrange("p s l -> p (s l)")

            def mk_tok(lo_, rev):
                def tok(ap2):
                    v = ap2[:, lo_:lo_ + L]
                    return v[:, ::-1] if rev else v
                return tok

            units = []
            for c in range(NCH):
                if d == 0:
                    lo = c * L
                elif c < T_CTX // L:
                    lo = T_CTX - (c + 1) * L
                else:
                    lo = T_ALL - (c - T_CTX // L + 1) * L
                for fp in range(4):
                    units.append(dict(c=c, fp=fp, tok=mk_tok(lo, d == 1), idx=c * 4 + fp))

            def stA(u):
                fp, tok = u["fp"], u["tok"]
                pbu = self.nps()
                u["pbu"] = pbu
                for s8 in range(8):
                    s_ = 8 * fp + s8
                    rhs = tok(uT.ap[:, s_ // 4, :])
                    self.mm(pbu.ap[:, s8 * L:(s8 + 1) * L], Bw[0].ap[:, s_, :], rhs, True, True, [Bw[0].d(), uT.d()], [pbu.d()])
                    self.mm(pbu.ap[:, 256 + s8 * L:256 + (s8 + 1) * L], Bw[1].ap[:, s_, :], rhs, True, True, [Bw[1].d(), uT.d()], [pbu.d()])

            def stB(u):
                fp, c, pbu = u["fp"], u["c"], u["pbu"]
                m1, m2, br, bi, m3, m4 = wk
                gr, gi = gbuf[u["idx"] % 2]
                u["g"] = (gr, gi)
                ss = slice(8 * fp, 8 * fp + 8)
                fl = slice(8 * fp * L, (8 * fp + 8) * L)
                bur = pbu.ap[:, 0:256].rearrange("p (s l) -> p s l", s=8)
                bui = pbu.ap[:, 256:512].rearrange("p (s l) -> p s l", s=8)
                t1r, t1i = v3(T1r)[:, ss, :], v3(T1i)[:, ss, :]
                self.tt("dve", m1.ap, bur, t1r, ALU.mult, [pbu.d()] + td, [m1.d()])
                self.tt("dve", m2.ap, bui, t1i, ALU.mult, [pbu.d()] + td, [m2.d()])
                self.tt("dve", br.ap, m1.ap, m2.ap, ALU.subtract, [m1.d(), m2.d()], [br.d()])
                self.tt("dve", m1.ap, bui, t1r, ALU.mult, [pbu.d()] + td, [m1.d()])
                self.tt("dve", m2.ap, bur, t1i, ALU.mult, [pbu.d()] + td, [m2.d()])
                self.tt("dve", bi.ap, m1.ap, m2.ap, ALU.add, [m1.d(), m2.d()], [bi.d()])
                if c > 0:
                    self.tt("dve", br.ap[:, :, 0], br.ap[:, :, 0], rc_r[:, ss], ALU.add, [br.d(), rcd[fp]], [br.d()])
                    self.tt("dve", bi.ap[:, :, 0], bi.ap[:, :, 0], rc_i[:, ss], ALU.add, [bi.d(), rcd[fp]], [bi.d()])
                self.scan(f2(gr), rz.ap[:, fl], f2(br), 0.0, [br.d()] + td, [gr.d()])
                self.scan(f2(gi), rz.ap[:, fl], f2(bi), 0.0, [bi.d()] + td, [gi.d()])

            def stC(u):
                fp = u["fp"]
                m1, m2, br, bi, m3, m4 = wk
                gr, gi = u["g"]
                hrb, hib = hb_[u["idx"] % 2]
                u["h"] = (hrb, hib)
                ss = slice(8 * fp, 8 * fp + 8)
                ct, st = v3(Ct)[:, ss, :], v3(St)[:, ss, :]
                gd = [gr.d(), gi.d()]
                self.tt("pool", m3.ap, gr.ap, ct, ALU.mult, gd + td, [m3.d()])
                self.tt("pool", m4.ap, gi.ap, st, ALU.mult, gd + td, [m4.d()])
                self.tt("pool", hrb.ap, m3.ap, m4.ap, ALU.subtract, [m3.d(), m4.d()], [hrb.d()])
                self.tt("pool", m3.ap, gr.ap, st, ALU.mult, gd + td, [m3.d()])
                self.tt("pool", m4.ap, gi.ap, ct, ALU.mult, gd + td, [m4.d()])
                self.tt("pool", hib.ap, m3.ap, m4.ap, ALU.add, [m3.d(), m4.d()], [hib.d()])

            def stD(u):
                fp = u["fp"]
                hrb, hib = u["h"]
                ss = slice(8 * fp, 8 * fp + 8)
                self.tt("dve", rc_r[:, ss], hrb.ap[:, :, L - 1], mag[:, ss], ALU.mult, [hrb.d()] + pd, [rcd[fp]])
                self.tt("dve", rc_i[:, ss], hib.ap[:, :, L - 1], mag[:, ss], ALU.mult, [hib.d()] + pd, [rcd[fp]])

            def stE(u):
                fp, tok = u["fp"], u["tok"]
                hrb, hib = u["h"]
                po = self.nps()
                for q in range(2):
                    ft = 2 * fp + q
                    for s4 in range(4):
                        s_ = 4 * ft + s4
                        s8 = q * 4 + s4
                        self.mm(po.ap[:, q * L:(q + 1) * L], Cw[0].ap[:, s_, :], hrb.ap[:, s8, :], s4 == 0, False, [Cw[0].d(), hrb.d()], [po.d()])
                        self.mm(po.ap[:, q * L:(q + 1) * L], Cw[1].ap[:, s_, :], hib.ap[:, s8, :], False, s4 == 3, [Cw[1].d(), hib.d()], [po.d()])
                for q in range(2):
                    ft = 2 * fp + q
                    ysl = tok(yT.ap[:, ft, :])
                    self.tt("dve", ysl, ysl, po.ap[:, q * L:(q + 1) * L], ALU.add, [po.d()] + yd, yd)

            rcd = [Dep() for _ in range(4)]
            NU = len(units)
            for i in range(NU + 2):
                if i < NU:
                    stA(units[i])
                    stB(units[i])
                if i >= 2:
                    stE(units[i - 2])
                if i < NU:
                    stC(units[i])
                if 1 <= i <= NU:
                    stD(units[i - 1])
        kb.barrier()
        AB.release(mb_w)
        AFa.release(mf1)
        gw = [(AFa.alloc(512), AFa.alloc(512)) for _ in range(2)]
        i = 0
        for ft in range(8):
            for (c0, cn) in chunks:
                a_, b_ = gw[i % 2]
                i += 1
                self.gelu_tanh(yT.ap[:, ft, c0:c0 + cn], yT.ap[:, ft, c0:c0 + cn], yd, ((a_, a_.ap[:, 0:cn]), (b_, b_.ap[:, 0:cn])), yd)
        wv = AB.alloc(8 * D, [8, D])
        wg = AB.alloc(8 * D, [8, D])
        for W, nm in ((wv, "s5_glu_v"), (wg, "s5_glu_g")):
            src = self.inp[nm][slot].rearrange("(kt p) n -> p kt n", p=128)
            for hh in range(2):
                self.dma("pool", W.ap[:, hh * 4:(hh + 1) * 4, :], src[:, hh * 4:(hh + 1) * 4, :], [], [W.d()])
        ys = [AFa.alloc(D) for _ in range(2)]
        sg = [AFa.alloc(512) for _ in range(2)]
        pw = self.post_work()
        for t in range(NT):
            yt = ys[t % 2]
            for c in range(2):
                pv, pg = self.nps(), self.nps()
                for kt in range(8):
                    self.mm(pv.ap, yT.ap[:, kt, t * 128:(t + 1) * 128], wv.ap[:, kt, c * 512:(c + 1) * 512], kt == 0, kt == 7, yd + [wv.d()], [pv.d()])
                for kt in range(8):
                    self.mm(pg.ap, yT.ap[:, kt, t * 128:(t + 1) * 128], wg.ap[:, kt, c * 512:(c + 1) * 512], kt == 0, kt == 7, yd + [wg.d()], [pg.d()])
                s_ = sg[c]
                self.act(s_.ap, pg.ap, AF.Sigmoid, [pg.d()], [s_.d()])
                self.tt("dve", yt.ap[:, c * 512:(c + 1) * 512], pv.ap, s_.ap, ALU.mult, [pv.d(), s_.d()], [yt.d()])
            self.post(t, [yt.ap[:, 0:512], yt.ap[:, 512:1024]], [yt.d()], pw[t % 2])
        kb.barrier()
        AB.release(mb)
        AFa.release(mf)

    def rglru(self, l, slot):
        kb = self.kb
        AB, AFa = self.AB, self.AF_
        gT = self.dr["gT"]
        kb.barrier()
        mb, mf = AB.mark(), AFa.mark()
        self.load_mod(l, 0)
        NJ = 11
        w_in = self.inp["lru_w_in"][slot].rearrange("(kt p) n -> p kt n", p=128)
        chunks = [(0, 256)] + [(256 + i * 512, 512) for i in range(4)]
        xsT = AB.alloc(12 * T_ALL, [12, T_ALL])
        mb_h = AB.mark()
        hT = AB.alloc(8 * T_ALL, [8, T_ALL])
        convw = AFa.alloc(NJ * 4, [NJ, 4])
        convb = AFa.alloc(NJ)
        ba = AFa.alloc(2 * NJ, [2, NJ])
        bx = AFa.alloc(2 * NJ, [2, NJ])
        c8 = AFa.alloc(2 * NJ, [2, NJ])
        for k in range(4):
            self.dma("sp", convw.ap[:, :, k], self.inp["lru_conv_w"][slot, k:k + 1, :].rearrange("o (j p) -> p (o j)", p=128), [], [convw.d()], slow=True)
        self.dma("sp", convb.ap, self.inp["lru_conv_b"][slot:slot + 1, :].rearrange("o (j p) -> p (o j)", p=128), [], [convb.d()], slow=True)
        for tl, nm in ((ba, "lru_b_a"), (bx, "lru_b_x"), (c8, "lru_lam")):
            for d in range(2):
                self.dma("sp", tl.ap[:, d, :], self.inp[nm][slot, d:d + 1, :].rearrange("o (j p) -> p (o j)", p=128), [], [tl.d()], slow=True)
        self.act(c8.ap, c8.ap, AF.Exp, [c8.d()], [c8.d()], scale=-1.0)
        self.act(c8.ap, c8.ap, AF.Ln, [c8.d()], [c8.d()], bias=1.0)
        self.ts("dve", c8.ap, c8.ap, -8.0, None, ALU.mult, None, [c8.d()], [c8.d()])
        mf1 = AFa.mark()
        hxs = [AFa.alloc(D) for _ in range(2)]
        for t in range(NT):
            hx = hxs[t % 2]
            self.modulate(t, hx)
            self.transpose_tile(hx, hT, t, t)
        kb.barrier()
        AFa.release(mf1)
        raw = AFa.alloc(T_ALL)
        acc = AFa.alloc(T_ALL)
        gw = [(AFa.alloc(512), AFa.alloc(512)) for _ in range(2)]
        wt = [AB.alloc(8 * 128, [8, 128]) for _ in range(2)]
        gtile = [AB.alloc(T_ALL) for _ in range(2)]
        for o in range(2 * NJ):
            W = wt[o % 2]
            cols = o * 128 if o < NJ else LRU_W + (o - NJ) * 128
            self.dma("pool", W.ap, w_in[:, :, cols:cols + 128], [], [W.d()])
            gt = gtile[o % 2]
            for ci, (c0, cn) in enumerate(chunks):
                ps = self.nps()
                hdeps = hT.dl(range(c0 // 128, (c0 + cn) // 128))
                for kt in range(8):
                    self.mm(ps.ap[:, 0:cn], W.ap[:, kt, :], hT.ap[:, kt, c0:c0 + cn], kt == 0, kt == 7, [W.d()] + hdeps, [ps.d()])
                if o < NJ:
                    a_, b_ = gw[ci % 2]
                    self.gelu_tanh(gt.ap[:, c0:c0 + cn], ps.ap[:, 0:cn], [ps.d()], ((a_, a_.ap[:, 0:cn]), (b_, b_.ap[:, 0:cn])), [gt.d()])
                else:
                    self.cp(self.ev(), raw.ap[:, c0:c0 + cn], ps.ap[:, 0:cn], [ps.d()], [raw.d()])
            if o < NJ:
                self.dma("sp", gT[o], gt.ap, [gt.d()], [self.ddep("gT", o)])
            else:
                j = o - NJ
                rd_, ad_ = [raw.d()], [acc.d()]
                self.ts("dve", acc.ap, raw.ap, convw.ap[:, j, 1:2], convb.ap[:, j:j + 1], ALU.mult, ALU.add, rd_ + [convw.d(), convb.d()], ad_)
                for (s0, s1) in ((0, T_CTX), (T_CTX, T_ALL)):
                    self.stt(acc.ap[:, s0 + 1:s1], raw.ap[:, s0:s1 - 1], convw.ap[:, j, 0:1], acc.ap[:, s0 + 1:s1], ALU.mult, ALU.add, rd_ + ad_, ad_)
                    self.stt(acc.ap[:, s0:s1 - 1], raw.ap[:, s0 + 1:s1], convw.ap[:, j, 2:3], acc.ap[:, s0:s1 - 1], ALU.mult, ALU.add, rd_ + ad_, ad_)
                    self.stt(acc.ap[:, s0:s1 - 2], raw.ap[:, s0 + 2:s1], convw.ap[:, j, 3:4], acc.ap[:, s0:s1 - 2], ALU.mult, ALU.add, rd_ + ad_, ad_)
                self.cp("act", xsT.ap[:, j + 1, :], acc.ap, ad_, [xsT.d(j + 1)])
        kb.barrier()
        AB.release(mb_h)
        AFa.release(mf1)
        bands = {}
        for nm in ("lru_w_a", "lru_w_x"):
            for d in range(2):
                bt = AB.alloc(NJ * 3 * 128, [NJ, 3, 128])
                bands[(nm, d)] = bt
                self.memset("dve", bt.ap, 0.0, [bt.d()])
                wsrc = self.inp[nm]
                for n in range(16):
                    r0, r1 = 88 * n, 88 * n + 88
                    tl_ = list(range(r0 // 128, (r1 - 1) // 128 + 1))
                    for kt in tl_:
                        ra, rb = max(r0, kt * 128), min(r1, (kt + 1) * 128)
                        for j in tl_:
                            ca, cb = max(r0, j * 128), min(r1, (j + 1) * 128)
                            dst = bt.ap[ra - kt * 128:rb - kt * 128, j, kt - j + 1, ca - j * 128:cb - j * 128]
                            src = wsrc[slot, d, n, ra - r0:rb - r0, ca - r0:cb - r0]
                            self.dma("pool", dst, src, [], [bt.d()])
        gtile = [AB.alloc(T_ALL) for _ in range(2)]
        h0 = AFa.alloc(T_ALL)
        h1 = AFa.alloc(T_ALL)
        wk = [[AFa.alloc(512) for _ in range(4)] for _ in range(2)]
        lat_rev = [(256 + i * 512, 512) for i in (3, 2, 1, 0)]
        it = 0
        for j in range(NJ):
            gt = gtile[j % 2]
            self.dma("sp", gt.ap, gT[j], [self.ddep("gT", j)], [gt.d()])
            nb = [kt for kt in (j - 1, j, j + 1) if 0 <= kt < NJ]
            for d in range(2):
                hb = h0 if d == 0 else h1
                order = chunks if d == 0 else [(0, 256)] + lat_rev
                Ba, Bx = bands[("lru_w_a", d)], bands[("lru_w_x", d)]
                for (c0, cn) in order:
                    R, I, A, S = wk[it % 2]
                    it += 1
                    pa, px = self.nps(), self.nps()
                    xdeps = [xsT.d(kt + 1) for kt in nb]
                    for idx, kt in enumerate(nb):
                        self.mm(pa.ap[:, 0:cn], Ba.ap[:, j, kt - j + 1, :], xsT.ap[:, kt + 1, c0:c0 + cn], idx == 0, idx == len(nb) - 1, [Ba.d()] + xdeps, [pa.d()])
                    for idx, kt in enumerate(nb):
                        self.mm(px.ap[:, 0:cn], Bx.ap[:, j, kt - j + 1, :], xsT.ap[:, kt + 1, c0:c0 + cn], idx == 0, idx == len(nb) - 1, [Bx.d()] + xdeps, [px.d()])
                    Ra, Ia, Aa, Sa = R.ap[:, 0:cn], I.ap[:, 0:cn], A.ap[:, 0:cn], S.ap[:, 0:cn]
                    self.act(Ra, pa.ap[:, 0:cn], AF.Sigmoid, [pa.d(), ba.d()], [R.d()], bias=ba.ap[:, d, j:j + 1])
                    self.act(Ia, px.ap[:, 0:cn], AF.Sigmoid, [px.d(), bx.d()], [I.d()], bias=bx.ap[:, d, j:j + 1])
                    self.act(Aa, Ra, AF.Exp, [R.d(), c8.d()], [A.d()], scale=c8.ap[:, d, j:j + 1])
                    self.tt("dve", Sa, Aa, Aa, ALU.mult, [A.d()], [S.d()])
                    self.act(Sa, Sa, AF.Sqrt, [S.d()], [S.d()], scale=-1.0, bias=1.0)
                    self.tt("dve", Ia, Ia, xsT.ap[:, j + 1, c0:c0 + cn], ALU.mult, [I.d(), xsT.d(j + 1)], [I.d()])
                    self.tt("pool", Ia, Ia, Sa, ALU.mult, [I.d(), S.d()], [I.d()])
                    if d == 0:
                        init = 0.0 if c0 == 0 else hb.ap[:, c0 - 1:c0]
                        self.scan(hb.ap[:, c0:c0 + cn], Aa, Ia, init, [A.d(), I.d(), hb.d()], [hb.d()])
                    else:
                        if c0 == 0:
                            init = 0.0
                        elif c0 + cn == T_ALL:
                            init = hb.ap[:, 0:1]
                        else:
                            init = hb.ap[:, c0 + cn:c0 + cn + 1]
                        self.scan(hb.ap[:, c0:c0 + cn][:, ::-1], Aa[:, ::-1], Ia[:, ::-1], init, [A.d(), I.d(), hb.d()], [hb.d()])
            self.tt("dve", h0.ap, h0.ap, h1.ap, ALU.add, [h0.d(), h1.d()], [h0.d()])
            self.tt("dve", xsT.ap[:, j, :], h0.ap, gt.ap, ALU.mult, [h0.d(), gt.d()], [xsT.d(j)])
        kb.barrier()
        AFa.release(mf1)
        w_out = self.inp["lru_w_out"][slot].rearrange("(kt p) n -> p kt n", p=128)
        wo = AB.alloc(NJ * D, [NJ, D])
        for (k0, k1) in ((0, 4), (4, 8), (8, 11)):
            self.dma("pool", wo.ap[:, k0:k1, :], w_out[:, k0:k1, :], [], [wo.d()])
        pw = self.post_work()
        zdeps = [xsT.d(j) for j in range(NJ)]
        for t in range(NT):
            pys = []
            for c in range(2):
                py = self.nps()
                for kt in range(NJ):
                    self.mm(py.ap, xsT.ap[:, kt, t * 128:(t + 1) * 128], wo.ap[:, kt, c * 512:(c + 1) * 512], kt == 0, kt == NJ - 1, zdeps + [wo.d()], [py.d()])
                pys.append(py)
            self.post(t, [pys[0].ap, pys[1].ap], [pys[0].d(), pys[1].d()], pw[t % 2])
        kb.barrier()
        AB.release(mb)
        AFa.release(mf)

    def attention(self, l, slot, lam_init, last):
        kb = self.kb
        AB, AFa = self.AB, self.AF_
        kb.barrier()
        mb, mf = AB.mark(), AFa.mark()
        self.load_mod(l, 0)
        w_in = self.inp["attn_w_in"][slot].rearrange("(kt p) n -> p kt n", p=128)
        w_inp = self.inp["attn_w_in_p"][slot].rearrange("(kt p) n -> p kt n", p=128)
        w_out = self.inp["attn_w_out"][slot].rearrange("(kt p) n -> p kt n", p=128)
        hT = AB.alloc(8 * T_ALL, [8, T_ALL])
        oT = AB.alloc(8 * T_ALL, [8, T_ALL])
        mf0 = AFa.mark()
        hxs = [AFa.alloc(D) for _ in range(2)]
        for t in range(NT):
            hx = hxs[t % 2]
            self.modulate(t, hx)
            self.transpose_tile(hx, hT, t, t)
        kb.barrier()
        AFa.release(mf0)
        cosT = AFa.alloc(T_ALL)
        sinT = AFa.alloc(T_ALL)
        self.dma("sp", cosT.ap, self.inp["k_cos"], [], [cosT.d()])
        self.dma("sp", sinT.ap, self.inp["k_sin"], [], [sinT.d()])
        lamt = AFa.alloc(256 + 16)
        L = lamt.ap
        ld = [lamt.d()]
        self.dma("sp", L[:, 0:256], self.inp["attn_lam"][slot:slot + 1, :].partition_broadcast(128), [], ld)
        self.tt("dve", L[:, 0:64], L[:, 0:64], L[:, 64:128], ALU.mult, ld, ld)
        self.tt("dve", L[:, 128:192], L[:, 128:192], L[:, 192:256], ALU.mult, ld, ld)
        self.red("sum", L[:, 256:257], L[:, 0:64], ld, ld)
        self.red("sum", L[:, 257:258], L[:, 128:192], ld, ld)
        self.act(L[:, 258:260], L[:, 256:258], AF.Exp, ld, ld)
        self.tt("dve", L[:, 260:261], L[:, 258:259], L[:, 259:260], ALU.subtract, ld, ld)
        self.ts("dve", L[:, 261:262], L[:, 260:261], float(lam_init), -1.0, ALU.add, ALU.mult, ld, ld)
        neglam = L[:, 261:262]
        gsub = AFa.alloc(16)
        self.dma("sp", gsub.ap[:, 0:1], self.inp["attn_subln"][slot:slot + 1, :].rearrange("o e -> e o"), [], [gsub.d()], slow=True)
        self.ts("dve", gsub.ap[:, 0:1], gsub.ap[:, 0:1], float(1.0 - lam_init), None, ALU.mult, None, [gsub.d()], [gsub.d()])
        chunks = [(0, 256)] + [(256 + i * 512, 512) for i in range(4)]
        qk = [AB.alloc(T_ALL) for _ in range(4)]
        vext = AB.alloc(NT * 2 * 130, [NT, 2, 130])
        mbw = AB.mark()
        wqk = AB.alloc(8 * 512, [8, 4, 128])
        wqkp = AB.alloc(8 * 512, [8, 4, 128])
        wv = AB.alloc(8 * 256, [8, 256])
        ework = [AB.alloc(512) for _ in range(3)]
        ta = [AFa.alloc(512) for _ in range(2)]
        tb = [AFa.alloc(512) for _ in range(2)]
        fw = AFa.alloc(5 * 512)
        qchunks = chunks[1:] if last else chunks
        self.ps_n = 3
        self.memset("dve", vext.ap, 1.0, [vext.d()])
        qtiles = list(range(2, NT)) if last else list(range(NT))
        it_e = 0
        it_f = 0
        for hp in range(4):
            for j in range(4):
                m, isk = j % 2, j // 2
                c0 = isk * 1024 + m * 512 + hp * 128
                self.dma("pool", wqk.ap[:, :, j, :], w_in[:, :, c0:c0 + 128], [], [wqk.d()])
                self.dma("pool", wqkp.ap[:, :, j, :], w_inp[:, :, c0:c0 + 128], [], [wqkp.d()])
            self.dma("pool", wv.ap, w_in[:, :, 2048 + hp * 256:2048 + (hp + 1) * 256], [], [wv.d()])
            for j in range(4):
                for ci, (c0, cn) in enumerate(chunks):
                    pa, pb_ = self.nps(), self.nps()
                    hdeps = hT.dl(range(c0 // 128, (c0 + cn) // 128))
                    for kt in range(8):
                        self.mm(pa.ap[:, 0:cn], wqk.ap[:, kt, j, :], hT.ap[:, kt, c0:c0 + cn], kt == 0, kt == 7, [wqk.d()] + hdeps, [pa.d()])
                    for kt in range(8):
                        self.mm(pb_.ap[:, 0:cn], wqkp.ap[:, kt, j, :], hT.ap[:, kt, c0:c0 + cn], kt == 0, kt == 7, [wqkp.d()] + hdeps, [pb_.d()])
                    a_, b_ = ta[ci % 2], tb[ci % 2]
                    self.tt("dve", a_.ap[:, 0:cn], pa.ap[:, 0:cn], cosT.ap[:, c0:c0 + cn], ALU.mult, [pa.d(), cosT.d()], [a_.d()])
                    self.tt("dve", b_.ap[:, 0:cn], pb_.ap[:, 0:cn], sinT.ap[:, c0:c0 + cn], ALU.mult, [pb_.d(), sinT.d()], [b_.d()])
                    self.tt("pool", qk[j].ap[:, c0:c0 + cn], a_.ap[:, 0:cn], b_.ap[:, 0:cn], ALU.add, [a_.d(), b_.d()], [qk[j].d(ci)])
            for t in range(NT):
                pv = self.nps()
                for kt in range(8):
                    self.mm(pv.ap[:, 0:256], hT.ap[:, kt, t * 128:(t + 1) * 128], wv.ap[:, kt, :], kt == 0, kt == 7, [hT.d(t), wv.d()], [pv.d()])
                self.cp(self.ev(), vext.ap[:, t, :, 0:128], pv.ap[:, 0:256].rearrange("p (h e) -> p h e", h=2), [pv.d()], [vext.d()])
            qkd = [qk[j].dl(range(5)) for j in range(4)]
            items = []
            for hh in range(2):
                for (c0, cn) in qchunks:
                    kts = [0, 1] if c0 == 0 else list(range(NT))
                    for m in range(2):
                        for i, kt in enumerate(kts):
                            items.append((hh, c0, cn, m, kt, i == 0, i == len(kts) - 1, m == 1 and i == len(kts) - 1))

            def finalize(hh, c0, cn):
                head = hp * 2 + hh
                n0, d0, n1, d1 = self.ps[3], self.ps[4], self.ps[5], self.ps[6]
                wdp = [fw.d()]
                r1, r2, t1, o, sqv = (fw.ap[:, i * 512:i * 512 + cn] for i in range(5))
                self.recip(r1, d0.ap[:, 0:cn], [d0.d()], wdp)
                self.recip(r2, d1.ap[:, 0:cn], [d1.d()], wdp)
                self.ts("dve", r2, r2, neglam, None, ALU.mult, None, wdp + ld, wdp)
                self.tt("dve", t1, n0.ap[:, 0:cn], r1, ALU.mult, [n0.d()] + wdp, wdp)
                self.tt("dve", r2, n1.ap[:, 0:cn], r2, ALU.mult, [n1.d()] + wdp, wdp)
                self.tt("dve", o, r2, t1, ALU.add, wdp, wdp)
                self.tt("pool", sqv, o, o, ALU.mult, wdp, wdp)
                pss = self.nps()
                self.mm(pss.ap[:, 0:cn], self.ones_f.ap, sqv, True, True, wdp + [self.ones_f.d()], [pss.d()])
                self.ts("dve", r1, pss.ap[:, 0:cn], 1.0 / 128.0, float(LN_EPS), ALU.mult, ALU.add, [pss.d()] + wdp, wdp)
                self.act(r1, r1, AF.Sqrt, wdp, wdp)
                self.recip(r1, r1, wdp, wdp)
                self.stt(oT.ap[:, head, c0:c0 + cn], o, gsub.ap[:, 0:1], r1, ALU.mult, ALU.mult, wdp + [gsub.d()],
                         [oT.d(t) for t in range(c0 // 128, (c0 + cn) // 128)])

            def do_pv(item, E):
                hh, c0, cn, m, kt, first, lastk, unit_end = item
                num, den = self.ps[3 + 2 * m], self.ps[4 + 2 * m]
                self.mm(num.ap[:, 0:cn], vext.ap[:, kt, hh, 0:128], E.ap[:, 0:cn], first, lastk, [E.d(), vext.d()], [num.d()])
                self.mm(den.ap[:, 0:cn], self.ones_b.ap, E.ap[:, 0:cn], first, lastk, [E.d(), self.ones_b.d()], [den.d()])
                if unit_end:
                    finalize(hh, c0, cn)

            pend = []
            for item in items:
                hh, c0, cn, m, kt = item[0:5]
                prow = slice(hh * 64, (hh + 1) * 64)
                Q, K = qk[m], qk[2 + m]
                S = self.nps()
                self.mm(S.ap[:, 0:cn], K.ap[prow, kt * 128:(kt + 1) * 128], Q.ap[prow, c0:c0 + cn], True, True,
                        qkd[m] + qkd[2 + m], [S.d()])
                E = ework[it_e % 3]
                it_e += 1
                self.act(E.ap[:, 0:cn], S.ap[:, 0:cn], AF.Exp, [S.d()], [E.d()], scale=0.125)
                pend.append((item, E))
                if len(pend) > 2:
                    do_pv(*pend.pop(0))
            while pend:
                do_pv(*pend.pop(0))
        kb.barrier()
        self.ps_n = 7
        AB.release(mbw)
        AFa.release(mf0)
        wo = AB.alloc(8 * D, [8, D])
        for hh in range(2):
            self.dma("pool", wo.ap[:, hh * 4:(hh + 1) * 4, :], w_out[:, hh * 4:(hh + 1) * 4, :], [], [wo.d()])
        pw = self.post_work()
        for it, t in enumerate(qtiles):
            pys = []
            for c in range(2):
                py = self.nps()
                for h in range(8):
                    self.mm(py.ap, oT.ap[:, h, t * 128:(t + 1) * 128], wo.ap[:, h, c * 512:(c + 1) * 512], h == 0, h == 7, [oT.d(t), wo.d()], [py.d()])
                pys.append(py)
            self.post(t, [pys[0].ap, pys[1].ap], [pys[0].d(), pys[1].d()], pw[it % 2])
        kb.barrier()
        AB.release(mb)
        AFa.release(mf)


def _consts():
    ident = np.eye(128, dtype=np.float32)
    ltri = (np.arange(128)[:, None] < np.arange(128)[None, :]).astype(np.float32)
    eoff = np.broadcast_to((np.arange(N_EXP) * C_CAP).astype(np.float32)[None, :], (128, N_EXP)).copy()
    half = 16
    freq = (10000.0 ** (-np.arange(half, dtype=np.float32) / half)).astype(np.float32)
    tpos = np.arange(T_LAT)
    row = (tpos // 64).astype(np.float32)
    col = (tpos % 64).astype(np.float32)
    cos = np.ones((128, T_ALL), np.float32)
    sin = np.zeros((128, T_ALL), np.float32)
    for p in range(128):
        d = p % 64
        pos = row if d < 32 else col
        dd = d % 32
        ang = (pos * freq[dd % 16]).astype(np.float32)
        cos[p, T_CTX:] = np.cos(ang)
        s = np.sin(ang)
        sin[p, T_CTX:] = -s if dd < 16 else s
    tau = np.broadcast_to(np.arange(1, 33, dtype=np.float32)[None, :], (128, 32)).copy()
    return dict(k_ident=ident, k_ltri=ltri, k_eoff=eoff, k_cos=cos, k_sin=sin, k_tau=tau)


def _perm_qk(w_in):
    qk = w_in[:, :, :2 * D]
    s = qk.shape
    v = qk.reshape(s[0], s[1], -1, 2, 16)
    return np.ascontiguousarray(v[:, :, :, ::-1, :].reshape(s))


_CACHE = {}


def _host_inputs(inputs):
    f = lambda a: np.ascontiguousarray(np.asarray(a, dtype=np.float32))
    shared = dict(
        w_ada=f(inputs["w_ada"]), b_ada=f(inputs["b_ada"]), ln_g=f(inputs["ln_g"]), ln_b=f(inputs["ln_b"]),
        attn_w_in=f(inputs["attn_w_in"]), attn_w_in_p=_perm_qk(f(inputs["attn_w_in"])), attn_w_out=f(inputs["attn_w_out"]),
        attn_lam=f(inputs["attn_lam"]).reshape(2, 256), attn_subln=f(inputs["attn_subln"]),
        router_w=f(inputs["router_w"]), router_b=f(inputs["router_b"]).reshape(1, N_EXP),
        moe_w_gate=f(inputs["moe_w_gate"]), moe_w_up=f(inputs["moe_w_up"]), moe_w_down=f(inputs["moe_w_down"]),
    )
    for nm in ("s5_w_in", "s5_a_re", "s5_a_im", "s5_b_re", "s5_b_im", "s5_c_re", "s5_c_im", "s5_log_dt", "s5_d", "s5_glu_v",
               "s5_glu_g", "lru_w_in", "lru_conv_w", "lru_conv_b", "lru_w_a", "lru_b_a", "lru_w_x", "lru_b_x", "lru_lam", "lru_w_out"):
        shared[nm] = f(inputs[nm])
    shared.update(_consts())
    return shared


def kernel(**inputs):
    shared = _host_inputs(inputs)
    x = np.asarray(inputs["x"], np.float32)
    ctx = np.asarray(inputs["ctx"], np.float32)
    c = np.asarray(inputs["c"], np.float32)
    c_ctx = np.asarray(inputs["c_ctx"], np.float32)
    prog = Prog()
    nc = prog.build()
    in_maps = []
    for b in range(NCORES):
        m = dict(shared)
        m["x"] = np.ascontiguousarray(x[b])
        m["ctx"] = np.ascontiguousarray(ctx[b])
        m["cc"] = np.ascontiguousarray(np.stack([c[b], c_ctx]))
        in_maps.append(m)
    res = run_bass_kernel_spmd(nc, in_maps, core_ids=list(range(NCORES)))
    return np.stack([np.asarray(r["out"], np.float32) for r in res.results])
```

```python
import contextlib
import math
import numpy as np
import concourse.bass as bass
import concourse.mybir as mybir
from concourse.bass_utils import run_bass_kernel_spmd

F32 = mybir.dt.float32
BF16 = mybir.dt.bfloat16
I32 = mybir.dt.int32
ALU = mybir.AluOpType
AF = mybir.ActivationFunctionType
AX = mybir.AxisListType

D = 1024
T_LAT = 2048
T_CTX = 256
T_ALL = T_LAT + T_CTX
NT = T_ALL // 128
DEPTH = 4
DN_ALPHA = (2 * DEPTH) ** 0.25
LN_EPS = 1e-5
N_EXP = 16
C_CAP = 1024
LRU_W = 1408
NCORES = 8


class Dep:
    __slots__ = ("w", "r")

    def __init__(self):
        self.w = None
        self.r = {}


class KB:
    EPOCH = 20000
    NPOOL = 8

    def __init__(self, nc):
        self.nc = nc
        self.stack = contextlib.ExitStack()
        self.ops = {e: [] for e in ("sp", "pool", "act", "dve", "pe")}
        self.cnt = {e: 0 for e in self.ops}
        self.esems = {e: [] for e in self.ops}
        self.known = {e: {} for e in self.ops}
        self.dma_sems = {}
        self.dma_cnt = {}
        self.dma_i = {e: 0 for e in self.ops}
        self.last_tok = {}
        self.nsem = 0
        self.ntens = 0
        self.out_tokens = []

    def sem(self, name):
        self.nsem += 1
        return self.stack.enter_context(self.nc.semaphore(f"{name}_{self.nsem}"))

    def sbuf(self, shape, dtype, name="t"):
        self.ntens += 1
        return self.stack.enter_context(self.nc.sbuf_tensor(f"{name}_{self.ntens}", list(shape), dtype))

    def psum(self, shape, dtype, name="ps"):
        self.ntens += 1
        return self.stack.enter_context(self.nc.psum_tensor(f"{name}_{self.ntens}", list(shape), dtype))

    def _need(self, eng, tok, waits):
        sem, val, _ = tok
        k = self.known[eng]
        key = id(sem)
        if k.get(key, (None, 0))[1] >= val:
            return
        k[key] = (sem, val)
        for i, (s, v) in enumerate(waits):
            if s is sem:
                waits[i] = (s, max(v, val))
                return
        waits.append((sem, val))

    def _collect(self, eng, reads, writes):
        waits = []
        for d in reads:
            if d.w is not None:
                if d.w[2] == eng and eng == "pe":
                    continue
                self._need(eng, d.w, waits)
        for d in writes:
            if d.w is not None and d.w[2] != eng:
                self._need(eng, d.w, waits)
            for t in d.r.values():
                if t[2] != eng:
                    self._need(eng, t, waits)
        return waits

    def _commit(self, tok, reads, writes):
        for d in reads:
            d.r[id(tok[0])] = tok
        for d in writes:
            d.w = tok
            d.r = {}
        self.last_tok[id(tok[0])] = tok

    def op(self, eng, fn, reads=(), writes=()):
        waits = self._collect(eng, reads, writes)
        c = self.cnt[eng]
        ep = c // self.EPOCH
        while len(self.esems[eng]) <= ep:
            self.esems[eng].append(self.sem(f"e_{eng}"))
        sem = self.esems[eng][ep]
        val = c % self.EPOCH + 1
        self.cnt[eng] = c + 1
        tok = (sem, val, eng)
        self.ops[eng].append((waits, fn, sem, 1))
        self._commit(tok, reads, writes)
        return tok

    def dma(self, q, fn, reads=(), writes=(), is_output=False):
        waits = self._collect(q, reads, writes)
        if q not in self.dma_sems:
            self.dma_sems[q] = [self.sem(f"d_{q}") for _ in range(self.NPOOL)]
            self.dma_cnt[q] = [0] * self.NPOOL
        i = self.dma_i[q] % self.NPOOL
        self.dma_i[q] += 1
        sem = self.dma_sems[q][i]
        prev = self.dma_cnt[q][i]
        if prev > 0:
            self._need(q, (sem, prev, "dma"), waits)
        self.dma_cnt[q][i] = prev + 16
        tok = (sem, prev + 16, "dma")
        self.ops[q].append((waits, fn, sem, 16))
        self._commit(tok, reads, writes)
        if is_output:
            self.out_tokens.append(tok)
        return tok

    def barrier(self):
        toks = list(self.last_tok.values())
        for eng in self.ops:
            waits = []
            for t in toks:
                self._need(eng, t, waits)
            if waits:
                self.ops[eng].append((waits, None, None, 0))

    def finish(self):
        waits = []
        for t in self.out_tokens:
            self._need("sp", t, waits)
        if waits:
            self.ops["sp"].append((waits, None, None, 0))
        nc = self.nc
        with nc.Block() as block:
            for name, deco in (("sp", block.sync), ("pool", block.gpsimd), ("act", block.scalar),
                               ("dve", block.vector), ("pe", block.tensor)):
                ops = self.ops[name]

                def body(e, ops=ops):
                    for waits, fn, sem, inc in ops:
                        for s, v in waits:
                            e.wait_ge(s, v)
                        if fn is None:
                            continue
                        ins = fn(e)
                        ins.then_inc(sem, inc)

                deco(body)
        self.stack.close()


class Tl:
    def __init__(self, ap):
        self.ap = ap
        self.ds = {}

    def d(self, key=0):
        if key not in self.ds:
            self.ds[key] = Dep()
        return self.ds[key]

    def dl(self, keys):
        return [self.d(k) for k in keys]


class Arena:
    def __init__(self, tens, n):
        self.t = tens
        self.n = n
        self.off = 0

    def alloc(self, n, shape=None):
        n2 = (n + 15) // 16 * 16
        assert self.off + n2 <= self.n, (self.off, n2, self.n)
        ap = self.t[:, self.off:self.off + n]
        self.off += n2
        if shape is not None:
            names = " ".join(f"a{i}" for i in range(len(shape)))
            kw = {f"a{i}": s for i, s in enumerate(shape)}
            ap = ap.rearrange(f"p ({names}) -> p {names}", **kw)
        return Tl(ap)

    def mark(self):
        return self.off

    def release(self, m):
        self.off = m


class Prog:
    def __init__(self, debug=None, layers=None):
        self.debug = debug or []
        self.layers = list(range(DEPTH)) if layers is None else list(layers)
        nc = bass.Bass("TRN2", target_bir_lowering=False)
        self.nc = nc
        self.kb = KB(nc)
        kb = self.kb
        self.inp = {}
        self.dr = {}
        self.dd = {}
        NB, NF = 64000, 18500
        self.AB = Arena(kb.sbuf([128, NB], BF16, "arb"), NB)
        self.AF_ = Arena(kb.sbuf([128, NF], F32, "arf"), NF)
        self.AI = Arena(kb.sbuf([128, 1280], I32, "ari"), 1280)
        self.ps = [Tl(kb.psum([128, 512], F32, f"ps{i}")[:]) for i in range(7)]
        self.psb = [Tl(kb.psum([128, 1024], BF16, f"psb{i}")[:]) for i in range(1)]
        self.ps_n = 7
        self.ps_i = 0
        self.psb_i = 0
        self.ev_i = 0

    def din(self, name, shape, dtype=F32):
        self.inp[name] = self.nc.dram_tensor(name, list(shape), dtype, kind="ExternalInput").ap()
        return self.inp[name]

    def dscr(self, name, shape, dtype=F32):
        kind = "ExternalOutput" if name in self.debug else "Internal"
        self.dr[name] = self.nc.dram_tensor(name, list(shape), dtype, kind=kind).ap()
        self.dd[name] = {}
        return self.dr[name]

    def ddep(self, name, key=0):
        dd = self.dd.setdefault(name, {})
        if key not in dd:
            dd[key] = Dep()
        return dd[key]

    def bound_reg(self, e):
        if getattr(self, "_breg", None) is None:
            self._breg = e.to_reg(N_EXP * C_CAP - 1)
        return self._breg

    def nps(self):
        p = self.ps[self.ps_i % self.ps_n]
        self.ps_i += 1
        return p

    def npsb(self):
        p = self.psb[self.psb_i % len(self.psb)]
        self.psb_i += 1
        return p

    def ev(self):
        self.ev_i += 1
        return "act" if self.ev_i % 2 else "dve"

    def tt(self, eng, out, in0, in1, op, r, w):
        self.kb.op(eng, lambda e: e.tensor_tensor(out=out, in0=in0, in1=in1, op=op), r, w)

    def ts(self, eng, out, in0, s1, s2, op0, op1, r, w):
        if s2 is None:
            self.kb.op(eng, lambda e: e.tensor_scalar(out=out, in0=in0, scalar1=s1, scalar2=None, op0=op0), r, w)
        else:
            self.kb.op(eng, lambda e: e.tensor_scalar(out=out, in0=in0, scalar1=s1, scalar2=s2, op0=op0, op1=op1), r, w)

    def stt(self, out, in0, sc, in1, op0, op1, r, w):
        self.kb.op("dve", lambda e: e.scalar_tensor_tensor(out=out, in0=in0, scalar=sc, in1=in1, op0=op0, op1=op1), r, w)

    def act(self, out, in_, func, r, w, bias=None, scale=None, accum=None):
        kw = {}
        if bias is not None:
            kw["bias"] = bias
        if scale is not None:
            kw["scale"] = scale
        if accum is not None:
            kw["accum_out"] = accum
        self.kb.op("act", lambda e: e.activation(out=out, in_=in_, func=func, **kw), r, w)

    def red(self, kind, out, in_, r, w):
        if kind == "max":
            self.kb.op("dve", lambda e: e.reduce_max(out=out, in_=in_, axis=AX.X), r, w)
        else:
            self.kb.op("dve", lambda e: e.reduce_sum(out=out, in_=in_, axis=AX.X), r, w)

    def recip(self, out, in_, r, w):
        self.kb.op("dve", lambda e: e.reciprocal(out=out, in_=in_), r, w)

    def scan(self, out, d0, d1, init, r, w):
        self.kb.op("dve", lambda e: e.tensor_tensor_scan(out=out, data0=d0, data1=d1, initial=init, op0=ALU.mult, op1=ALU.add), r, w)

    def gelu_tanh(self, out, src, srcdeps, wk, outdeps):
        (xs, xsap), (t, tap) = wk
        self.cp("act", xsap, src, srcdeps, [xs.d()])
        self.tt("dve", tap, xsap, xsap, ALU.mult, [xs.d()], [t.d()])
        self.ts("dve", tap, tap, 0.044715, 1.0, ALU.mult, ALU.add, [t.d()], [t.d()])
        self.tt("dve", tap, tap, xsap, ALU.mult, [t.d(), xs.d()], [t.d()])
        self.act(tap, tap, AF.Sigmoid, [t.d()], [t.d()], scale=1.5957691216057308)
        self.tt("pool", out, xsap, tap, ALU.mult, [xs.d(), t.d()], outdeps)

    def sincos(self, ang, cos_out, sin_out, tmpf, tmpi, r, w):
        TWO_PI = 2.0 * math.pi
        for out, shift in ((sin_out, 0.0), (cos_out, 0.25)):
            self.ts("dve", tmpf, ang, 1.0 / TWO_PI, shift, ALU.mult, ALU.add, r, w)
            self.cp("dve", tmpi, tmpf, w, w)
            self.cp("dve", out, tmpi, w, w)
            self.tt("dve", tmpf, tmpf, out, ALU.subtract, w, w)
            self.ts("dve", out, tmpf, 0.5, None, ALU.is_gt, None, w, w)
            self.tt("dve", tmpf, tmpf, out, ALU.subtract, w, w)
            self.ts("dve", out, tmpf, -0.5, None, ALU.is_lt, None, w, w)
            self.tt("dve", tmpf, tmpf, out, ALU.add, w, w)
            self.act(out, tmpf, AF.Sin, w, w, scale=TWO_PI * (1.0 - 1e-6))

    def cp(self, eng, out, in_, r, w):
        if eng == "act":
            self.kb.op("act", lambda e: e.activation(out=out, in_=in_, func=AF.Copy), r, w)
        else:
            self.kb.op(eng, lambda e: e.tensor_copy(out=out, in_=in_), r, w)

    def mm(self, out, lhsT, rhs, start, stop, r, w):
        self.kb.op("pe", lambda e: e.matmul(out, lhsT=lhsT, rhs=rhs, start=start, stop=stop), r, w)

    def tr(self, out, in_, ident, r, w):
        self.kb.op("pe", lambda e: e.transpose(out, in_, ident), r, w)

    def dma(self, q, out, in_, r, w, is_output=False, slow=False):
        if slow:
            self.kb.dma(q, lambda e: e.dma_start(out=out, in_=in_, allow_slow_non_contiguous=True), r, w, is_output=is_output)
        else:
            self.kb.dma(q, lambda e: e.dma_start(out=out, in_=in_), r, w, is_output=is_output)

    def memset(self, eng, ap, val, w):
        self.kb.op(eng, lambda e: e.memset(ap, val), (), w)

    def build(self):
        kb = self.kb
        din, dscr = self.din, self.dscr
        x_in = din("x", [T_LAT, D])
        ctx_in = din("ctx", [T_CTX, D])
        cc_in = din("cc", [2, D])
        w_ada = din("w_ada", [DEPTH, D, 6 * D])
        b_ada = din("b_ada", [DEPTH, 6 * D])
        ln_g = din("ln_g", [DEPTH, 2, D])
        ln_b = din("ln_b", [DEPTH, 2, D])
        din("attn_w_in", [2, D, 3 * D])
        din("attn_w_in_p", [2, D, 2 * D])
        din("attn_w_out", [2, D, D])
        din("attn_lam", [2, 256])
        din("attn_subln", [2, 128])
        din("router_w", [D, N_EXP])
        din("router_b", [1, N_EXP])
        din("moe_w_gate", [DEPTH, N_EXP, D, D])
        din("moe_w_up", [DEPTH, N_EXP, D, D])
        din("moe_w_down", [DEPTH, N_EXP, D, D])
        din("s5_w_in", [1, D, D])
        for nm in ("s5_a_re", "s5_a_im"):
            din(nm, [1, 2, 64, 64])
        for nm in ("s5_b_re", "s5_b_im"):
            din(nm, [1, 2, 64, 64, 16])
        for nm in ("s5_c_re", "s5_c_im"):
            din(nm, [1, 2, 64, 16, 64])
        din("s5_log_dt", [1, 2, 64])
        din("s5_d", [1, D])
        din("s5_glu_v", [1, D, D])
        din("s5_glu_g", [1, D, D])
        din("lru_w_in", [1, D, 2 * LRU_W])
        din("lru_conv_w", [1, 4, LRU_W])
        din("lru_conv_b", [1, LRU_W])
        din("lru_w_a", [1, 2, 16, 88, 88])
        din("lru_b_a", [1, 2, LRU_W])
        din("lru_w_x", [1, 2, 16, 88, 88])
        din("lru_b_x", [1, 2, LRU_W])
        din("lru_lam", [1, 2, LRU_W])
        din("lru_w_out", [1, LRU_W, D])
        din("k_tau", [128, 32])
        din("k_ident", [128, 128])
        din("k_ltri", [128, 128])
        din("k_eoff", [128, N_EXP])
        din("k_cos", [128, T_ALL])
        din("k_sin", [128, T_ALL])
        out = self.nc.dram_tensor("out", [T_LAT, D], F32, kind="ExternalOutput").ap()
        xs = dscr("xs", [T_ALL, D])
        mod = dscr("mod", [DEPTH, 2, 6 * D])
        dscr("Xg", [N_EXP * C_CAP, D], BF16)
        dscr("Yg", [N_EXP * C_CAP, D], F32)
        dscr("gT", [11, 128, T_ALL], BF16)

        AB, AFa = self.AB, self.AF_
        self.ident_f = AFa.alloc(128)
        self.ident_b = AB.alloc(128)
        self.ltri_b = AB.alloc(128)
        self.ones_b = AB.alloc(128)
        self.eoff = AFa.alloc(N_EXP)
        self.rb_bc = AFa.alloc(N_EXP)
        self.rw = AFa.alloc(8 * N_EXP, [8, N_EXP])
        self.dma("sp", self.ident_f.ap, self.inp["k_ident"], [], [self.ident_f.d()])
        self.cp("dve", self.ident_b.ap, self.ident_f.ap, [self.ident_f.d()], [self.ident_b.d()])
        tmpf = AFa.alloc(128)
        self.dma("sp", tmpf.ap, self.inp["k_ltri"], [], [tmpf.d()])
        self.cp("dve", self.ltri_b.ap, tmpf.ap, [tmpf.d()], [self.ltri_b.d()])
        self.memset("dve", self.ones_b.ap, 1.0, [self.ones_b.d()])
        self.ones_f = AFa.alloc(128)
        self.memset("dve", self.ones_f.ap, 1.0, [self.ones_f.d()])
        self.dma("sp", self.eoff.ap, self.inp["k_eoff"], [], [self.eoff.d()])
        self.dma("sp", self.rb_bc.ap, self.inp["router_b"].partition_broadcast(128), [], [self.rb_bc.d()])
        self.dma("sp", self.rw.ap, self.inp["router_w"].rearrange("(kt p) e -> p kt e", p=128), [], [self.rw.d()])
        self.dest_i = Tl(self.AI.t[:, 0:2 * NT].rearrange("p (t k) -> p t k", k=2))
        self.dest_g = Tl(self.AI.t[:, 64:64 + 2 * NT].rearrange("p (t k) -> p t k", k=2))
        self.itmp = Tl(self.AI.t[:, 256:1280])
        self.gatew = AFa.alloc(2 * NT, [NT, 2])
        self.bc = {nm: AFa.alloc(D) for nm in ("sh_l", "sc_l", "g_l", "sh_c", "sc_c", "g_c", "lng", "lnb")}
        self.base_mark_b = AB.mark()
        self.base_mark_f = AFa.mark()

        self.dma("sp", xs[0:T_CTX, :], ctx_in, [], [self.ddep("xs", t) for t in range(2)])
        self.dma("sp", xs[T_CTX:, :], x_in, [], [self.ddep("xs", t) for t in range(2, NT)])

        self.adaln(cc_in, w_ada, b_ada, mod)

        for l in self.layers:
            kind, slot, last = l % 3, l // 3, l == DEPTH - 1
            tiles = list(range(2, NT)) if last else list(range(NT))
            if kind == 0:
                lam_init = 0.8 - 0.6 * math.exp(-0.3 * l)
                self.attention(l, slot, lam_init, last)
            elif kind == 1:
                self.s5(l, slot)
            else:
                self.rglru(l, slot)
            self.moe(l, tiles)

        kb.barrier()
        self.dma("sp", out, xs[T_CTX:, :], [self.ddep("xs", t) for t in range(2, NT)], [], is_output=True)
        kb.finish()
        return self.nc

    def adaln(self, cc_in, w_ada, b_ada, mod):
        AFa = self.AF_
        m = AFa.mark()
        craw = AFa.alloc(16, [8, 2])
        sT = AFa.alloc(16, [8, 2])
        for r in range(2):
            self.dma("sp", craw.ap[:, :, r], cc_in[r:r + 1, :].rearrange("o (kt p) -> p (o kt)", p=128), [], [craw.d()], slow=True)
        self.act(sT.ap, craw.ap, AF.Silu, [craw.d()], [sT.d()])
        bbs = [AFa.alloc(512) for _ in range(2)]
        obs = [AFa.alloc(512) for _ in range(2)]
        wbuf = [AFa.alloc(8 * 256, [8, 256]) for _ in range(2)]
        i = 0
        for l in range(DEPTH):
            for n in range(24):
                wb, bb, ob = wbuf[i % 2], bbs[i % 2], obs[i % 2]
                i += 1
                cs = slice(n * 256, (n + 1) * 256)
                self.dma("sp", bb.ap[0:2, 0:256], b_ada[l:l + 1, cs].partition_broadcast(2), [], [bb.d()])
                self.dma("sp", wb.ap, w_ada[l].rearrange("(kt p) n -> p kt n", p=128)[:, :, cs], [], [wb.d()])
                ps = self.nps()
                for kt in range(8):
                    self.mm(ps.ap[0:2, 0:256], sT.ap[:, kt, :], wb.ap[:, kt, :], kt == 0, kt == 7, [sT.d(), wb.d()], [ps.d()])
                self.tt("dve", ob.ap[0:2, 0:256], ps.ap[0:2, 0:256], bb.ap[0:2, 0:256], ALU.add, [ps.d(), bb.d()], [ob.d()])
                self.dma("sp", mod[l, :, cs], ob.ap[0:2, 0:256], [ob.d()], [self.ddep("mod", l)])
        self.kb.barrier()
        AFa.release(m)

    def load_mod(self, l, sub):
        mod = self.dr["mod"]
        for r, sfx in ((0, "l"), (1, "c")):
            for j, nm in enumerate(("sh", "sc", "g")):
                t = self.bc[f"{nm}_{sfx}"]
                col = (3 * sub + j) * D
                self.dma("sp", t.ap, mod[l, r:r + 1, col:col + D].partition_broadcast(128), [self.ddep("mod", l)], [t.d()])
            t = self.bc[f"sc_{sfx}"]
            self.ts("dve", t.ap, t.ap, 1.0, None, ALU.add, None, [t.d()], [t.d()])
        self.dma("sp", self.bc["lng"].ap, self.inp["ln_g"][l, sub:sub + 1, :].partition_broadcast(128), [], [self.bc["lng"].d()])
        self.dma("sp", self.bc["lnb"].ap, self.inp["ln_b"][l, sub:sub + 1, :].partition_broadcast(128), [], [self.bc["lnb"].d()])

    def modulate(self, t, hx):
        xs = self.dr["xs"]
        sfx = "c" if t < 2 else "l"
        self.dma("sp", hx.ap, xs[t * 128:(t + 1) * 128, :], [self.ddep("xs", t)], [hx.d()])
        sc, sh = self.bc[f"sc_{sfx}"], self.bc[f"sh_{sfx}"]
        self.tt("dve", hx.ap, hx.ap, sc.ap, ALU.mult, [hx.d(), sc.d()], [hx.d()])
        self.tt("pool", hx.ap, hx.ap, sh.ap, ALU.add, [hx.d(), sh.d()], [hx.d()])

    def post(self, t, ysrc, ydeps, work):
        xs = self.dr["xs"]
        sfx = "c" if t < 2 else "l"
        g = self.bc[f"g_{sfx}"]
        xr, t1, st, mv = work
        self.dma("sp", xr.ap, xs[t * 128:(t + 1) * 128, :], [self.ddep("xs", t)], [xr.d()])
        for h in range(2):
            sl = slice(h * 512, (h + 1) * 512)
            self.tt("dve", t1.ap[:, sl], ysrc[h], g.ap[:, sl], ALU.mult, ydeps + [g.d()], [t1.d()])
        self.stt(t1.ap, xr.ap, float(DN_ALPHA), t1.ap, ALU.mult, ALU.add, [xr.d(), t1.d()], [t1.d()])
        for h in range(2):
            self.kb.op("dve", lambda e, h=h: e.bn_stats(out=st.ap[:, h * 6:(h + 1) * 6], in_=t1.ap[:, h * 512:(h + 1) * 512]),
                       [t1.d()], [st.d()])
        self.kb.op("dve", lambda e: e.bn_aggr(out=mv.ap[:, 0:2], in_=st.ap[:, 0:12]), [st.d()], [mv.d()])
        self.ts("dve", mv.ap[:, 2:3], mv.ap[:, 1:2], float(LN_EPS), None, ALU.add, None, [mv.d()], [mv.d()])
        self.act(mv.ap[:, 3:4], mv.ap[:, 2:3], AF.Sqrt, [mv.d()], [mv.d()])
        self.kb.op("dve", lambda e: e.reciprocal(out=mv.ap[:, 4:5], in_=mv.ap[:, 3:4]), [mv.d()], [mv.d()])
        self.ts("dve", t1.ap, t1.ap, mv.ap[:, 0:1], mv.ap[:, 4:5], ALU.subtract, ALU.mult, [t1.d(), mv.d()], [t1.d()])
        self.tt("pool", t1.ap, t1.ap, self.bc["lng"].ap, ALU.mult, [t1.d(), self.bc["lng"].d()], [t1.d()])
        self.tt("dve", t1.ap, t1.ap, self.bc["lnb"].ap, ALU.add, [t1.d(), self.bc["lnb"].d()], [t1.d()])
        self.dma("sp", xs[t * 128:(t + 1) * 128, :], t1.ap, [t1.d()], [self.ddep("xs", t)])

    def post_work(self):
        AFa = self.AF_
        return [(AFa.alloc(D), AFa.alloc(D), AFa.alloc(12), AFa.alloc(8)) for _ in range(2)]

    def transpose_tile(self, hx, dst_b, dst_key, t, dst_f=None):
        for half in range(2):
            ps = self.nps()
            for j in range(4):
                kt = half * 4 + j
                self.tr(ps.ap[:, j * 128:(j + 1) * 128], hx.ap[:, kt * 128:(kt + 1) * 128], self.ident_f.ap,
                        [hx.d(), self.ident_f.d()], [ps.d()])
            src = ps.ap.rearrange("p (j q) -> p j q", j=4)
            self.cp(self.ev(), dst_b.ap[:, half * 4:(half + 1) * 4, t * 128:(t + 1) * 128], src, [ps.d()], [dst_b.d(dst_key)])
            if dst_f is not None:
                self.cp(self.ev(), dst_f.ap[:, half * 4:(half + 1) * 4, :], src, [ps.d()], [dst_f.d()])

    def moe(self, l, tiles):
        kb = self.kb
        AB, AFa = self.AB, self.AF_
        Xg, Yg = self.dr["Xg"], self.dr["Yg"]
        kb.barrier()
        mb, mf = AB.mark(), AFa.mark()
        self.load_mod(l, 1)
        wg = [AB.alloc(8 * D, [8, D]) for _ in range(2)]
        wu = [AB.alloc(8 * D, [8, D]) for _ in range(2)]
        wd = [AB.alloc(8 * D, [8, D]) for _ in range(2)]
        wsrc = {"g": self.inp["moe_w_gate"], "u": self.inp["moe_w_up"], "d": self.inp["moe_w_down"]}

        def load_w(e):
            for W, nm in ((wg[e % 2], "g"), (wu[e % 2], "u"), (wd[e % 2], "d")):
                src = wsrc[nm][l, e].rearrange("(kt p) n -> p kt n", p=128)
                for hh in range(2):
                    self.dma("pool", W.ap[:, hh * 4:(hh + 1) * 4, :], src[:, hh * 4:(hh + 1) * 4, :], [], [W.d()])

        load_w(0)
        mbA = AB.mark()
        hxs = [AFa.alloc(D) for _ in range(2)]
        hbs = [AB.alloc(D) for _ in range(2)]
        hTf = [AFa.alloc(8 * 128, [8, 128]) for _ in range(2)]
        rt = [AFa.alloc(256) for _ in range(2)]
        base = AFa.alloc(N_EXP)
        mask_b = [AB.alloc(N_EXP) for _ in range(2)]
        self.memset("dve", base.ap, 0.0, [base.d()])
        for it, t in enumerate(tiles):
            hx, hb, hf, r, mk = hxs[it % 2], hbs[it % 2], hTf[it % 2], rt[it % 2], mask_b[it % 2]
            R = r.ap
            rd = [r.d()]
            self.modulate(t, hx)
            self.cp("act", hb.ap, hx.ap, [hx.d()], [hb.d()])
            for half in range(2):
                ps = self.nps()
                for j in range(4):
                    kt = half * 4 + j
                    self.tr(ps.ap[:, j * 128:(j + 1) * 128], hx.ap[:, kt * 128:(kt + 1) * 128], self.ident_f.ap,
                            [hx.d(), self.ident_f.d()], [ps.d()])
                self.cp(self.ev(), hf.ap[:, half * 4:(half + 1) * 4, :], ps.ap.rearrange("p (j q) -> p j q", j=4), [ps.d()], [hf.d()])
            pl = self.nps()
            for kt in range(8):
                self.mm(pl.ap[:, 0:N_EXP], hf.ap[:, kt, :], self.rw.ap[:, kt, :], kt == 0, kt == 7, [hf.d(), self.rw.d()], [pl.d()])
            lg, pr, sel, tmp, oh1, oh2 = (R[:, 0:16], R[:, 16:32], R[:, 32:48], R[:, 48:64], R[:, 64:80], R[:, 80:96])
            g4, s6, sc1 = R[:, 96:100], R[:, 100:124], R[:, 124:140]
            mx, sm, rs, m1, m2, p1, p2, ps_, d1, d2 = (R[:, 140 + i:141 + i] for i in range(10))
            self.cp("dve", lg, pl.ap[:, 0:N_EXP], [pl.d()], rd)
            self.red("max", mx, lg, rd, rd)
            self.ts("dve", mx, mx, -1.0, None, ALU.mult, None, rd, rd)
            self.act(pr, lg, AF.Exp, rd, rd, bias=mx, accum=sm)
            self.recip(rs, sm, rd, rd)
            self.ts("dve", pr, pr, rs, None, ALU.mult, None, rd, rd)
            self.tt("dve", sel, pr, self.rb_bc.ap, ALU.add, rd + [self.rb_bc.d()], rd)
            sv = sel.rearrange("p (g k) -> p g k", k=4)
            s6v = s6.rearrange("p (q g) -> p q g", q=6)
            pairs = [(0, 1), (0, 2), (0, 3), (1, 2), (1, 3), (2, 3)]
            for qi, (a, b) in enumerate(pairs):
                self.tt("dve", s6v[:, qi, :], sv[:, :, a], sv[:, :, b], ALU.add, rd, rd)
            self.tt("dve", g4, s6v[:, 0, :], s6v[:, 1, :], ALU.max, rd, rd)
            for qi in range(2, 6):
                self.tt("dve", g4, g4, s6v[:, qi, :], ALU.max, rd, rd)
            self.red("max", mx, g4, rd, rd)
            self.ts("dve", g4, g4, mx, -1e30, ALU.is_lt, ALU.mult, rd, rd)
            tv = tmp.rearrange("p (g k) -> p g k", k=4)
            for k in range(4):
                self.tt("dve", tv[:, :, k], sv[:, :, k], g4, ALU.add, rd, rd)
            self.red("max", m1, tmp, rd, rd)
            self.ts("dve", oh1, tmp, m1, None, ALU.is_ge, None, rd, rd)
            self.stt(sc1, oh1, -1e30, tmp, ALU.mult, ALU.add, rd, rd)
            self.red("max", m2, sc1, rd, rd)
            self.ts("dve", oh2, sc1, m2, None, ALU.is_ge, None, rd, rd)
            self.tt("dve", sc1, pr, oh1, ALU.mult, rd, rd)
            self.red("sum", p1, sc1, rd, rd)
            self.tt("dve", sc1, pr, oh2, ALU.mult, rd, rd)
            self.red("sum", p2, sc1, rd, rd)
            self.tt("dve", ps_, p1, p2, ALU.add, rd, rd)
            self.recip(ps_, ps_, rd, rd)
            gw = self.gatew
            self.tt("dve", gw.ap[:, t, 0:1], p1, ps_, ALU.mult, rd, [gw.d(t)])
            self.tt("dve", gw.ap[:, t, 1:2], p2, ps_, ALU.mult, rd, [gw.d(t)])
            self.tt("dve", mk.ap, oh1, oh2, ALU.add, rd, [mk.d()])
            pc = self.nps()
            self.mm(pc.ap[:, 0:N_EXP], self.ltri_b.ap, mk.ap, True, True, [self.ltri_b.d(), mk.d()], [pc.d()])
            self.mm(pc.ap[:, 16:16 + N_EXP], self.ones_b.ap, mk.ap, True, True, [self.ones_b.d(), mk.d()], [pc.d()])
            self.tt("dve", sc1, pc.ap[:, 0:N_EXP], base.ap, ALU.add, [pc.d(), base.d()], rd)
            self.tt("dve", base.ap, base.ap, pc.ap[:, 16:16 + N_EXP], ALU.add, [pc.d(), base.d()], [base.d()])
            self.ts("dve", lg, sc1, float(C_CAP) - 0.5, 1.0e6, ALU.is_gt, ALU.mult, rd, rd)
            self.tt("dve", sc1, sc1, lg, ALU.add, rd, rd)
            self.tt("dve", sc1, sc1, self.eoff.ap, ALU.add, rd + [self.eoff.d()], rd)
            self.tt("dve", lg, sc1, oh1, ALU.mult, rd, rd)
            self.red("sum", d1, lg, rd, rd)
            self.tt("dve", lg, sc1, oh2, ALU.mult, rd, rd)
            self.red("sum", d2, lg, rd, rd)
            di = self.dest_i
            self.cp("dve", di.ap[:, t, 0:1], d1, rd, [di.d(t)])
            self.cp("dve", di.ap[:, t, 1:2], d2, rd, [di.d(t)])
            dg = self.dest_g
            self.ts("dve", d1, d1, float(N_EXP * C_CAP - 1), None, ALU.min, None, rd, rd)
            self.ts("dve", d2, d2, float(N_EXP * C_CAP - 1), None, ALU.min, None, rd, rd)
            self.cp("dve", dg.ap[:, t, 0:1], d1, rd, [dg.d(t)])
            self.cp("dve", dg.ap[:, t, 1:2], d2, rd, [dg.d(t)])
            for k in range(2):
                self.kb.dma("pool", lambda e, k=k, t=t, hb=hb: e.indirect_dma_start(
                    out=Xg, out_offset=bass.IndirectOffsetOnAxis(ap=di.ap[:, t, k:k + 1], axis=0),
                    in_=hb.ap, in_offset=None, bounds_check=self.bound_reg(e), oob_is_err=False),
                    [di.d(t), hb.d()], [self.ddep("Xg")])
        if "dest" in self.debug:
            pass
        kb.barrier()
        AB.release(mbA)
        AFa.release(mf)
        mf = AFa.mark()
        RB = 256
        xtm = [AB.alloc(2 * D, [2, D]) for _ in range(2)]
        xT = [AB.alloc(8 * RB, [8, RB]) for _ in range(2)]
        hT = [AB.alloc(8 * RB, [8, RB]) for _ in range(2)]
        sg = [AFa.alloc(RB) for _ in range(2)]
        yst = [AFa.alloc(D) for _ in range(2)]
        blocks = [(e, rb) for e in range(N_EXP) for rb in range(C_CAP // RB)]

        def stT(bi):
            e, rb = blocks[bi]
            X, XT = xtm[bi % 2], xT[bi % 2]
            r0 = e * C_CAP + rb * RB
            self.dma("sp", X.ap, Xg[r0:r0 + RB, :].rearrange("(j p) d -> p j d", p=128), [self.ddep("Xg")], [X.d()])
            for j in range(RB // 128):
                pb = self.npsb()
                for kt in range(8):
                    self.tr(pb.ap[:, kt * 128:(kt + 1) * 128], X.ap[:, j, kt * 128:(kt + 1) * 128], self.ident_b.ap,
                            [X.d(), self.ident_b.d()], [pb.d()])
                self.cp(self.ev(), XT.ap[:, :, j * 128:(j + 1) * 128], pb.ap.rearrange("p (k q) -> p k q", k=8), [pb.d()], [XT.d()])

        def stGU(bi):
            e, rb = blocks[bi]
            Wg, Wu = wg[e % 2], wu[e % 2]
            XT, HT = xT[bi % 2], hT[bi % 2]
            for j in range(8):
                pg, pu = self.nps(), self.nps()
                for kt in range(8):
                    self.mm(pg.ap[:, 0:RB], Wg.ap[:, kt, j * 128:(j + 1) * 128], XT.ap[:, kt, :], kt == 0, kt == 7, [Wg.d(), XT.d()], [pg.d()])
                for kt in range(8):
                    self.mm(pu.ap[:, 0:RB], Wu.ap[:, kt, j * 128:(j + 1) * 128], XT.ap[:, kt, :], kt == 0, kt == 7, [Wu.d(), XT.d()], [pu.d()])
                s_ = sg[j % 2]
                self.act(s_.ap, pg.ap[:, 0:RB], AF.Silu, [pg.d()], [s_.d()])
                self.tt("dve", HT.ap[:, j, :], s_.ap, pu.ap[:, 0:RB], ALU.mult, [s_.d(), pu.d()], [HT.d()])

        def stY(bi):
            e, rb = blocks[bi]
            Wd, HT = wd[e % 2], hT[bi % 2]
            r0 = e * C_CAP + rb * RB
            for r in range(RB // 128):
                ys = yst[r % 2]
                for c in range(2):
                    py = self.nps()
                    for j in range(8):
                        self.mm(py.ap, HT.ap[:, j, r * 128:(r + 1) * 128], Wd.ap[:, j, c * 512:(c + 1) * 512], j == 0, j == 7,
                                [HT.d(), Wd.d()], [py.d()])
                    self.cp(self.ev(), ys.ap[:, c * 512:(c + 1) * 512], py.ap, [py.d()], [ys.d()])
                rr = r0 + r * 128
                self.dma("sp", Yg[rr:rr + 128, :], ys.ap, [ys.d()], [self.ddep("Yg")])

        stT(0)
        for bi, (e, rb) in enumerate(blocks):
            if rb == 0 and e + 1 < N_EXP:
                load_w(e + 1)
            stGU(bi)
            if bi + 1 < len(blocks):
                stT(bi + 1)
            stY(bi)
        kb.barrier()
        AB.release(mb)
        AFa.release(mf)
        mb, mf = AB.mark(), AFa.mark()
        y1 = [AFa.alloc(D) for _ in range(2)]
        y2 = [AFa.alloc(D) for _ in range(2)]
        pw = self.post_work()
        di, gw, dg = self.dest_i, self.gatew, self.dest_g
        for it, t in enumerate(tiles):
            a, b = y1[it % 2], y2[it % 2]
            for k, dst in ((0, a), (1, b)):
                self.memset("pool", dst.ap, 0.0, [dst.d()])
                self.kb.dma("pool", lambda e, k=k, t=t, dst=dst: e.indirect_dma_start(
                    out=dst.ap, out_offset=None, in_=Yg,
                    in_offset=bass.IndirectOffsetOnAxis(ap=dg.ap[:, t, k:k + 1], axis=0)),
                    [dg.d(t), self.ddep("Yg")], [dst.d()])
            self.ts("dve", a.ap, a.ap, gw.ap[:, t, 0:1], None, ALU.mult, None, [a.d(), gw.d(t)], [a.d()])
            self.stt(a.ap, b.ap, gw.ap[:, t, 1:2], a.ap, ALU.mult, ALU.add, [a.d(), b.d(), gw.d(t)], [a.d()])
            self.post(t, [a.ap[:, 0:512], a.ap[:, 512:1024]], [a.d()], pw[it % 2])
        kb.barrier()
        AB.release(mb)
        AFa.release(mf)

    def s5(self, l, slot):
        kb = self.kb
        AB, AFa = self.AB, self.AF_
        kb.barrier()
        mb, mf = AB.mark(), AFa.mark()
        self.load_mod(l, 0)
        L = 32
        NCH = T_ALL // L
        chunks = [(0, 256)] + [(256 + i * 512, 512) for i in range(4)]
        yT = AB.alloc(8 * T_ALL, [8, T_ALL])
        uT = AB.alloc(8 * T_ALL, [8, T_ALL])
        mb_w = AB.mark()
        mf1 = AFa.mark()
        hxs = [AFa.alloc(D) for _ in range(2)]
        for t in range(NT):
            hx = hxs[t % 2]
            self.modulate(t, hx)
            self.transpose_tile(hx, yT, t, t)
        win = AB.alloc(8 * D, [8, D])
        w_in = self.inp["s5_w_in"][slot].rearrange("(kt p) n -> p kt n", p=128)
        for hh in range(2):
            self.dma("pool", win.ap[:, hh * 4:(hh + 1) * 4, :], w_in[:, hh * 4:(hh + 1) * 4, :], [], [win.d()])
        for ft in range(8):
            for ci, (c0, cn) in enumerate(chunks):
                ps = self.nps()
                hdeps = yT.dl(range(c0 // 128, (c0 + cn) // 128))
                for kt in range(8):
                    self.mm(ps.ap[:, 0:cn], win.ap[:, kt, ft * 128:(ft + 1) * 128], yT.ap[:, kt, c0:c0 + cn], kt == 0, kt == 7, [win.d()] + hdeps, [ps.d()])
                self.cp(self.ev(), uT.ap[:, ft, c0:c0 + cn], ps.ap[:, 0:cn], [ps.d()], [uT.d()])
        kb.barrier()
        AB.release(mb_w)
        AFa.release(mf1)
        dsk = AFa.alloc(8)
        self.dma("sp", dsk.ap, self.inp["s5_d"][slot:slot + 1, :].rearrange("o (f p) -> p (o f)", p=128), [], [dsk.d()], slow=True)
        yd = [yT.d("y")]
        for ft in range(8):
            self.ts("dve", yT.ap[:, ft, :], uT.ap[:, ft, :], dsk.ap[:, ft:ft + 1], None, ALU.mult, None, [uT.d(), dsk.d()], yd)
        tau = AFa.alloc(32)
        self.dma("sp", tau.ap, self.inp["k_tau"], [], [tau.d()])
        Bw = [AB.alloc(32 * 128, [32, 128]) for _ in range(2)]
        Cw = [AB.alloc(32 * 128, [32, 128]) for _ in range(2)]
        hb_ = [[AB.alloc(8 * L, [8, L]) for _ in range(2)] for _ in range(2)]
        mf2 = AFa.mark()
        for d in range(2):
            kb.barrier()
            AFa.release(mf2)
            stg = AFa.alloc(32 * 128, [32, 128])
            for ri, nm in enumerate(("s5_b_re", "s5_b_im")):
                self.memset("pool", stg.ap, 0.0, stg.dl(range(64)))
                for g in range(64):
                    dst = stg.ap[(g % 8) * 16:(g % 8) * 16 + 16, g // 2, (g % 2) * 64:(g % 2) * 64 + 64]
                    self.dma("sp", dst, self.inp[nm][slot, d, g].rearrange("p n -> n p"), [], [stg.d(g)], slow=True)
                self.cp("act", Bw[ri].ap, stg.ap, stg.dl(range(64)), [Bw[ri].d()])
            for ri, nm in enumerate(("s5_c_re", "s5_c_im")):
                self.memset("pool", stg.ap, 0.0, stg.dl(range(64)))
                for g in range(64):
                    dst = stg.ap[(g % 2) * 64:(g % 2) * 64 + 64, g // 2, (g % 8) * 16:(g % 8) * 16 + 16]
                    self.dma("sp", dst, self.inp[nm][slot, d, g].rearrange("n p -> p n"), [], [stg.d(g)], slow=True)
                if ri == 0:
                    self.cp("act", Cw[0].ap, stg.ap, stg.dl(range(64)), [Cw[0].d()])
                else:
                    self.ts("dve", Cw[1].ap, stg.ap, -1.0, None, ALU.mult, None, stg.dl(range(64)), [Cw[1].d()])
            kb.barrier()
            AFa.release(mf2)
            P = AFa.alloc(16 * 32, [16, 32])
            pd = [P.d()]
            ar, ai, dt, mag, ang, cs, sn, zr, zi, den, kr, ki, t0, t1, rc_r, rc_i = (P.ap[:, i, :] for i in range(16))
            for two in range(2):
                prt = slice(two * 64, two * 64 + 64)
                self.dma("sp", P.ap[prt, 0, :], self.inp["s5_a_re"][slot, d].rearrange("(s two) p -> two p s", two=2)[two], [], pd, slow=True)
                self.dma("sp", P.ap[prt, 1, :], self.inp["s5_a_im"][slot, d].rearrange("(s two) p -> two p s", two=2)[two], [], pd, slow=True)
                self.dma("sp", P.ap[prt, 2, :], self.inp["s5_log_dt"][slot, d:d + 1, :].rearrange("o (s two) -> two o s", two=2)[two].partition_broadcast(64), [], pd, slow=True)
            self.act(dt, dt, AF.Exp, pd, pd)
            self.tt("dve", mag, ar, dt, ALU.mult, pd, pd)
            self.act(mag, mag, AF.Exp, pd, pd)
            self.tt("dve", ang, ai, dt, ALU.mult, pd, pd)
            it_ = self.itmp
            self.sincos(ang, cs, sn, t0, it_.ap[:, 0:32], pd + [it_.d()], pd + [it_.d()])
            self.tt("dve", zr, mag, cs, ALU.mult, pd, pd)
            self.tt("dve", zi, mag, sn, ALU.mult, pd, pd)
            self.tt("dve", den, ar, ar, ALU.mult, pd, pd)
            self.tt("dve", t0, ai, ai, ALU.mult, pd, pd)
            self.tt("dve", den, den, t0, ALU.add, pd, pd)
            self.recip(den, den, pd, pd)
            self.ts("dve", zr, zr, -1.0, None, ALU.add, None, pd, pd)
            self.tt("dve", kr, zr, ar, ALU.mult, pd, pd)
            self.tt("dve", t0, zi, ai, ALU.mult, pd, pd)
            self.tt("dve", kr, kr, t0, ALU.add, pd, pd)
            self.tt("dve", kr, kr, den, ALU.mult, pd, pd)
            self.tt("dve", ki, zi, ar, ALU.mult, pd, pd)
            self.tt("dve", t0, zr, ai, ALU.mult, pd, pd)
            self.tt("dve", ki, ki, t0, ALU.subtract, pd, pd)
            self.tt("dve", ki, ki, den, ALU.mult, pd, pd)
            NTB = 32 * L
            Ct, St, T1r, T1i, rz, tmp = (AFa.alloc(NTB) for _ in range(6))
            td = [Ct.d()]
            v3 = lambda tl: tl.ap.rearrange("p (s l) -> p s l", s=32)
            bc_s = lambda a2: a2.unsqueeze(2).to_broadcast([128, 32, L])
            self.tt("dve", v3(tmp), bc_s(ang), tau.ap.unsqueeze(1).to_broadcast([128, 32, L]), ALU.mult, pd + [tau.d()], td)
            self.sincos(tmp.ap, Ct.ap, St.ap, T1r.ap, it_.ap[:, 0:NTB], td + [it_.d()], td + [it_.d()])
            self.tt("dve", v3(T1r), v3(Ct), bc_s(kr), ALU.mult, td + pd, td)
            self.tt("dve", v3(tmp), v3(St), bc_s(ki), ALU.mult, td + pd, td)
            self.tt("dve", T1r.ap, T1r.ap, tmp.ap, ALU.add, td, td)
            self.tt("dve", v3(T1i), v3(Ct), bc_s(ki), ALU.mult, td + pd, td)
            self.tt("dve", v3(tmp), v3(St), bc_s(kr), ALU.mult, td + pd, td)
            self.tt("dve", T1i.ap, T1i.ap, tmp.ap, ALU.subtract, td, td)
            self.cp("dve", v3(rz), bc_s(mag), td + pd, td)
            self.memset("dve", v3(rz)[:, :, 0], 0.0, td)
            self.memset("dve", rc_r, 0.0, pd)
            self.memset("dve", rc_i, 0.0, pd)
            wk = [AFa.alloc(8 * L, [8, L]) for _ in range(6)]
            gbuf = [[AFa.alloc(8 * L, [8, L]) for _ in range(2)] for _ in range(2)]
            f2 = lambda tl: tl.ap.rearrange("p s l -> p (s l)")

            def mk_tok(lo_, rev):
                def tok(ap2):
                    v = ap2[:, lo_:lo_ + L]
                    return v[:, ::-1] if rev else v
                return tok

            units = []
            for c in range(NCH):
                if d == 0:
                    lo = c * L
                elif c < T_CTX // L:
                    lo = T_CTX - (c + 1) * L
                else:
                    lo = T_ALL - (c - T_CTX // L + 1) * L
                for fp in range(4):
                    units.append(dict(c=c, fp=fp, tok=mk_tok(lo, d == 1), idx=c * 4 + fp))

            def stA(u):
                fp, tok = u["fp"], u["tok"]
                pbu = self.nps()
                u["pbu"] = pbu
                for s8 in range(8):
                    s_ = 8 * fp + s8
                    rhs = tok(uT.ap[:, s_ // 4, :])
                    self.mm(pbu.ap[:, s8 * L:(s8 + 1) * L], Bw[0].ap[:, s_, :], rhs, True, True, [Bw[0].d(), uT.d()], [pbu.d()])
                    self.mm(pbu.ap[:, 256 + s8 * L:256 + (s8 + 1) * L], Bw[1].ap[:, s_, :], rhs, True, True, [Bw[1].d(), uT.d()], [pbu.d()])

            def stB(u):
                fp, c, pbu = u["fp"], u["c"], u["pbu"]
                m1, m2, br, bi, m3, m4 = wk
                gr, gi = gbuf[u["idx"] % 2]
                u["g"] = (gr, gi)
                ss = slice(8 * fp, 8 * fp + 8)
                fl = slice(8 * fp * L, (8 * fp + 8) * L)
                bur = pbu.ap[:, 0:256].rearrange("p (s l) -> p s l", s=8)
                bui = pbu.ap[:, 256:512].rearrange("p (s l) -> p s l", s=8)
                t1r, t1i = v3(T1r)[:, ss, :], v3(T1i)[:, ss, :]
                self.tt("dve", m1.ap, bur, t1r, ALU.mult, [pbu.d()] + td, [m1.d()])
                self.tt("dve", m2.ap, bui, t1i, ALU.mult, [pbu.d()] + td, [m2.d()])
                self.tt("dve", br.ap, m1.ap, m2.ap, ALU.subtract, [m1.d(), m2.d()], [br.d()])
                self.tt("dve", m1.ap, bui, t1r, ALU.mult, [pbu.d()] + td, [m1.d()])
                self.tt("dve", m2.ap, bur, t1i, ALU.mult, [pbu.d()] + td, [m2.d()])
                self.tt("dve", bi.ap, m1.ap, m2.ap, ALU.add, [m1.d(), m2.d()], [bi.d()])
                if c > 0:
                    self.tt("dve", br.ap[:, :, 0], br.ap[:, :, 0], rc_r[:, ss], ALU.add, [br.d(), rcd[fp]], [br.d()])
                    self.tt("dve", bi.ap[:, :, 0], bi.ap[:, :, 0], rc_i[:, ss], ALU.add, [bi.d(), rcd[fp]], [bi.d()])
                self.scan(f2(gr), rz.ap[:, fl], f2(br), 0.0, [br.d()] + td, [gr.d()])
                self.scan(f2(gi), rz.ap[:, fl], f2(bi), 0.0, [bi.d()] + td, [gi.d()])

            def stC(u):
                fp = u["fp"]
                m1, m2, br, bi, m3, m4 = wk
                gr, gi = u["g"]
                hrb, hib = hb_[u["idx"] % 2]
                u["h"] = (hrb, hib)
                ss = slice(8 * fp, 8 * fp + 8)
                ct, st = v3(Ct)[:, ss, :], v3(St)[:, ss, :]
                gd = [gr.d(), gi.d()]
                self.tt("pool", m3.ap, gr.ap, ct, ALU.mult, gd + td, [m3.d()])
                self.tt("pool", m4.ap, gi.ap, st, ALU.mult, gd + td, [m4.d()])
                self.tt("pool", hrb.ap, m3.ap, m4.ap, ALU.subtract, [m3.d(), m4.d()], [hrb.d()])
                self.tt("pool", m3.ap, gr.ap, st, ALU.mult, gd + td, [m3.d()])
                self.tt("pool", m4.ap, gi.ap, ct, ALU.mult, gd + td, [m4.d()])
                self.tt("pool", hib.ap, m3.ap, m4.ap, ALU.add, [m3.d(), m4.d()], [hib.d()])

            def stD(u):
                fp = u["fp"]
                hrb, hib = u["h"]
                ss = slice(8 * fp, 8 * fp + 8)
                self.tt("dve", rc_r[:, ss], hrb.ap[:, :, L - 1], mag[:, ss], ALU.mult, [hrb.d()] + pd, [rcd[fp]])
                self.tt("dve", rc_i[:, ss], hib.ap[:, :, L - 1], mag[:, ss], ALU.mult, [hib.d()] + pd, [rcd[fp]])

            def stE(u):
                fp, tok = u["fp"], u["tok"]
                hrb, hib = u["h"]
                po = self.nps()
                for q in range(2):
                    ft = 2 * fp + q
                    for s4 in range(4):
                        s_ = 4 * ft + s4
                        s8 = q * 4 + s4
                        self.mm(po.ap[:, q * L:(q + 1) * L], Cw[0].ap[:, s_, :], hrb.ap[:, s8, :], s4 == 0, False, [Cw[0].d(), hrb.d()], [po.d()])
                        self.mm(po.ap[:, q * L:(q + 1) * L], Cw[1].ap[:, s_, :], hib.ap[:, s8, :], False, s4 == 3, [Cw[1].d(), hib.d()], [po.d()])
                for q in range(2):
                    ft = 2 * fp + q
                    ysl = tok(yT.ap[:, ft, :])
                    self.tt("dve", ysl, ysl, po.ap[:, q * L:(q + 1) * L], ALU.add, [po.d()] + yd, yd)

            rcd = [Dep() for _ in range(4)]
            NU = len(units)
            for i in range(NU + 2):
                if i < NU:
                    stA(units[i])
                    stB(units[i])
                if i >= 2:
                    stE(units[i - 2])
                if i < NU:
                    stC(units[i])
                if 1 <= i <= NU:
                    stD(units[i - 1])
        kb.barrier()
        AB.release(mb_w)
        AFa.release(mf1)
        gw = [(AFa.alloc(512), AFa.alloc(512)) for _ in range(2)]
        i = 0
        for ft in range(8):
            for (c0, cn) in chunks:
                a_, b_ = gw[i % 2]
                i += 1
                self.gelu_tanh(yT.ap[:, ft, c0:c0 + cn], yT.ap[:, ft, c0:c0 + cn], yd, ((a_, a_.ap[:, 0:cn]), (b_, b_.ap[:, 0:cn])), yd)
        wv = AB.alloc(8 * D, [8, D])
        wg = AB.alloc(8 * D, [8, D])
        for W, nm in ((wv, "s5_glu_v"), (wg, "s5_glu_g")):
            src = self.inp[nm][slot].rearrange("(kt p) n -> p kt n", p=128)
            for hh in range(2):
                self.dma("pool", W.ap[:, hh * 4:(hh + 1) * 4, :], src[:, hh * 4:(hh + 1) * 4, :], [], [W.d()])
        ys = [AFa.alloc(D) for _ in range(2)]
        sg = [AFa.alloc(512) for _ in range(2)]
        pw = self.post_work()
        for t in range(NT):
            yt = ys[t % 2]
            for c in range(2):
                pv, pg = self.nps(), self.nps()
                for kt in range(8):
                    self.mm(pv.ap, yT.ap[:, kt, t * 128:(t + 1) * 128], wv.ap[:, kt, c * 512:(c + 1) * 512], kt == 0, kt == 7, yd + [wv.d()], [pv.d()])
                for kt in range(8):
                    self.mm(pg.ap, yT.ap[:, kt, t * 128:(t + 1) * 128], wg.ap[:, kt, c * 512:(c + 1) * 512], kt == 0, kt == 7, yd + [wg.d()], [pg.d()])
                s_ = sg[c]
                self.act(s_.ap, pg.ap, AF.Sigmoid, [pg.d()], [s_.d()])
                self.tt("dve", yt.ap[:, c * 512:(c + 1) * 512], pv.ap, s_.ap, ALU.mult, [pv.d(), s_.d()], [yt.d()])
            self.post(t, [yt.ap[:, 0:512], yt.ap[:, 512:1024]], [yt.d()], pw[t % 2])
        kb.barrier()
        AB.release(mb)
        AFa.release(mf)

    def rglru(self, l, slot):
        kb = self.kb
        AB, AFa = self.AB, self.AF_
        gT = self.dr["gT"]
        kb.barrier()
        mb, mf = AB.mark(), AFa.mark()
        self.load_mod(l, 0)
        NJ = 11
        w_in = self.inp["lru_w_in"][slot].rearrange("(kt p) n -> p kt n", p=128)
        chunks = [(0, 256)] + [(256 + i * 512, 512) for i in range(4)]
        xsT = AB.alloc(12 * T_ALL, [12, T_ALL])
        mb_h = AB.mark()
        hT = AB.alloc(8 * T_ALL, [8, T_ALL])
        convw = AFa.alloc(NJ * 4, [NJ, 4])
        convb = AFa.alloc(NJ)
        ba = AFa.alloc(2 * NJ, [2, NJ])
        bx = AFa.alloc(2 * NJ, [2, NJ])
        c8 = AFa.alloc(2 * NJ, [2, NJ])
        for k in range(4):
            self.dma("sp", convw.ap[:, :, k], self.inp["lru_conv_w"][slot, k:k + 1, :].rearrange("o (j p) -> p (o j)", p=128), [], [convw.d()], slow=True)
        self.dma("sp", convb.ap, self.inp["lru_conv_b"][slot:slot + 1, :].rearrange("o (j p) -> p (o j)", p=128), [], [convb.d()], slow=True)
        for tl, nm in ((ba, "lru_b_a"), (bx, "lru_b_x"), (c8, "lru_lam")):
            for d in range(2):
                self.dma("sp", tl.ap[:, d, :], self.inp[nm][slot, d:d + 1, :].rearrange("o (j p) -> p (o j)", p=128), [], [tl.d()], slow=True)
        self.act(c8.ap, c8.ap, AF.Exp, [c8.d()], [c8.d()], scale=-1.0)
        self.act(c8.ap, c8.ap, AF.Ln, [c8.d()], [c8.d()], bias=1.0)
        self.ts("dve", c8.ap, c8.ap, -8.0, None, ALU.mult, None, [c8.d()], [c8.d()])
        mf1 = AFa.mark()
        hxs = [AFa.alloc(D) for _ in range(2)]
        for t in range(NT):
            hx = hxs[t % 2]
            self.modulate(t, hx)
            self.transpose_tile(hx, hT, t, t)
        kb.barrier()
        AFa.release(mf1)
        raw = AFa.alloc(T_ALL)
        acc = AFa.alloc(T_ALL)
        gw = [(AFa.alloc(512), AFa.alloc(512)) for _ in range(2)]
        wt = [AB.alloc(8 * 128, [8, 128]) for _ in range(2)]
        gtile = [AB.alloc(T_ALL) for _ in range(2)]
        for o in range(2 * NJ):
            W = wt[o % 2]
            cols = o * 128 if o < NJ else LRU_W + (o - NJ) * 128
            self.dma("pool", W.ap, w_in[:, :, cols:cols + 128], [], [W.d()])
            gt = gtile[o % 2]
            for ci, (c0, cn) in enumerate(chunks):
                ps = self.nps()
                hdeps = hT.dl(range(c0 // 128, (c0 + cn) // 128))
                for kt in range(8):
                    self.mm(ps.ap[:, 0:cn], W.ap[:, kt, :], hT.ap[:, kt, c0:c0 + cn], kt == 0, kt == 7, [W.d()] + hdeps, [ps.d()])
                if o < NJ:
                    a_, b_ = gw[ci % 2]
                    self.gelu_tanh(gt.ap[:, c0:c0 + cn], ps.ap[:, 0:cn], [ps.d()], ((a_, a_.ap[:, 0:cn]), (b_, b_.ap[:, 0:cn])), [gt.d()])
                else:
                    self.cp(self.ev(), raw.ap[:, c0:c0 + cn], ps.ap[:, 0:cn], [ps.d()], [raw.d()])
            if o < NJ:
                self.dma("sp", gT[o], gt.ap, [gt.d()], [self.ddep("gT", o)])
            else:
                j = o - NJ
                rd_, ad_ = [raw.d()], [acc.d()]
                self.ts("dve", acc.ap, raw.ap, convw.ap[:, j, 1:2], convb.ap[:, j:j + 1], ALU.mult, ALU.add, rd_ + [convw.d(), convb.d()], ad_)
                for (s0, s1) in ((0, T_CTX), (T_CTX, T_ALL)):
                    self.stt(acc.ap[:, s0 + 1:s1], raw.ap[:, s0:s1 - 1], convw.ap[:, j, 0:1], acc.ap[:, s0 + 1:s1], ALU.mult, ALU.add, rd_ + ad_, ad_)
                    self.stt(acc.ap[:, s0:s1 - 1], raw.ap[:, s0 + 1:s1], convw.ap[:, j, 2:3], acc.ap[:, s0:s1 - 1], ALU.mult, ALU.add, rd_ + ad_, ad_)
                    self.stt(acc.ap[:, s0:s1 - 2], raw.ap[:, s0 + 2:s1], convw.ap[:, j, 3:4], acc.ap[:, s0:s1 - 2], ALU.mult, ALU.add, rd_ + ad_, ad_)
                self.cp("act", xsT.ap[:, j + 1, :], acc.ap, ad_, [xsT.d(j + 1)])
        kb.barrier()
        AB.release(mb_h)
        AFa.release(mf1)
        bands = {}
        for nm in ("lru_w_a", "lru_w_x"):
            for d in range(2):
                bt = AB.alloc(NJ * 3 * 128, [NJ, 3, 128])
                bands[(nm, d)] = bt
                bt.pieces = []
                self.memset("dve", bt.ap, 0.0, [bt.d()])
                wsrc = self.inp[nm]
                for n in range(16):
                    r0, r1 = 88 * n, 88 * n + 88
                    tl_ = list(range(r0 // 128, (r1 - 1) // 128 + 1))
                    for kt in tl_:
                        ra, rb = max(r0, kt * 128), min(r1, (kt + 1) * 128)
                        for j in tl_:
                            ca, cb = max(r0, j * 128), min(r1, (j + 1) * 128)
                            dst = bt.ap[ra - kt * 128:rb - kt * 128, j, kt - j + 1, ca - j * 128:cb - j * 128]
                            src = wsrc[slot, d, n, ra - r0:rb - r0, ca - r0:cb - r0]
                            pd_ = bt.d(("pc", n, kt, j))
                            bt.pieces.append(pd_)
                            self.dma("pool", dst, src, [bt.d()], [pd_])
        gtile = [AB.alloc(T_ALL) for _ in range(2)]
        h0 = AFa.alloc(T_ALL)
        h1 = AFa.alloc(T_ALL)
        wk = [[AFa.alloc(512) for _ in range(4)] for _ in range(2)]
        lat_rev = [(256 + i * 512, 512) for i in (3, 2, 1, 0)]
        it = 0
        for j in range(NJ):
            gt = gtile[j % 2]
            self.dma("sp", gt.ap, gT[j], [self.ddep("gT", j)], [gt.d()])
            nb = [kt for kt in (j - 1, j, j + 1) if 0 <= kt < NJ]
            for d in range(2):
                hb = h0 if d == 0 else h1
                order = chunks if d == 0 else [(0, 256)] + lat_rev
                Ba, Bx = bands[("lru_w_a", d)], bands[("lru_w_x", d)]
                for (c0, cn) in order:
                    R, I, A, S = wk[it % 2]
                    it += 1
                    pa, px = self.nps(), self.nps()
                    xdeps = [xsT.d(kt + 1) for kt in nb]
                    for idx, kt in enumerate(nb):
                        self.mm(pa.ap[:, 0:cn], Ba.ap[:, j, kt - j + 1, :], xsT.ap[:, kt + 1, c0:c0 + cn], idx == 0, idx == len(nb) - 1, [Ba.d()] + Ba.pieces + xdeps, [pa.d()])
                    for idx, kt in enumerate(nb):
                        self.mm(px.ap[:, 0:cn], Bx.ap[:, j, kt - j + 1, :], xsT.ap[:, kt + 1, c0:c0 + cn], idx == 0, idx == len(nb) - 1, [Bx.d()] + Bx.pieces + xdeps, [px.d()])
                    Ra, Ia, Aa, Sa = R.ap[:, 0:cn], I.ap[:, 0:cn], A.ap[:, 0:cn], S.ap[:, 0:cn]
                    self.act(Ra, pa.ap[:, 0:cn], AF.Sigmoid, [pa.d(), ba.d()], [R.d()], bias=ba.ap[:, d, j:j + 1])
                    self.act(Ia, px.ap[:, 0:cn], AF.Sigmoid, [px.d(), bx.d()], [I.d()], bias=bx.ap[:, d, j:j + 1])
                    self.act(Aa, Ra, AF.Exp, [R.d(), c8.d()], [A.d()], scale=c8.ap[:, d, j:j + 1])
                    self.tt("dve", Sa, Aa, Aa, ALU.mult, [A.d()], [S.d()])
                    self.act(Sa, Sa, AF.Sqrt, [S.d()], [S.d()], scale=-1.0, bias=1.0)
                    self.tt("dve", Ia, Ia, xsT.ap[:, j + 1, c0:c0 + cn], ALU.mult, [I.d(), xsT.d(j + 1)], [I.d()])
                    self.tt("pool", Ia, Ia, Sa, ALU.mult, [I.d(), S.d()], [I.d()])
                    if d == 0:
                        init = 0.0 if c0 == 0 else hb.ap[:, c0 - 1:c0]
                        self.scan(hb.ap[:, c0:c0 + cn], Aa, Ia, init, [A.d(), I.d(), hb.d()], [hb.d()])
                    else:
                        if c0 == 0:
                            init = 0.0
                        elif c0 + cn == T_ALL:
                            init = hb.ap[:, 0:1]
                        else:
                            init = hb.ap[:, c0 + cn:c0 + cn + 1]
                        self.scan(hb.ap[:, c0:c0 + cn][:, ::-1], Aa[:, ::-1], Ia[:, ::-1], init, [A.d(), I.d(), hb.d()], [hb.d()])
            self.tt("dve", h0.ap, h0.ap, h1.ap, ALU.add, [h0.d(), h1.d()], [h0.d()])
            self.tt("dve", xsT.ap[:, j, :], h0.ap, gt.ap, ALU.mult, [h0.d(), gt.d()], [xsT.d(j)])
        kb.barrier()
        AFa.release(mf1)
        w_out = self.inp["lru_w_out"][slot].rearrange("(kt p) n -> p kt n", p=128)
        wo = AB.alloc(NJ * D, [NJ, D])
        for (k0, k1) in ((0, 4), (4, 8), (8, 11)):
            self.dma("pool", wo.ap[:, k0:k1, :], w_out[:, k0:k1, :], [], [wo.d()])
        pw = self.post_work()
        zdeps = [xsT.d(j) for j in range(NJ)]
        for t in range(NT):
            pys = []
            for c in range(2):
                py = self.nps()
                for kt in range(NJ):
                    self.mm(py.ap, xsT.ap[:, kt, t * 128:(t + 1) * 128], wo.ap[:, kt, c * 512:(c + 1) * 512], kt == 0, kt == NJ - 1, zdeps + [wo.d()], [py.d()])
                pys.append(py)
            self.post(t, [pys[0].ap, pys[1].ap], [pys[0].d(), pys[1].d()], pw[t % 2])
        kb.barrier()
        AB.release(mb)
        AFa.release(mf)

    def attention(self, l, slot, lam_init, last):
        kb = self.kb
        AB, AFa = self.AB, self.AF_
        kb.barrier()
        mb, mf = AB.mark(), AFa.mark()
        self.load_mod(l, 0)
        w_in = self.inp["attn_w_in"][slot].rearrange("(kt p) n -> p kt n", p=128)
        w_inp = self.inp["attn_w_in_p"][slot].rearrange("(kt p) n -> p kt n", p=128)
        w_out = self.inp["attn_w_out"][slot].rearrange("(kt p) n -> p kt n", p=128)
        hT = AB.alloc(8 * T_ALL, [8, T_ALL])
        oT = AB.alloc(8 * T_ALL, [8, T_ALL])
        mf0 = AFa.mark()
        hxs = [AFa.alloc(D) for _ in range(2)]
        for t in range(NT):
            hx = hxs[t % 2]
            self.modulate(t, hx)
            self.transpose_tile(hx, hT, t, t)
        kb.barrier()
        AFa.release(mf0)
        cosT = AFa.alloc(T_ALL)
        sinT = AFa.alloc(T_ALL)
        self.dma("sp", cosT.ap, self.inp["k_cos"], [], [cosT.d()])
        self.dma("sp", sinT.ap, self.inp["k_sin"], [], [sinT.d()])
        lamt = AFa.alloc(256 + 16)
        L = lamt.ap
        ld = [lamt.d()]
        self.dma("sp", L[:, 0:256], self.inp["attn_lam"][slot:slot + 1, :].partition_broadcast(128), [], ld)
        self.tt("dve", L[:, 0:64], L[:, 0:64], L[:, 64:128], ALU.mult, ld, ld)
        self.tt("dve", L[:, 128:192], L[:, 128:192], L[:, 192:256], ALU.mult, ld, ld)
        self.red("sum", L[:, 256:257], L[:, 0:64], ld, ld)
        self.red("sum", L[:, 257:258], L[:, 128:192], ld, ld)
        self.act(L[:, 258:260], L[:, 256:258], AF.Exp, ld, ld)
        self.tt("dve", L[:, 260:261], L[:, 258:259], L[:, 259:260], ALU.subtract, ld, ld)
        self.ts("dve", L[:, 261:262], L[:, 260:261], float(lam_init), -1.0, ALU.add, ALU.mult, ld, ld)
        neglam = L[:, 261:262]
        gsub = AFa.alloc(16)
        self.dma("sp", gsub.ap[:, 0:1], self.inp["attn_subln"][slot:slot + 1, :].rearrange("o e -> e o"), [], [gsub.d()], slow=True)
        self.ts("dve", gsub.ap[:, 0:1], gsub.ap[:, 0:1], float(1.0 - lam_init), None, ALU.mult, None, [gsub.d()], [gsub.d()])
        chunks = [(0, 256)] + [(256 + i * 512, 512) for i in range(4)]
        qk = [AB.alloc(T_ALL) for _ in range(4)]
        vext = AB.alloc(NT * 2 * 130, [NT, 2, 130])
        mbw = AB.mark()
        wqk = AB.alloc(8 * 512, [8, 4, 128])
        wqkp = AB.alloc(8 * 512, [8, 4, 128])
        wv = AB.alloc(8 * 256, [8, 256])
        ework = [AB.alloc(512) for _ in range(3)]
        ta = [AFa.alloc(512) for _ in range(2)]
        tb = [AFa.alloc(512) for _ in range(2)]
        fw = AFa.alloc(5 * 512)
        qchunks = chunks[1:] if last else chunks
        self.ps_n = 3
        self.memset("dve", vext.ap, 1.0, [vext.d()])
        qtiles = list(range(2, NT)) if last else list(range(NT))
        it_e = 0
        it_f = 0
        for hp in range(4):
            for j in range(4):
                m, isk = j % 2, j // 2
                c0 = isk * 1024 + m * 512 + hp * 128
                self.dma("pool", wqk.ap[:, :, j, :], w_in[:, :, c0:c0 + 128], [], [wqk.d()])
                self.dma("pool", wqkp.ap[:, :, j, :], w_inp[:, :, c0:c0 + 128], [], [wqkp.d()])
            self.dma("pool", wv.ap, w_in[:, :, 2048 + hp * 256:2048 + (hp + 1) * 256], [], [wv.d()])
            for j in range(4):
                for ci, (c0, cn) in enumerate(chunks):
                    pa, pb_ = self.nps(), self.nps()
                    hdeps = hT.dl(range(c0 // 128, (c0 + cn) // 128))
                    for kt in range(8):
                        self.mm(pa.ap[:, 0:cn], wqk.ap[:, kt, j, :], hT.ap[:, kt, c0:c0 + cn], kt == 0, kt == 7, [wqk.d()] + hdeps, [pa.d()])
                    for kt in range(8):
                        self.mm(pb_.ap[:, 0:cn], wqkp.ap[:, kt, j, :], hT.ap[:, kt, c0:c0 + cn], kt == 0, kt == 7, [wqkp.d()] + hdeps, [pb_.d()])
                    a_, b_ = ta[ci % 2], tb[ci % 2]
                    self.tt("dve", a_.ap[:, 0:cn], pa.ap[:, 0:cn], cosT.ap[:, c0:c0 + cn], ALU.mult, [pa.d(), cosT.d()], [a_.d()])
                    self.tt("dve", b_.ap[:, 0:cn], pb_.ap[:, 0:cn], sinT.ap[:, c0:c0 + cn], ALU.mult, [pb_.d(), sinT.d()], [b_.d()])
                    self.tt("pool", qk[j].ap[:, c0:c0 + cn], a_.ap[:, 0:cn], b_.ap[:, 0:cn], ALU.add, [a_.d(), b_.d()], [qk[j].d(ci)])
            for t in range(NT):
                pv = self.nps()
                for kt in range(8):
                    self.mm(pv.ap[:, 0:256], hT.ap[:, kt, t * 128:(t + 1) * 128], wv.ap[:, kt, :], kt == 0, kt == 7, [hT.d(t), wv.d()], [pv.d()])
                self.cp(self.ev(), vext.ap[:, t, :, 0:128], pv.ap[:, 0:256].rearrange("p (h e) -> p h e", h=2), [pv.d()], [vext.d()])
            qkd = [qk[j].dl(range(5)) for j in range(4)]
            items = []
            for hh in range(2):
                for (c0, cn) in qchunks:
                    kts = [0, 1] if c0 == 0 else list(range(NT))
                    for m in range(2):
                        for i, kt in enumerate(kts):
                            items.append((hh, c0, cn, m, kt, i == 0, i == len(kts) - 1, m == 1 and i == len(kts) - 1))

            def finalize(hh, c0, cn):
                head = hp * 2 + hh
                n0, d0, n1, d1 = self.ps[3], self.ps[4], self.ps[5], self.ps[6]
                wdp = [fw.d()]
                r1, r2, t1, o, sqv = (fw.ap[:, i * 512:i * 512 + cn] for i in range(5))
                self.recip(r1, d0.ap[:, 0:cn], [d0.d()], wdp)
                self.recip(r2, d1.ap[:, 0:cn], [d1.d()], wdp)
                self.ts("dve", r2, r2, neglam, None, ALU.mult, None, wdp + ld, wdp)
                self.tt("dve", t1, n0.ap[:, 0:cn], r1, ALU.mult, [n0.d()] + wdp, wdp)
                self.tt("dve", r2, n1.ap[:, 0:cn], r2, ALU.mult, [n1.d()] + wdp, wdp)
                self.tt("dve", o, r2, t1, ALU.add, wdp, wdp)
                self.tt("pool", sqv, o, o, ALU.mult, wdp, wdp)
                pss = self.nps()
                self.mm(pss.ap[:, 0:cn], self.ones_f.ap, sqv, True, True, wdp + [self.ones_f.d()], [pss.d()])
                self.ts("dve", r1, pss.ap[:, 0:cn], 1.0 / 128.0, float(LN_EPS), ALU.mult, ALU.add, [pss.d()] + wdp, wdp)
                self.act(r1, r1, AF.Sqrt, wdp, wdp)
                self.recip(r1, r1, wdp, wdp)
                self.stt(oT.ap[:, head, c0:c0 + cn], o, gsub.ap[:, 0:1], r1, ALU.mult, ALU.mult, wdp + [gsub.d()],
                         [oT.d(t) for t in range(c0 // 128, (c0 + cn) // 128)])

            def do_pv(item, E):
                hh, c0, cn, m, kt, first, lastk, unit_end = item
                num, den = self.ps[3 + 2 * m], self.ps[4 + 2 * m]
                self.mm(num.ap[:, 0:cn], vext.ap[:, kt, hh, 0:128], E.ap[:, 0:cn], first, lastk, [E.d(), vext.d()], [num.d()])
                self.mm(den.ap[:, 0:cn], self.ones_b.ap, E.ap[:, 0:cn], first, lastk, [E.d(), self.ones_b.d()], [den.d()])
                if unit_end:
                    finalize(hh, c0, cn)

            pend = []
            for item in items:
                hh, c0, cn, m, kt = item[0:5]
                prow = slice(hh * 64, (hh + 1) * 64)
                Q, K = qk[m], qk[2 + m]
                S = self.nps()
                self.mm(S.ap[:, 0:cn], K.ap[prow, kt * 128:(kt + 1) * 128], Q.ap[prow, c0:c0 + cn], True, True,
                        qkd[m] + qkd[2 + m], [S.d()])
                E = ework[it_e % 3]
                it_e += 1
                self.act(E.ap[:, 0:cn], S.ap[:, 0:cn], AF.Exp, [S.d()], [E.d()], scale=0.125)
                pend.append((item, E))
                if len(pend) > 2:
                    do_pv(*pend.pop(0))
            while pend:
                do_pv(*pend.pop(0))
        kb.barrier()
        self.ps_n = 7
        AB.release(mbw)
        AFa.release(mf0)
        wo = AB.alloc(8 * D, [8, D])
        for hh in range(2):
            self.dma("pool", wo.ap[:, hh * 4:(hh + 1) * 4, :], w_out[:, hh * 4:(hh + 1) * 4, :], [], [wo.d()])
        pw = self.post_work()
        for it, t in enumerate(qtiles):
            pys = []
            for c in range(2):
                py = self.nps()
                for h in range(8):
                    self.mm(py.ap, oT.ap[:, h, t * 128:(t + 1) * 128], wo.ap[:, h, c * 512:(c + 1) * 512], h == 0, h == 7, [oT.d(t), wo.d()], [py.d()])
                pys.append(py)
            self.post(t, [pys[0].ap, pys[1].ap], [pys[0].d(), pys[1].d()], pw[it % 2])
        kb.barrier()
        AB.release(mb)
        AFa.release(mf)


def _consts():
    ident = np.eye(128, dtype=np.float32)
    ltri = (np.arange(128)[:, None] < np.arange(128)[None, :]).astype(np.float32)
    eoff = np.broadcast_to((np.arange(N_EXP) * C_CAP).astype(np.float32)[None, :], (128, N_EXP)).copy()
    half = 16
    freq = (10000.0 ** (-np.arange(half, dtype=np.float32) / half)).astype(np.float32)
    tpos = np.arange(T_LAT)
    row = (tpos // 64).astype(np.float32)
    col = (tpos % 64).astype(np.float32)
    cos = np.ones((128, T_ALL), np.float32)
    sin = np.zeros((128, T_ALL), np.float32)
    for p in range(128):
        d = p % 64
        pos = row if d < 32 else col
        dd = d % 32
        ang = (pos * freq[dd % 16]).astype(np.float32)
        cos[p, T_CTX:] = np.cos(ang)
        s = np.sin(ang)
        sin[p, T_CTX:] = -s if dd < 16 else s
    tau = np.broadcast_to(np.arange(1, 33, dtype=np.float32)[None, :], (128, 32)).copy()
    return dict(k_ident=ident, k_ltri=ltri, k_eoff=eoff, k_cos=cos, k_sin=sin, k_tau=tau)


def _perm_qk(w_in):
    qk = w_in[:, :, :2 * D]
    s = qk.shape
    v = qk.reshape(s[0], s[1], -1, 2, 16)
    return np.ascontiguousarray(v[:, :, :, ::-1, :].reshape(s))


_CACHE = {}


def _host_inputs(inputs):
    f = lambda a: np.ascontiguousarray(np.asarray(a, dtype=np.float32))
    shared = dict(
        w_ada=f(inputs["w_ada"]), b_ada=f(inputs["b_ada"]), ln_g=f(inputs["ln_g"]), ln_b=f(inputs["ln_b"]),
        attn_w_in=f(inputs["attn_w_in"]), attn_w_in_p=_perm_qk(f(inputs["attn_w_in"])), attn_w_out=f(inputs["attn_w_out"]),
        attn_lam=f(inputs["attn_lam"]).reshape(2, 256), attn_subln=f(inputs["attn_subln"]),
        router_w=f(inputs["router_w"]), router_b=f(inputs["router_b"]).reshape(1, N_EXP),
        moe_w_gate=f(inputs["moe_w_gate"]), moe_w_up=f(inputs["moe_w_up"]), moe_w_down=f(inputs["moe_w_down"]),
    )
    for nm in ("s5_w_in", "s5_a_re", "s5_a_im", "s5_b_re", "s5_b_im", "s5_c_re", "s5_c_im", "s5_log_dt", "s5_d", "s5_glu_v",
               "s5_glu_g", "lru_w_in", "lru_conv_w", "lru_conv_b", "lru_w_a", "lru_b_a", "lru_w_x", "lru_b_x", "lru_lam", "lru_w_out"):
        shared[nm] = f(inputs[nm])
    shared.update(_consts())
    return shared


def kernel(**inputs):
    shared = _host_inputs(inputs)
    x = np.asarray(inputs["x"], np.float32)
    ctx = np.asarray(inputs["ctx"], np.float32)
    c = np.asarray(inputs["c"], np.float32)
    c_ctx = np.asarray(inputs["c_ctx"], np.float32)
    prog = Prog()
    nc = prog.build()
    in_maps = []
    for b in range(NCORES):
        m = dict(shared)
        m["x"] = np.ascontiguousarray(x[b])
        m["ctx"] = np.ascontiguousarray(ctx[b])
        m["cc"] = np.ascontiguousarray(np.stack([c[b], c_ctx]))
        in_maps.append(m)
    res = run_bass_kernel_spmd(nc, in_maps, core_ids=list(range(NCORES)))
    return np.stack([np.asarray(r["out"], np.float32) for r in res.results])
```
